# Optimizing a Trainium2 kernel written in Bass

```python
import jax, jax.numpy as jnp
from jax import lax
import numpy as np

D_MODEL = 1024
BATCH = 8
SEQ = 2048
DEPTH = 2

CONV_DIM = 512
CONV_WIDTH = 3
RET_HEADS = 4
RET_HEAD_DIM = 128
RET_DIM = RET_HEADS * RET_HEAD_DIM
GLA_HEADS = 4
GLA_KEY_HEAD_DIM = 64
GLA_VAL_HEAD_DIM = 128
GLA_KEY_DIM = GLA_HEADS * GLA_KEY_HEAD_DIM
GLA_VAL_DIM = GLA_HEADS * GLA_VAL_HEAD_DIM
GLA_GATE_RANK = 16
GLA_GATE_TAU = 16.0
N_BRANCHES = 3
CHUNK = 64
D_FF = (((8 * D_MODEL + 2) // 3 + 255) // 256) * 256
ROPE_BASE = 10000.0
EPS = 1e-6
IN_COLS = 3 * CONV_DIM + 4 * RET_DIM + 2 * GLA_KEY_DIM + 2 * GLA_VAL_DIM + GLA_GATE_RANK + N_BRANCHES * D_MODEL

kernel_name = "hybrid_conv_retention_gla_gated_block"


def rms_norm(x, w):
    xf = x.astype(jnp.float32)
    y = xf * lax.rsqrt(jnp.mean(xf * xf, axis=-1, keepdims=True) + EPS)
    return (y * w.astype(jnp.float32)).astype(x.dtype)


def head_norm(y, w, center):
    b, t, h, d = y.shape
    if center:
        y = y - jnp.mean(y, axis=-1, keepdims=True)
    y = y * lax.rsqrt(jnp.mean(y * y, axis=-1, keepdims=True) + EPS)
    return y.reshape(b, t, h * d) * w.astype(jnp.float32)


def rotary(x, positions):
    half = x.shape[-1] // 2
    inv_freq = ROPE_BASE ** (-jnp.arange(half, dtype=jnp.float32) / half)
    ang = positions.astype(jnp.float32)[..., None] * inv_freq
    cos = jnp.cos(ang)[:, :, None, :]
    sin = jnp.sin(ang)[:, :, None, :]
    x1, x2 = x[..., :half], x[..., half:]
    return jnp.concatenate([x1 * cos - x2 * sin, x2 * cos + x1 * sin], axis=-1)


def to_chunks(x):
    b, t, h, d = x.shape
    return x.reshape(b, t // CHUNK, CHUNK, h, d).transpose(1, 0, 3, 2, 4)


def from_chunks(x):
    nc, b, h, c, d = x.shape
    return x.transpose(1, 0, 3, 2, 4).reshape(b, nc * c, h, d)


def short_conv(u, b_gate, c_gate, conv_w):
    z = c_gate * u
    t = z.shape[1]
    zp = jnp.pad(z, ((0, 0), (CONV_WIDTH - 1, 0), (0, 0)))
    y = sum(conv_w[i] * zp[:, i:i + t, :] for i in range(CONV_WIDTH))
    return b_gate * y


def retention(q, k, v):
    b, t, h, dk = q.shape
    dv = v.shape[-1]
    log_g = jnp.log(1.0 - 2.0 ** (-5.0 - jnp.arange(h, dtype=jnp.float32)))
    idx = jnp.arange(CHUNK, dtype=jnp.float32)
    diff = idx[:, None] - idx[None, :]
    causal = diff >= 0
    decay_mask = jnp.where(causal, jnp.exp(log_g[:, None, None] * jnp.maximum(diff, 0.0)), 0.0)
    q_decay = jnp.exp(log_g[:, None] * (idx + 1.0))[None, :, :, None]
    k_decay = jnp.exp(log_g[:, None] * (CHUNK - 1.0 - idx))[None, :, :, None]
    chunk_decay = jnp.exp(log_g * CHUNK)[None, :, None, None]
    q = q * (dk ** -0.5)

    def step(state, inp):
        qi, ki, vi = inp
        s = jnp.einsum('bhid,bhjd->bhij', qi, ki) * decay_mask
        intra = jnp.einsum('bhij,bhjv->bhiv', s, vi)
        inter = jnp.einsum('bhid,bhdv->bhiv', qi * q_decay, state)
        new_state = state * chunk_decay + jnp.einsum('bhjd,bhjv->bhdv', ki * k_decay, vi)
        return new_state, intra + inter

    state0 = jnp.zeros((b, h, dk, dv), jnp.float32)
    _, out = lax.scan(step, state0, (to_chunks(q), to_chunks(k), to_chunks(v)))
    return from_chunks(out)


def gated_linear_attention(q, k, v, log_a):
    b, t, h, dk = q.shape
    dv = v.shape[-1]
    q = q * (dk ** -0.5)
    tril = jnp.tril(jnp.ones((CHUNK, CHUNK), dtype=bool))[None, None, :, :, None]

    def step(state, inp):
        qi, ki, vi, gi = inp
        cum = jnp.cumsum(gi, axis=2)
        rel = cum[:, :, :, None, :] - cum[:, :, None, :, :]
        pair_decay = jnp.where(tril, jnp.exp(jnp.where(tril, rel, 0.0)), 0.0)
        s = jnp.einsum('bhid,bhjd,bhijd->bhij', qi, ki, pair_decay)
        intra = jnp.einsum('bhij,bhjv->bhiv', s, vi)
        inter = jnp.einsum('bhid,bhdv->bhiv', qi * jnp.exp(cum), state)
        last = cum[:, :, -1:, :]
        new_state = state * jnp.exp(last)[:, :, 0, :, None] + jnp.einsum(
            'bhjd,bhjv->bhdv', ki * jnp.exp(last - cum), vi)
        return new_state, intra + inter

    state0 = jnp.zeros((b, h, dk, dv), jnp.float32)
    _, out = lax.scan(step, state0, (to_chunks(q), to_chunks(k), to_chunks(v), to_chunks(log_a)))
    return from_chunks(out)


def split_columns(proj):
    sizes = ([CONV_DIM] * 3 + [RET_DIM] * 4
             + [GLA_KEY_DIM, GLA_KEY_DIM, GLA_VAL_DIM, GLA_VAL_DIM, GLA_GATE_RANK]
             + [D_MODEL] * N_BRANCHES)
    offsets = [int(o) for o in np.cumsum(sizes)[:-1]]
    return jnp.split(proj, offsets, axis=-1)


def mixer_block(h, positions, w_in, conv_w, ret_gn_w, gla_w_a2, gla_b_a, gla_gn_w,
                w_branch_a, w_branch_b, w_branch_c, w_out):
    bsz, t, _ = h.shape
    proj = (h @ w_in).astype(jnp.float32)
    (cu, cb, cc, rq, rk, rv, rg, gq, gk, gv, gr, ga_down,
     gate_a, gate_b, gate_c) = split_columns(proj)

    y_a = short_conv(cu, cb, cc, conv_w.astype(jnp.float32))

    rq = rotary(rq.reshape(bsz, t, RET_HEADS, RET_HEAD_DIM), positions)
    rk = rotary(rk.reshape(bsz, t, RET_HEADS, RET_HEAD_DIM), positions)
    rv = rv.reshape(bsz, t, RET_HEADS, RET_HEAD_DIM)
    y_b = head_norm(retention(rq, rk, rv), ret_gn_w, center=True) * jax.nn.silu(rg)

    gate_logits = ga_down @ gla_w_a2.astype(jnp.float32) + gla_b_a.astype(jnp.float32)
    log_a = (jax.nn.log_sigmoid(gate_logits) / GLA_GATE_TAU).reshape(bsz, t, GLA_HEADS, GLA_KEY_HEAD_DIM)
    gq = gq.reshape(bsz, t, GLA_HEADS, GLA_KEY_HEAD_DIM)
    gk = gk.reshape(bsz, t, GLA_HEADS, GLA_KEY_HEAD_DIM)
    gv = gv.reshape(bsz, t, GLA_HEADS, GLA_VAL_HEAD_DIM)
    y_c = head_norm(gated_linear_attention(gq, gk, gv, log_a), gla_gn_w, center=False) * jax.nn.silu(gr)

    merged = (jax.nn.sigmoid(gate_a) * (y_a @ w_branch_a.astype(jnp.float32))
              + jax.nn.sigmoid(gate_b) * (y_b @ w_branch_b.astype(jnp.float32))
              + jax.nn.sigmoid(gate_c) * (y_c @ w_branch_c.astype(jnp.float32)))
    return (merged @ w_out.astype(jnp.float32)).astype(h.dtype)


def swiglu(h, w_gate, w_up, w_down):
    return (jax.nn.silu(h @ w_gate) * (h @ w_up)) @ w_down


def setup_inputs(seed: int = 0) -> dict:
    key = jax.random.key(seed)
    ks = jax.random.split(key, 20)
    f32 = jnp.float32

    def normal(k, shape, scale):
        return jax.random.normal(k, shape, f32) * scale

    def gain(k, shape):
        return 1.0 + 0.1 * jax.random.normal(k, shape, f32)

    return {
        "x": normal(ks[0], (BATCH, SEQ, D_MODEL), 1.0),
        "positions": jnp.broadcast_to(jnp.arange(SEQ, dtype=jnp.int32), (BATCH, SEQ)),
        "norm_mix_pre": gain(ks[1], (DEPTH, D_MODEL)),
        "w_in": normal(ks[2], (DEPTH, D_MODEL, IN_COLS), D_MODEL ** -0.5),
        "conv_w": normal(ks[3], (DEPTH, CONV_WIDTH, CONV_DIM), CONV_WIDTH ** -0.5),
        "ret_gn_w": gain(ks[4], (DEPTH, RET_DIM)),
        "gla_w_a2": normal(ks[5], (DEPTH, GLA_GATE_RANK, GLA_KEY_DIM), GLA_GATE_RANK ** -0.5),
        "gla_b_a": normal(ks[6], (DEPTH, GLA_KEY_DIM), 0.1),
        "gla_gn_w": gain(ks[7], (DEPTH, GLA_VAL_DIM)),
        "w_branch_a": normal(ks[8], (DEPTH, CONV_DIM, D_MODEL), CONV_DIM ** -0.5),
        "w_branch_b": normal(ks[9], (DEPTH, RET_DIM, D_MODEL), RET_DIM ** -0.5),
        "w_branch_c": normal(ks[10], (DEPTH, GLA_VAL_DIM, D_MODEL), GLA_VAL_DIM ** -0.5),
        "w_out": normal(ks[11], (DEPTH, D_MODEL, D_MODEL), D_MODEL ** -0.5),
        "norm_mix_post": gain(ks[12], (DEPTH, D_MODEL)),
        "norm_ffn_pre": gain(ks[13], (DEPTH, D_MODEL)),
        "w_ffn_gate": normal(ks[14], (DEPTH, D_MODEL, D_FF), D_MODEL ** -0.5),
        "w_ffn_up": normal(ks[15], (DEPTH, D_MODEL, D_FF), D_MODEL ** -0.5),
        "w_ffn_down": normal(ks[16], (DEPTH, D_FF, D_MODEL), D_FF ** -0.5),
        "norm_ffn_post": gain(ks[17], (DEPTH, D_MODEL)),
    }


def reference(x, positions, norm_mix_pre, w_in, conv_w, ret_gn_w, gla_w_a2, gla_b_a, gla_gn_w,
              w_branch_a, w_branch_b, w_branch_c, w_out, norm_mix_post, norm_ffn_pre,
              w_ffn_gate, w_ffn_up, w_ffn_down, norm_ffn_post):
    for layer in range(DEPTH):
        h = rms_norm(x, norm_mix_pre[layer])
        m = mixer_block(h, positions, w_in[layer], conv_w[layer], ret_gn_w[layer], gla_w_a2[layer],
                        gla_b_a[layer], gla_gn_w[layer], w_branch_a[layer], w_branch_b[layer],
                        w_branch_c[layer], w_out[layer])
        x = x + rms_norm(m, norm_mix_post[layer]).astype(x.dtype)
        h = rms_norm(x, norm_ffn_pre[layer])
        f = swiglu(h, w_ffn_gate[layer], w_ffn_up[layer], w_ffn_down[layer])
        x = x + rms_norm(f, norm_ffn_post[layer]).astype(x.dtype)
    return x
```

```python
import math
import os
KSTOP = int(os.environ.get('KSTOP', '99'))
from contextlib import ExitStack

import numpy as np
import concourse.bass as bass
import concourse.mybir as mybir
from concourse.bass_utils import run_bass_kernel_spmd

F32 = mybir.dt.float32
BF16 = mybir.dt.bfloat16
I32 = mybir.dt.int32
ALU = mybir.AluOpType
AF = mybir.ActivationFunctionType
AX = mybir.AxisListType

D = 1024
T = 2048
DEPTH = 2
NTC = 4
KC = 8
IN_COLS = 8208
DFF = 2816
NF = 22
EPS = 1e-6
NSLOT = 4
SLOT = 4096
LP = 52

O_CU, O_CB, O_CC = 0, 512, 1024
O_RQ, O_RK, O_RV, O_RG = 1536, 2048, 2560, 3072
O_GQ, O_GK, O_GV, O_GR, O_GA = 3584, 3840, 4096, 4608, 5120
O_GATE = 5136

C_INVF, C_SIGN, C_KDEC, C_RMASK, C_QDEC, C_CAUS, C_UNEG, C_ID = 0, 1, 2, 6, 518, 1030, 1158, 1286
NCST = 1414


class Eng:
    def __init__(self, name, e, sem):
        self.name, self.e, self.sem = name, e, sem
        self.cnt = 0
        self.seen = {}


class Reg:
    __slots__ = ("w", "rs", "dsem", "dcnt", "excl")

    def __init__(self, excl=False):
        self.excl = excl
        self.w = None
        self.rs = {}
        self.dsem = None
        self.dcnt = 0


class FW:
    def __init__(self, nc, stack):
        self.nc = nc
        self.stack = stack
        self.E = {}
        for name, e in (("pe", nc.tensor), ("act", nc.scalar), ("dve", nc.vector),
                        ("pool", nc.gpsimd), ("sp", nc.sync)):
            sem = stack.enter_context(nc.semaphore("s_" + name))
            self.E[name] = Eng(name, e, sem)
        self.nsem = 5
        self.limit = None

    def sbuf(self, name, shape, dt):
        return self.stack.enter_context(self.nc.sbuf_tensor("sb_" + name, list(shape), dt))

    def psum(self, name, shape, dt):
        return self.stack.enter_context(self.nc.psum_tensor(name, list(shape), dt))

    def newsem(self):
        self.nsem += 1
        return self.stack.enter_context(self.nc.semaphore("d%d" % self.nsem))

    def _need(self, E, tok, need, same_ok):
        if tok is None:
            return
        if tok[0] == "e":
            F, n = tok[1], tok[2]
            if F is E and E.name in ("pe", "sp"):
                return
            key = F.name
            sem = F.sem
        else:
            _, sem, n, sid = tok
            key = ("d", sid)
        if E.seen.get(key, 0) >= n:
            return
        if need.get(key, (None, 0))[1] < n:
            need[key] = (sem, n)

    def _waits(self, E, reads, writes):
        need = {}
        for r in reads:
            self._need(E, r.w, need, False)
            if r.excl:
                for t in r.rs.values():
                    if t[0] == "e" and t[1] is E:
                        continue
                    self._need(E, t, need, True)
        for w in writes:
            self._need(E, w.w, need, False)
            for t in w.rs.values():
                self._need(E, t, need, True)
        for key, (sem, n) in need.items():
            E.e.wait_ge(sem, n)
            E.seen[key] = n

    def op(self, eng, fn, reads=(), writes=()):
        if self.limit is not None:
            if self.limit <= 0:
                return None
            self.limit -= 1
        E = self.E[eng]
        self._waits(E, reads, writes)
        inst = fn(E.e)
        E.cnt += 1
        inst.then_inc(E.sem, 1)
        tok = ("e", E, E.cnt)
        for r in reads:
            r.rs[E.name] = tok
        for w in writes:
            w.w = tok
            w.rs = {}
        return inst

    def dma(self, q, segs, reads=(), writes=(), owner=None):
        E = self.E[q]
        self._waits(E, reads, writes)
        owner = owner if owner is not None else (writes[0] if writes else reads[0])
        if owner.dsem is None:
            owner.dsem = self.newsem()
        for (o, i) in segs:
            inst = E.e.dma_start(out=o, in_=i)
            owner.dcnt += 16
            inst.then_inc(owner.dsem, 16)
        tok = ("d", owner.dsem, owner.dcnt, id(owner))
        for r in reads:
            r.rs[("d", id(owner))] = tok
        for w in writes:
            w.w = tok
            w.rs = {}

    def wait_all(self, eng, regs):
        self._waits(self.E[eng], [], regs)


def build_program(debug=False, depth=DEPTH, ntc=NTC, phases="ABCDEFG"):
    nc = bass.Bass("TRN2", target_bir_lowering=False)

    def din(name, shape, dt=F32):
        return nc.dram_tensor(name, list(shape), dt, kind="ExternalInput").ap()

    dd = max(depth, 1)
    x_d = din("x", [T, D])
    pos_d = din("pos", [1, T], I32)
    w_in_d = din("w_in", [dd, D, IN_COLS])
    wa2_d = din("gla_w_a2", [dd, 16, 256])
    bA_d = din("bA", [dd, 128, 256])
    wbr_d = [din("w_branch_" + n, [dd, 512, D]) for n in "abc"]
    wout_d = din("w_out", [dd, D, D])
    wg_d = din("w_ffn_gate", [dd, D, DFF])
    wu_d = din("w_ffn_up", [dd, D, DFF])
    wd_d = din("w_ffn_down", [dd, DFF, D])
    pv_d = din("pv", [128, dd * LP])
    cst_d = din("cst", [128, NCST])
    out_d = nc.dram_tensor("out", [T, D], F32, kind="ExternalOutput").ap()
    dbg_d = {}
    if debug:
        for n in ("ya", "yb", "yc"):
            dbg_d[n] = nc.dram_tensor("dbg_" + n, [4, 128, T], BF16, kind="ExternalOutput").ap()
        for n in ("xmix", "xffn"):
            dbg_d[n] = nc.dram_tensor("dbg_" + n, [8, 128, T], F32, kind="ExternalOutput").ap()

    st = ExitStack()
    with st:
        fw = FW(nc, st)
        op = fw.op

        x = fw.sbuf("x", [128, KC, T], F32)
        RX = [[Reg() for _ in range(NTC)] for _ in range(KC)]
        hT = fw.sbuf("hT", [128, KC, 512], BF16)
        RH = [Reg() for _ in range(KC)]
        ybuf = [fw.sbuf("y%d" % i, [128, 4, 512], BF16) for i in range(3)]
        RY = [[Reg() for _ in range(4)] for _ in range(3)]
        ACC = fw.sbuf("acc", [128, KC, 512], F32)
        RA = [Reg() for _ in range(KC)]
        mrg = fw.sbuf("mrg", [128, KC, 512], BF16)
        RM = [Reg() for _ in range(KC)]
        actb = fw.sbuf("actb", [128, 2, 2, 512], BF16)
        RACT = [[Reg() for _ in range(2)] for _ in range(2)]
        slots = [fw.sbuf("slot%d" % i, [128, SLOT], BF16) for i in range(NSLOT)]
        RS = [Reg() for _ in range(NSLOT)]
        P = [fw.psum("ps%d" % i, [128, 512], F32) for i in range(8)]
        RP = [Reg(excl=True) for _ in range(8)]

        def wt(name, shape=(128, 512), dt=F32):
            return fw.sbuf(name, shape, dt), Reg()

        T0, R0 = wt("T0"); T1, R1 = wt("T1"); T2, R2 = wt("T2"); T3, R3 = wt("T3")
        T4, R4 = wt("T4"); T5, R5 = wt("T5")
        sqb = [wt("sqb%d" % i, dt=BF16) for i in range(2)]
        qh, Rqh = wt("qh", dt=BF16); qt, Rqt = wt("qt", dt=BF16); kh, Rkh = wt("kh", dt=BF16)
        vtok, Rvt = wt("vtok", (128, 4, 256), BF16)
        ktok, Rkt = wt("ktok", (128, 4, 128), BF16)
        stm, Rstm = wt("stm", (128, 128), BF16)
        zb, Rzb = wt("zb", (128, 514), F32)
        zc, Rzc = wt("zc", (128, 4, 2), F32)
        spb, Rspb = wt("spb", (128, 4, 256), F32)
        gdb, Rgdb = wt("gdb", (128, 512), F32)
        gq = [wt("gq%d" % i, dt=BF16) for i in range(2)]
        nm, Rnm = wt("nm", (128, 4), F32)
        tS, RtS = wt("tS", (128, 128), F32)
        Sret, RSr = wt("Sret", (128, 4, 128), F32)
        Sretb, RSrb = wt("Sretb", (128, 4, 128), BF16)
        RSr = [Reg() for _ in range(4)]; RSrb = [Reg() for _ in range(4)]
        Sgla, RSg = wt("Sgla", (128, 2, 128), F32)
        Sglab, RSgb = wt("Sglab", (128, 2, 128), BF16)
        RSg = [Reg() for _ in range(2)]; RSgb = [Reg() for _ in range(2)]
        cosT, Rcos = wt("cosT"); sinT, Rsin = wt("sinT")
        posi, Rposi = wt("posi", (128, 512), I32)
        kint, Rkint = wt("kint", (128, 512), I32)
        cst, Rcst = wt("cst", (128, NCST), F32)
        pv, Rpv = wt("pv", (128, dd * LP), F32)
        bA, RbA = wt("bA", (128, dd, 256), F32)
        wa2, Rwa2 = wt("wa2", (128, dd, 256), F32)
        idb, Ridb = wt("idb", (128, 128), BF16)
        onesD, RoD = wt("onesD", (128, 128), BF16)
        onesH, RoH = wt("onesH", (128, 128), BF16)
        xin = [wt("xin%d" % i, (128, D), F32) for i in range(2)]

        DBG = Reg()

        def tt(eng, out, in0, in1, o, reads, writes):
            op(eng, lambda e: e.tensor_tensor(out=out, in0=in0, in1=in1, op=o), reads, writes)

        def ts(eng, out, in0, s1, s2, o0, o1, reads, writes):
            if o1 is None:
                op(eng, lambda e: e.tensor_scalar(out=out, in0=in0, scalar1=s1, scalar2=None, op0=o0), reads, writes)
            else:
                op(eng, lambda e: e.tensor_scalar(out=out, in0=in0, scalar1=s1, scalar2=s2, op0=o0, op1=o1), reads, writes)

        def stt(out, in0, sc, in1, o0, o1, reads, writes):
            op("dve", lambda e: e.scalar_tensor_tensor(out=out, in0=in0, scalar=sc, in1=in1, op0=o0, op1=o1), reads, writes)

        def act(out, in_, func, reads, writes, bias=None, scale=None):
            kw = {}
            if bias is not None:
                kw["bias"] = bias
            if scale is not None:
                kw["scale"] = scale
            op("act", lambda e: e.activation(out=out, in_=in_, func=func, **kw), reads, writes)

        def acopy(out, in_, reads, writes):
            op("act", lambda e: e.copy(out=out, in_=in_), reads, writes)

        def mm(out, pairs, reads, writes):
            def f(e):
                inst = None
                n = len(pairs)
                for i, (l, r) in enumerate(pairs):
                    inst = e.matmul(out, lhsT=l, rhs=r, start=(i == 0), stop=(i == n - 1))
                return inst
            op("pe", f, reads, writes)

        def mm1(out, l, r, start, stop, reads, writes):
            op("pe", lambda e: e.matmul(out, lhsT=l, rhs=r, start=start, stop=stop), reads, writes)

        def tr(out, in_, ident, reads, writes):
            op("pe", lambda e: e.transpose(out=out, in_=in_, identity=ident), reads, writes)

        def pcol(l, off):
            return pv[:, l * LP + off: l * LP + off + 1]

        def tsl(tc):
            return slice(tc * 512, (tc + 1) * 512)

        units = []

        def unit(specs, fn):
            units.append((specs, fn))

        def wseg(dram_ap_rows_by_cols, kc):
            return dram_ap_rows_by_cols.rearrange("(k p) n -> p k n", p=128)

        def setup(_):
            fw.dma("sp", [(cst[:], cst_d)], writes=[Rcst])
            fw.dma("sp", [(pv[:], pv_d)], writes=[Rpv])
            fw.dma("sp", [(bA[:], bA_d.rearrange("l p n -> p l n"))], writes=[RbA])
            op("dve", lambda e: e.memset(wa2[:], 0.0), [], [Rwa2])
            fw.dma("sp", [(wa2[0:16], wa2_d.rearrange("l r n -> r l n"))], writes=[Rwa2])
            for (g_, rg_) in gq:
                op("dve", lambda e: e.memset(g_[:], 0.0), [], [rg_])
            op("dve", lambda e: e.tensor_copy(out=idb[:], in_=cst[:, C_ID:C_ID + 128]), [Rcst], [Ridb])
            op("dve", lambda e: e.memset(onesD[:], 1.0 / D), [], [RoD])
            op("dve", lambda e: e.memset(onesH[:], 1.0 / 128), [], [RoH])

        unit([], setup)

        def load_x(tb):
            def f(_):
                xt_, rx_ = xin[tb % 2]
                fw.dma("sp", [(xt_[:], x_d[tb * 128:(tb + 1) * 128, :])], writes=[rx_])
                tc = tb // 4
                for half in range(2):
                    pb = (tb * 2 + half) % 4
                    for j in range(4):
                        k = half * 4 + j
                        tr(P[pb][:, j * 128:(j + 1) * 128], xt_[:, k * 128:(k + 1) * 128], cst[:, C_ID:C_ID + 128],
                           [rx_, Rcst], [RP[pb]])
                    dst = x[:, half * 4:half * 4 + 4, tb * 128:(tb + 1) * 128]
                    src = P[pb][:].rearrange("p (a b) -> p a b", a=4)
                    if half == 0:
                        op("act", lambda e: e.copy(out=dst, in_=src), [RP[pb]], [RX[k_][tc] for k_ in range(half * 4, half * 4 + 4)])
                    else:
                        op("dve", lambda e: e.tensor_copy(out=dst, in_=src), [RP[pb]], [RX[k_][tc] for k_ in range(half * 4, half * 4 + 4)])
            return f

        for tb in range(16):
            unit([], load_x(tb))

        def rstd_from(pb):
            act(T0[:], P[pb][:], AF.Ln, [RP[pb]], [R0], bias=EPS, scale=1.0)
            act(T1[:], T0[:], AF.Exp, [R0], [R1], scale=-0.5)

        def prenorm(l, tc, goff):
            def f(_):
                for k in range(KC):
                    sq, rsq = sqb[k % 2]
                    act(sq[:], x[:, k, tsl(tc)], AF.Square, [RX[k][tc]], [rsq])
                    mm1(P[7][:], onesD[:], sq[:], k == 0, k == KC - 1, [rsq, RoD], [RP[7]])
                rstd_from(7)
                for k in range(KC):
                    stt(hT[:, k, :], x[:, k, tsl(tc)], pcol(l, goff + k), T1[:], ALU.mult, ALU.mult,
                        [RX[k][tc], R1, Rpv], [RH[k]])
            return f

        def postnorm(l, tc, goff, dbgname=None):
            def f(_):
                for k in range(KC):
                    sq, rsq = sqb[k % 2]
                    act(sq[:], ACC[:, k, :], AF.Square, [RA[k]], [rsq])
                    mm1(P[7][:], onesD[:], sq[:], k == 0, k == KC - 1, [rsq, RoD], [RP[7]])
                rstd_from(7)
                for k in range(KC):
                    stt(T3[:], ACC[:, k, :], pcol(l, goff + k), T1[:], ALU.mult, ALU.mult, [RA[k], R1, Rpv], [R3])
                    tt("dve", x[:, k, tsl(tc)], x[:, k, tsl(tc)], T3[:], ALU.add, [R3, RX[k][tc]], [RX[k][tc]])
                    if debug and dbgname is not None and l == 0:
                        fw.dma("sp", [(dbg_d[dbgname][k, :, tsl(tc)], x[:, k, tsl(tc)])], reads=[RX[k][tc]])
            return f

        def proj(pb, wv, c0, reads_w, n=128):
            mm(P[pb][0:n, :], [(wv[:, k, c0:c0 + n], hT[:, k, :]) for k in range(KC)], [reads_w] + RH, [RP[pb]])

        def proj_tok(pb, ncol, wv, c0, reads_w, blk, o0):
            mm(P[pb][:, o0:o0 + ncol], [(hT[:, k, blk * 128:(blk + 1) * 128], wv[:, k, c0:c0 + ncol]) for k in range(KC)],
               [reads_w] + RH, [RP[pb]])

        def rope_tables(b_tc):
            tc = b_tc

            def f(_):
                fw.dma("sp", [(posi[:], pos_d[0:1, tsl(tc)].partition_broadcast(128))], writes=[Rposi])
                op("dve", lambda e: e.tensor_copy(out=T0[:], in_=posi[:]), [Rposi], [R0])
                ts("dve", T2[:], T0[:], cst[:, C_INVF:C_INVF + 1], None, ALU.mult, None, [R0, Rcst], [R2])
                ts("dve", kint[:], T2[:], 1.0 / (2 * math.pi), None, ALU.mult, None, [R2], [Rkint])
                op("dve", lambda e: e.tensor_copy(out=T0[:], in_=kint[:]), [Rkint], [R0])
                C1 = 6.28125
                C2 = 2 * math.pi - C1
                stt(T2[:], T0[:], -C1, T2[:], ALU.mult, ALU.add, [R0, R2], [R2])
                stt(T2[:], T0[:], -C2, T2[:], ALU.mult, ALU.add, [R0, R2], [R2])
                ts("dve", T2[:], T2[:], math.pi, -math.pi, ALU.min, ALU.max, [R2], [R2])
                act(T3[:], T2[:], AF.Sin, [R2], [R3])
                ts("dve", sinT[:], T3[:], cst[:, C_SIGN:C_SIGN + 1], None, ALU.mult, None, [R3, Rcst], [Rsin])
                act(T3[:], T2[:], AF.Sin, [R2], [R3], scale=0.5)
                tt("dve", T3[:], T3[:], T3[:], ALU.mult, [R3], [R3])
                ts("dve", cosT[:], T3[:], -2.0, 1.0, ALU.mult, ALU.add, [R3], [Rcos])
            return f

        def reset_states(_):
            op("dve", lambda e: e.memset(zc[:], 0.0), [], [Rzc])
            op("dve", lambda e: e.memset(Sret[:], 0.0), [], RSr)
            op("dve", lambda e: e.memset(Sretb[:], 0.0), [], RSrb)
            op("dve", lambda e: e.memset(Sgla[:], 0.0), [], RSg)
            op("dve", lambda e: e.memset(Sglab[:], 0.0), [], RSgb)

        def conv_unit(l, tc, c):
            win = w_in_d[l]
            specs = [[(0, 8, 128, wseg(win[:, O_CU + c * 128:O_CU + (c + 1) * 128], 8)),
                      (1024, 8, 128, wseg(win[:, O_CB + c * 128:O_CB + (c + 1) * 128], 8)),
                      (2048, 8, 128, wseg(win[:, O_CC + c * 128:O_CC + (c + 1) * 128], 8))]]

            def f(pc):
                (sl, rs) = pc[0]
                wv = sl[:, 0:3072].rearrange("p (s k n) -> p s k n", s=3, k=8)
                proj(0, wv[:, 0], 0, rs)
                proj(1, wv[:, 1], 0, rs)
                proj(2, wv[:, 2], 0, rs)
                acopy(T0[:], P[0][:], [RP[0]], [R0])
                acopy(zb[:, 0:2], zc[:, c, :], [Rzc], [Rzb])
                tt("dve", zb[:, 2:514], P[2][:], T0[:], ALU.mult, [RP[2], R0], [Rzb])
                acopy(zc[:, c, :], zb[:, 512:514], [Rzb], [Rzc])
                ts("dve", T2[:], zb[:, 2:514], pcol(l, 32 + 8 + c), None, ALU.mult, None, [Rzb, Rpv], [R2])
                stt(T2[:], zb[:, 1:513], pcol(l, 32 + 4 + c), T2[:], ALU.mult, ALU.add, [Rzb, Rpv, R2], [R2])
                stt(T2[:], zb[:, 0:512], pcol(l, 32 + 0 + c), T2[:], ALU.mult, ALU.add, [Rzb, Rpv, R2], [R2])
                tt("dve", ybuf[0][:, c, :], P[1][:], T2[:], ALU.mult, [RP[1], R2], [RY[0][c]])
                if debug and l == 0:
                    fw.dma("sp", [(dbg_d["ya"][c, :, tsl(tc)], ybuf[0][:, c, :])], reads=[RY[0][c]])
            unit(specs, f)

        def rope(pb):
            tt("dve", T0[:], P[pb][:], cosT[:], ALU.mult, [RP[pb], Rcos], [R0])
            acopy(T5[:], P[pb][:], [RP[pb]], [R5])
            acopy(T2[0:64, :], T5[64:128, :], [R5], [R2])
            acopy(T2[64:128, :], T5[0:64, :], [R5], [R2])
            tt("dve", T2[:], T2[:], sinT[:], ALU.mult, [R2, Rsin], [R2])
            tt("dve", T3[:], T0[:], T2[:], ALU.add, [R0, R2], [R3])

        def headnorm(pbo, pbm, sg_t, sg_r, gcol, out_ap, out_reg):
            sq, rsq = sqb[0]
            act(sq[:], P[pbo][:], AF.Square, [RP[pbo]], [rsq])
            mm1(P[pbm][:], onesH[:], sq[:], True, True, [rsq, RoH], [RP[pbm]])
            rstd_from(pbm)
            tt("dve", T3[:], P[pbo][:], T1[:], ALU.mult, [RP[pbo], R1], [R3])
            stt(out_ap, T3[:], gcol, sg_t[:], ALU.mult, ALU.mult, [R3, Rpv, sg_r], [out_reg])

        GAM = [1.0 - 2.0 ** (-5.0 - h) for h in range(4)]

        def ret_unit(l, tc, h):
            win = w_in_d[l]
            specs = [[(i * 1024, 8, 128, wseg(win[:, o + h * 128:o + (h + 1) * 128], 8))
                      for i, o in enumerate((O_RQ, O_RK, O_RV, O_RG))]]

            def f(pc):
                (sl, rs) = pc[0]
                wv = sl[:, 0:4096].rearrange("p (s k n) -> p s k n", s=4, k=8)
                proj(0, wv[:, 0], 0, rs)
                proj(1, wv[:, 1], 0, rs)
                proj(2, wv[:, 3], 0, rs)
                for blk in range(4):
                    proj_tok(3, 128, wv[:, 2], 0, rs, blk, blk * 128)
                if KSTOP <= 1: return
                if 'KOPS' in os.environ: fw.limit = int(os.environ['KOPS'])
                rope(0)
                op("dve", lambda e: e.tensor_copy(out=qh[:], in_=T3[:]), [R3], [Rqh])
                tt("dve", qt[:].rearrange("p (a b) -> p a b", a=4), T3[:].rearrange("p (a b) -> p a b", a=4),
                   cst[:, C_QDEC + h * 128:C_QDEC + (h + 1) * 128].unsqueeze(1).to_broadcast([128, 4, 128]),
                   ALU.mult, [R3, Rcst], [Rqt])
                if KSTOP <= 2:
                    fw.limit = None
                    return
                rope(1)
                op("dve", lambda e: e.tensor_copy(out=kh[:], in_=T3[:]), [R3], [Rkh])
                if KSTOP <= 3: return
                act(T4[:], P[2][:], AF.Silu, [RP[2]], [R4])
                if KSTOP <= 4: return
                pv3 = P[3][:].rearrange("p (a b) -> p a b", a=4)
                op("dve", lambda e: e.tensor_reduce(out=nm[:], in_=pv3, axis=AX.X, op=ALU.add), [RP[3]], [Rnm])
                ts("dve", nm[:], nm[:], -1.0 / 128, None, ALU.mult, None, [Rnm], [Rnm])
                for blk in range(4):
                    ts("dve", vtok[:, blk, 0:128], P[3][:, blk * 128:(blk + 1) * 128], nm[:, blk:blk + 1], None,
                       ALU.add, None, [RP[3], Rnm], [Rvt])
                if KSTOP <= 5: return
                p4b = P[4][:].bitcast(BF16)
                for blk in range(4):
                    tr(p4b[:, blk * 128:(blk + 1) * 128], kh[:, blk * 128:(blk + 1) * 128], idb[:], [Rkh, Ridb], [RP[4]])
                ts("dve", ktok[:].rearrange("p a b -> p (a b)"), p4b[:, 0:512], cst[:, C_KDEC + h:C_KDEC + h + 1], None,
                   ALU.mult, None, [RP[4], Rcst], [Rkt])
                if KSTOP <= 6: return
                g128 = GAM[h] ** 128
                for blk in range(4):
                    bs = slice(blk * 128, (blk + 1) * 128)
                    mm(P[5][:, 0:128], [(kh[:, bs], qh[:, bs])], [Rkh, Rqh], [RP[5]])
                    tt("dve", stm[:], P[5][:, 0:128], cst[:, C_RMASK + h * 128:C_RMASK + (h + 1) * 128], ALU.mult,
                       [RP[5], Rcst], [Rstm])
                    mm(P[6][:, bs], [(vtok[:, blk, 0:128], stm[:]), (Sretb[:, h, :], qt[:, bs])],
                       [Rvt, Rstm, RSrb[h], Rqt], [RP[6]])
                    mm(P[7][:, 0:128], [(ktok[:, blk, :], vtok[:, blk, 0:128])], [Rkt, Rvt], [RP[7]])
                    stt(Sret[:, h, :], Sret[:, h, :], g128, P[7][:, 0:128], ALU.mult, ALU.add, [RSr[h], RP[7]], [RSr[h]])
                    acopy(Sretb[:, h, :], Sret[:, h, :], [RSr[h]], [RSrb[h]])
                if KSTOP <= 7: return
                headnorm(6, 7, T4, R4, pcol(l, 44 + h), ybuf[1][:, h, :], RY[1][h])
                if debug and l == 0:
                    fw.dma("sp", [(dbg_d["yb"][h, :, tsl(tc)], ybuf[1][:, h, :])], reads=[RY[1][h]])
            unit(specs, f)

        def gla_prelude(l, tc):
            win = w_in_d[l]
            specs = [[(0, 8, 128, wseg(win[:, O_GA:O_GA + 128], 8))]]

            def f(pc):
                (sl, rs) = pc[0]
                wv = sl[:, 0:1024].rearrange("p (k n) -> p k n", k=8)
                mm(P[0][:, :], [(wv[:, k, :], hT[:, k, :]) for k in range(KC)], [rs] + RH, [RP[0]])
                acopy(gdb[:], P[0][:, :], [RP[0]], [Rgdb])
                for half in range(2):
                    pb = 1 + half
                    for j in range(2):
                        blk = half * 2 + j
                        mm(P[pb][:, j * 256:(j + 1) * 256], [(gdb[:, blk * 128:(blk + 1) * 128], wa2[:, l, :])],
                           [Rgdb, Rwa2], [RP[pb]])
                    sv = spb[:, half * 2:half * 2 + 2, :]
                    tt("dve", sv, P[pb][:].rearrange("p (a b) -> p a b", a=2),
                       bA[:, l, :].unsqueeze(1).to_broadcast([128, 2, 256]), ALU.add, [RP[pb], RbA], [Rspb])
                    act(sv, sv, AF.Exp, [Rspb], [Rspb], scale=-1.0)
                    act(sv, sv, AF.Ln, [Rspb], [Rspb], bias=1.0, scale=1.0)
                for hp in range(2):
                    for blk in range(4):
                        mm(P[3 + hp][:, blk * 128:(blk + 1) * 128],
                           [(spb[:, blk, hp * 128:(hp + 1) * 128], cst[:, C_UNEG:C_UNEG + 128])], [Rspb, Rcst], [RP[3 + hp]])
            unit(specs, f)

        def gla_unit(l, tc, hp):
            win = w_in_d[l]
            specs = [[(0, 8, 128, wseg(win[:, O_GQ + hp * 128:O_GQ + (hp + 1) * 128], 8)),
                      (1024, 8, 128, wseg(win[:, O_GK + hp * 128:O_GK + (hp + 1) * 128], 8)),
                      (2048, 8, 256, wseg(win[:, O_GV + hp * 256:O_GV + (hp + 1) * 256], 8))],
                     [(0, 8, 256, wseg(win[:, O_GR + hp * 256:O_GR + (hp + 1) * 256], 8))]]

            def f(pc):
                (sl, rs), (sl2, rs2) = pc
                wq = sl[:, 0:1024].rearrange("p (k n) -> p k n", k=8)
                wk = sl[:, 1024:2048].rearrange("p (k n) -> p k n", k=8)
                wvv = sl[:, 2048:4096].rearrange("p (k n) -> p k n", k=8)
                wr = sl2[:, 0:2048].rearrange("p (k n) -> p k n", k=8)
                pc_ = 3 + hp
                act(T0[:], P[pc_][:], AF.Exp, [RP[pc_]], [R0])
                act(T2[:], P[pc_][:], AF.Exp, [RP[pc_]], [R2], scale=-1.0)
                proj(0, wq, 0, rs)
                proj(1, wk, 0, rs)
                for e_ in range(2):
                    ps = slice(64 * e_, 64 * e_ + 64)
                    stt(gq[e_][0][ps, :], P[0][ps, :], 0.125, T0[ps, :], ALU.mult, ALU.mult, [RP[0], R0], [gq[e_][1]])
                tt("dve", kh[:], P[1][:], T2[:], ALU.mult, [RP[1], R2], [Rkh])
                for half, pb in ((0, 2), (1, 5)):
                    for j in range(2):
                        proj_tok(pb, 256, wvv, 0, rs, half * 2 + j, j * 256)
                    acopy(vtok[:, half * 2:half * 2 + 2, :], P[pb][:].rearrange("p (a b) -> p a b", a=2), [RP[pb]], [Rvt])
                p6b = P[6][:].bitcast(BF16)
                for blk in range(4):
                    tr(p6b[:, blk * 128:(blk + 1) * 128], kh[:, blk * 128:(blk + 1) * 128], idb[:], [Rkh, Ridb], [RP[6]])
                acopy(ktok[:].rearrange("p a b -> p (a b)"), p6b[:, 0:512], [RP[6]], [Rkt])
                proj(7, wr, 0, rs2)
                act(T4[:], P[7][:], AF.Silu, [RP[7]], [R4])
                proj(5, wr, 128, rs2)
                act(T5[:], P[5][:], AF.Silu, [RP[5]], [R5])
                for blk in range(4):
                    bs = slice(blk * 128, (blk + 1) * 128)
                    for e_ in range(2):
                        gq_, rgq_ = gq[e_]
                        mm(P[0][:, 0:128], [(kh[:, bs], gq_[:, bs])], [Rkh, rgq_], [RP[0]])
                        tt("dve", stm[:], P[0][:, 0:128], cst[:, C_CAUS:C_CAUS + 128], ALU.mult, [RP[0], Rcst], [Rstm])
                        mm(P[1 + e_][:, bs], [(vtok[:, blk, e_ * 128:(e_ + 1) * 128], stm[:]), (Sglab[:, hp, :], gq_[:, bs])],
                           [Rvt, Rstm, RSgb[hp], rgq_], [RP[1 + e_]])
                    for e_ in range(2):
                        mm(P[6][:, e_ * 128:(e_ + 1) * 128], [(ktok[:, blk, :], vtok[:, blk, e_ * 128:(e_ + 1) * 128])],
                           [Rkt, Rvt], [RP[6]])
                    for e_ in range(2):
                        ps = slice(64 * e_, 64 * e_ + 64)
                        tt("dve", tS[ps, :], P[6][ps, e_ * 128:(e_ + 1) * 128], Sgla[ps, hp, :], ALU.add, [RP[6], RSg[hp]], [RtS])
                    ts("dve", Sgla[:, hp, :], tS[:], T0[:, blk * 128 + 127:blk * 128 + 128], None, ALU.mult, None,
                       [RtS, R0], [RSg[hp]])
                    acopy(Sglab[:, hp, :], Sgla[:, hp, :], [RSg[hp]], [RSgb[hp]])
                for e_, (sg_t, sg_r) in enumerate(((T4, R4), (T5, R5))):
                    hh = 2 * hp + e_
                    headnorm(1 + e_, 7, sg_t, sg_r, pcol(l, 48 + hh), ybuf[2][:, hh, :], RY[2][hh])
                    if debug and l == 0:
                        fw.dma("sp", [(dbg_d["yc"][hh, :, tsl(tc)], ybuf[2][:, hh, :])], reads=[RY[2][hh]])
            unit(specs, f)

        def merge_unit(l, tc, i):
            win = w_in_d[l]
            c0 = O_GATE + i * 1024
            specs = [[(0, 8, 512, wseg(win[:, c0:c0 + 512], 8))],
                     [(0, 8, 512, wseg(win[:, c0 + 512:c0 + 1024], 8))],
                     [(0, 4, 1024, wseg(wbr_d[i][l], 4))]]

            def f(pc):
                (slb, rsb) = pc[2]
                wb_ = slb[:, 0:4096].rearrange("p (k n) -> p k n", k=4)
                for half in range(2):
                    (sl, rs) = pc[half]
                    wg_ = sl[:, 0:4096].rearrange("p (k n) -> p k n", k=8)
                    for j in range(4):
                        oc = half * 4 + j
                        pa, pb = (0, 1) if j % 2 == 0 else (2, 3)
                        proj(pa, wg_, j * 128, rs)
                        mm(P[pb][:], [(wb_[:, kk, oc * 128:(oc + 1) * 128], ybuf[i][:, kk, :]) for kk in range(4)],
                           [rsb] + RY[i], [RP[pb]])
                        tw, rw = (T0, R0) if j % 2 == 0 else (T2, R2)
                        act(tw[:], P[pa][:], AF.Tanh, [RP[pa]], [rw], scale=0.5)
                        if i == 0:
                            stt(ACC[:, oc, :], tw[:], 1.0, P[pb][:], ALU.add, ALU.mult, [rw, RP[pb]], [RA[oc]])
                        else:
                            stt(tw[:], tw[:], 1.0, P[pb][:], ALU.add, ALU.mult, [rw, RP[pb]], [rw])
                            tt("dve", ACC[:, oc, :], ACC[:, oc, :], tw[:], ALU.add, [RA[oc], rw], [RA[oc]])
                            if i == 2:
                                op("act", lambda e: e.mul(out=mrg[:, oc, :], in_=ACC[:, oc, :], mul=0.5), [RA[oc]], [RM[oc]])
            unit(specs, f)

        def out_unit(l, tc, half):
            specs = [[(0, 8, 512, wseg(wout_d[l][:, half * 512:(half + 1) * 512], 8))]]

            def f(pc):
                (sl, rs) = pc[0]
                wo = sl[:, 0:4096].rearrange("p (k n) -> p k n", k=8)
                for j in range(4):
                    oc = half * 4 + j
                    pa = j % 4
                    mm(P[pa][:], [(wo[:, k, j * 128:(j + 1) * 128], mrg[:, k, :]) for k in range(KC)], [rs] + RM, [RP[pa]])
                    acopy(ACC[:, oc, :], P[pa][:], [RP[pa]], [RA[oc]])
            unit(specs, f)

        def ffn_unit(l, tc, g):
            c0 = g * 256
            specs = [[(0, 8, 256, wseg(wg_d[l][:, c0:c0 + 256], 8)),
                      (2048, 8, 256, wseg(wu_d[l][:, c0:c0 + 256], 8))],
                     [(0, 2, 1024, wseg(wd_d[l][c0:c0 + 256, :], 2))]]

            def f(pc):
                (sl, rs), (sl2, rs2) = pc
                wg_ = sl[:, 0:2048].rearrange("p (k n) -> p k n", k=8)
                wu_ = sl[:, 2048:4096].rearrange("p (k n) -> p k n", k=8)
                wd_ = sl2[:, 0:2048].rearrange("p (k n) -> p k n", k=2)
                par = g % 2
                for j in range(2):
                    pa, pb = (0, 1) if j == 0 else (2, 3)
                    proj(pa, wg_, j * 128, rs)
                    proj(pb, wu_, j * 128, rs)
                    tw, rw = (T0, R0) if j == 0 else (T2, R2)
                    act(tw[:], P[pa][:], AF.Silu, [RP[pa]], [rw])
                    tt("dve", actb[:, par, j, :], tw[:], P[pb][:], ALU.mult, [rw, RP[pb]], [RACT[par][j]])
                for oc in range(KC):
                    pa = 4 + oc % 3
                    mm(P[pa][:], [(wd_[:, j, oc * 128:(oc + 1) * 128], actb[:, par, j, :]) for j in range(2)],
                       [rs2] + RACT[par], [RP[pa]])
                    if g == 0:
                        acopy(ACC[:, oc, :], P[pa][:], [RP[pa]], [RA[oc]])
                    else:
                        tt("dve", ACC[:, oc, :], ACC[:, oc, :], P[pa][:], ALU.add, [RA[oc], RP[pa]], [RA[oc]])
            unit(specs, f)

        ROUT = [Reg(), Reg()]

        def store_x(tb):
            def f(_):
                tc = tb // 4
                ot, ro = xin[tb % 2]
                for half in range(2):
                    pb = (tb * 2 + half) % 4
                    for j in range(4):
                        k = half * 4 + j
                        tr(P[pb][:, j * 128:(j + 1) * 128], x[:, k, tb * 128:(tb + 1) * 128], cst[:, C_ID:C_ID + 128],
                           [RX[k][tc], Rcst], [RP[pb]])
                    if half == 0:
                        acopy(ot[:, 0:512], P[pb][:], [RP[pb]], [ro])
                    else:
                        op("dve", lambda e: e.tensor_copy(out=ot[:, 512:1024], in_=P[pb][:]), [RP[pb]], [ro])
                fw.dma("sp", [(out_d[tb * 128:(tb + 1) * 128, :], ot[:])], reads=[ro], owner=ROUT[tb % 2])
            return f

        for l in range(depth):
            unit([], reset_states)
            for tc in range(ntc):
                unit([], prenorm(l, tc, 0))
                if "A" in phases:
                    for c in range(4):
                        conv_unit(l, tc, c)
                if "B" in phases:
                    if os.environ.get('KROPE', '1') == '1':
                        unit([], rope_tables(tc))
                    for h in range(4):
                        ret_unit(l, tc, h)
                if "C" in phases:
                    gla_prelude(l, tc)
                    for hp in range(2):
                        gla_unit(l, tc, hp)
                if "D" in phases:
                    for i in range(3):
                        merge_unit(l, tc, i)
                if "E" in phases:
                    for half in range(2):
                        out_unit(l, tc, half)
                    unit([], postnorm(l, tc, 8, "xmix"))
                if "F" in phases:
                    unit([], prenorm(l, tc, 16))
                    for g in range(NF // 2):
                        ffn_unit(l, tc, g)
                    unit([], postnorm(l, tc, 24, "xffn"))
        for tb in range(16):
            unit([], store_x(tb))

        pieces = []
        upieces = []
        for ui, (specs, fn) in enumerate(units):
            idxs = []
            for segs in specs:
                idxs.append(len(pieces))
                pieces.append((ui, segs))
            upieces.append(idxs)
        nextp = 0

        def issue_loads(cur_unit):
            nonlocal nextp
            while nextp < len(pieces):
                if nextp >= NSLOT and pieces[nextp - NSLOT][0] >= cur_unit:
                    break
                ui, segs = pieces[nextp]
                s = nextp % NSLOT
                dsegs = []
                for (off, kc, n, src) in segs:
                    dst = slots[s][:, off:off + kc * n].rearrange("p (k n) -> p k n", k=kc)
                    dsegs.append((dst, src))
                fw.dma("pool", dsegs, writes=[RS[s]])
                nextp += 1

        for ui, (specs, fn) in enumerate(units):
            issue_loads(ui)
            for pi_ in upieces[ui]:
                assert pi_ < nextp, "piece not loaded: raise NSLOT"
            fn([(slots[pi_ % NSLOT], RS[pi_ % NSLOT]) for pi_ in upieces[ui]])

        fw.wait_all("sp", [xin[0][1], xin[1][1]])
        if debug:
            fw.wait_all("sp", [r for rr in RY for r in rr] + [r for rr in RX for r in rr])
    return nc


def _consts():
    c = np.zeros((128, NCST), np.float64)
    p = np.arange(128)
    half = 64
    invf = 10000.0 ** (-(np.arange(half, dtype=np.float32) / np.float32(half)))
    c[:, C_INVF] = np.concatenate([invf, invf]).astype(np.float32)
    c[:, C_SIGN] = np.where(p < 64, -1.0, 1.0)
    i = np.arange(128)
    for h in range(4):
        lg = math.log(1.0 - 2.0 ** (-5.0 - h))
        c[:, C_KDEC + h] = np.exp(lg * (127 - p))
        diff = i[None, :] - p[:, None]
        c[:, C_RMASK + h * 128:C_RMASK + (h + 1) * 128] = np.where(diff >= 0, np.exp(lg * np.maximum(diff, 0)), 0.0) * 128 ** -0.5
        c[:, C_QDEC + h * 128:C_QDEC + (h + 1) * 128] = (np.exp(lg * (i + 1.0)) * 128 ** -0.5)[None, :]
    c[:, C_CAUS:C_CAUS + 128] = (i[None, :] >= p[:, None]).astype(np.float64)
    c[:, C_UNEG:C_UNEG + 128] = np.where(p[:, None] <= i[None, :], -1.0 / 16.0, 0.0)
    c[:, C_ID:C_ID + 128] = np.eye(128)
    return c.astype(np.float32)


_CACHE = {}


def kernel(x, positions, norm_mix_pre, w_in, conv_w, ret_gn_w, gla_w_a2, gla_b_a, gla_gn_w,
           w_branch_a, w_branch_b, w_branch_c, w_out, norm_mix_post, norm_ffn_pre,
           w_ffn_gate, w_ffn_up, w_ffn_down, norm_ffn_post, _debug=False, _dd=DEPTH, _ncores=8):
    f32 = lambda a: np.ascontiguousarray(np.asarray(a, dtype=np.float32))
    x = f32(x)
    positions = np.ascontiguousarray(np.asarray(positions, dtype=np.int32))
    pv = np.zeros((128, _dd * LP), np.float32)
    for l in range(_dd):
        b = l * LP
        for j, g in enumerate((norm_mix_pre, norm_mix_post, norm_ffn_pre, norm_ffn_post)):
            pv[:, b + 8 * j:b + 8 * j + 8] = f32(g)[l].reshape(8, 128).T
        pv[:, b + 32:b + 44] = f32(conv_w)[l].reshape(3, 4, 128).transpose(2, 0, 1).reshape(128, 12)
        pv[:, b + 44:b + 48] = f32(ret_gn_w)[l].reshape(4, 128).T
        pv[:, b + 48:b + 52] = f32(gla_gn_w)[l].reshape(4, 128).T
    bA = np.ascontiguousarray(np.broadcast_to(f32(gla_b_a)[:_dd, None, :], (_dd, 128, 256)))
    shared = {
        "w_in": f32(w_in[:_dd]), "gla_w_a2": f32(gla_w_a2[:_dd]), "bA": bA,
        "w_branch_a": f32(w_branch_a[:_dd]), "w_branch_b": f32(w_branch_b[:_dd]), "w_branch_c": f32(w_branch_c[:_dd]),
        "w_out": f32(w_out[:_dd]), "w_ffn_gate": f32(w_ffn_gate[:_dd]), "w_ffn_up": f32(w_ffn_up[:_dd]),
        "w_ffn_down": f32(w_ffn_down[:_dd]), "pv": pv, "cst": _consts(),
    }
    key = bool(_debug)
    if key not in _CACHE:
        _CACHE[key] = build_program(debug=_debug)
    nc = _CACHE[key]
    in_maps = []
    for b in range(_ncores):
        m = dict(shared)
        m["x"] = x[b]
        m["pos"] = positions[b:b + 1]
        in_maps.append(m)
    res = run_bass_kernel_spmd(nc, in_maps, core_ids=list(range(_ncores)))
    out = np.stack([np.asarray(r["out"], dtype=np.float32) for r in res.results], axis=0)
    if _debug:
        kernel.last = res.results
    return out
```

```python
import math
import os
KSTOP = int(os.environ.get('KSTOP', '99'))
from contextlib import ExitStack

import numpy as np
import concourse.bass as bass
import concourse.mybir as mybir
from concourse.bass_utils import run_bass_kernel_spmd

F32 = mybir.dt.float32
BF16 = mybir.dt.bfloat16
I32 = mybir.dt.int32
ALU = mybir.AluOpType
AF = mybir.ActivationFunctionType
AX = mybir.AxisListType

D = 1024
T = 2048
DEPTH = 2
NTC = 4
KC = 8
IN_COLS = 8208
DFF = 2816
NF = 22
EPS = 1e-6
NSLOT = 4
SLOT = 4096
LP = 52

O_CU, O_CB, O_CC = 0, 512, 1024
O_RQ, O_RK, O_RV, O_RG = 1536, 2048, 2560, 3072
O_GQ, O_GK, O_GV, O_GR, O_GA = 3584, 3840, 4096, 4608, 5120
O_GATE = 5136

C_INVF, C_SIGN, C_KDEC, C_RMASK, C_QDEC, C_CAUS, C_UNEG, C_ID = 0, 1, 2, 6, 518, 1030, 1158, 1286
NCST = 1414


class Eng:
    def __init__(self, name, e, sem):
        self.name, self.e, self.sem = name, e, sem
        self.cnt = 0
        self.seen = {}


class Reg:
    __slots__ = ("w", "rs", "dsem", "dcnt", "excl")

    def __init__(self, excl=False):
        self.excl = excl
        self.w = None
        self.rs = {}
        self.dsem = None
        self.dcnt = 0


class FW:
    def __init__(self, nc, stack):
        self.nc = nc
        self.stack = stack
        self.E = {}
        for name, e in (("pe", nc.tensor), ("act", nc.scalar), ("dve", nc.vector),
                        ("pool", nc.gpsimd), ("sp", nc.sync)):
            sem = stack.enter_context(nc.semaphore("s_" + name))
            self.E[name] = Eng(name, e, sem)
        self.nsem = 5
        self.limit = None

    def sbuf(self, name, shape, dt):
        return self.stack.enter_context(self.nc.sbuf_tensor("sb_" + name, list(shape), dt))

    def psum(self, name, shape, dt):
        return self.stack.enter_context(self.nc.psum_tensor(name, list(shape), dt))

    def newsem(self):
        self.nsem += 1
        return self.stack.enter_context(self.nc.semaphore("d%d" % self.nsem))

    def _need(self, E, tok, need, same_ok):
        if tok is None:
            return
        if tok[0] == "e":
            F, n = tok[1], tok[2]
            if F is E and E.name in ("pe", "sp"):
                return
            key = F.name
            sem = F.sem
        else:
            _, sem, n, sid = tok
            key = ("d", sid)
        if E.seen.get(key, 0) >= n:
            return
        if need.get(key, (None, 0))[1] < n:
            need[key] = (sem, n)

    def _waits(self, E, reads, writes):
        need = {}
        for r in reads:
            self._need(E, r.w, need, False)
            if r.excl:
                for t in r.rs.values():
                    if t[0] == "e" and t[1] is E:
                        continue
                    self._need(E, t, need, True)
        for w in writes:
            self._need(E, w.w, need, False)
            for t in w.rs.values():
                self._need(E, t, need, True)
        for key, (sem, n) in need.items():
            E.e.wait_ge(sem, n)
            E.seen[key] = n

    def op(self, eng, fn, reads=(), writes=()):
        if self.limit is not None:
            if self.limit <= 0:
                return None
            self.limit -= 1
        E = self.E[eng]
        self._waits(E, reads, writes)
        inst = fn(E.e)
        E.cnt += 1
        inst.then_inc(E.sem, 1)
        tok = ("e", E, E.cnt)
        for r in reads:
            r.rs[E.name] = tok
        for w in writes:
            w.w = tok
            w.rs = {}
        return inst

    def dma(self, q, segs, reads=(), writes=(), owner=None):
        E = self.E[q]
        self._waits(E, reads, writes)
        owner = owner if owner is not None else (writes[0] if writes else reads[0])
        if owner.dsem is None:
            owner.dsem = self.newsem()
        for (o, i) in segs:
            inst = E.e.dma_start(out=o, in_=i)
            owner.dcnt += 16
            inst.then_inc(owner.dsem, 16)
        tok = ("d", owner.dsem, owner.dcnt, id(owner))
        for r in reads:
            r.rs[("d", id(owner))] = tok
        for w in writes:
            w.w = tok
            w.rs = {}

    def wait_all(self, eng, regs):
        self._waits(self.E[eng], [], regs)


def build_program(debug=False, depth=DEPTH, ntc=NTC, phases="ABCDEFG"):
    nc = bass.Bass("TRN2", target_bir_lowering=False)

    def din(name, shape, dt=F32):
        return nc.dram_tensor(name, list(shape), dt, kind="ExternalInput").ap()

    dd = max(depth, 1)
    x_d = din("x", [T, D])
    pos_d = din("pos", [1, T], I32)
    w_in_d = din("w_in", [dd, D, IN_COLS])
    wa2_d = din("gla_w_a2", [dd, 16, 256])
    bA_d = din("bA", [dd, 128, 256])
    wbr_d = [din("w_branch_" + n, [dd, 512, D]) for n in "abc"]
    wout_d = din("w_out", [dd, D, D])
    wg_d = din("w_ffn_gate", [dd, D, DFF])
    wu_d = din("w_ffn_up", [dd, D, DFF])
    wd_d = din("w_ffn_down", [dd, DFF, D])
    pv_d = din("pv", [128, dd * LP])
    cst_d = din("cst", [128, NCST])
    out_d = nc.dram_tensor("out", [T, D], F32, kind="ExternalOutput").ap()
    dbg_d = {}
    if debug:
        for n in ("ya", "yb", "yc"):
            dbg_d[n] = nc.dram_tensor("dbg_" + n, [4, 128, T], BF16, kind="ExternalOutput").ap()
        for n in ("xmix", "xffn"):
            dbg_d[n] = nc.dram_tensor("dbg_" + n, [8, 128, T], F32, kind="ExternalOutput").ap()

    st = ExitStack()
    with st:
        fw = FW(nc, st)
        op = fw.op

        x = fw.sbuf("x", [128, KC, T], F32)
        RX = [[Reg() for _ in range(NTC)] for _ in range(KC)]
        hT = fw.sbuf("hT", [128, KC, 512], BF16)
        RH = [Reg() for _ in range(KC)]
        ybuf = [fw.sbuf("y%d" % i, [128, 4, 512], BF16) for i in range(3)]
        RY = [[Reg() for _ in range(4)] for _ in range(3)]
        ACC = fw.sbuf("acc", [128, KC, 512], F32)
        RA = [Reg() for _ in range(KC)]
        mrg = fw.sbuf("mrg", [128, KC, 512], BF16)
        RM = [Reg() for _ in range(KC)]
        actb = fw.sbuf("actb", [128, 2, 2, 512], BF16)
        RACT = [[Reg() for _ in range(2)] for _ in range(2)]
        slots = [fw.sbuf("slot%d" % i, [128, SLOT], BF16) for i in range(NSLOT)]
        RS = [Reg() for _ in range(NSLOT)]
        P = [fw.psum("ps%d" % i, [128, 512], F32) for i in range(8)]
        RP = [Reg(excl=True) for _ in range(8)]

        def wt(name, shape=(128, 512), dt=F32):
            return fw.sbuf(name, shape, dt), Reg()

        T0, R0 = wt("T0"); T1, R1 = wt("T1"); T2, R2 = wt("T2"); T3, R3 = wt("T3")
        T4, R4 = wt("T4"); T5, R5 = wt("T5")
        sqb = [wt("sqb%d" % i, dt=BF16) for i in range(2)]
        qh, Rqh = wt("qh", dt=BF16); qt, Rqt = wt("qt", dt=BF16); kh, Rkh = wt("kh", dt=BF16)
        vtok, Rvt = wt("vtok", (128, 4, 256), BF16)
        ktok, Rkt = wt("ktok", (128, 4, 128), BF16)
        stm, Rstm = wt("stm", (128, 128), BF16)
        zb, Rzb = wt("zb", (128, 514), F32)
        zc, Rzc = wt("zc", (128, 4, 2), F32)
        spb, Rspb = wt("spb", (128, 4, 256), F32)
        gdb, Rgdb = wt("gdb", (128, 512), F32)
        gq = [wt("gq%d" % i, dt=BF16) for i in range(2)]
        nm, Rnm = wt("nm", (128, 4), F32)
        tS, RtS = wt("tS", (128, 128), F32)
        Sret, RSr = wt("Sret", (128, 4, 128), F32)
        Sretb, RSrb = wt("Sretb", (128, 4, 128), BF16)
        RSr = [Reg() for _ in range(4)]; RSrb = [Reg() for _ in range(4)]
        Sgla, RSg = wt("Sgla", (128, 2, 128), F32)
        Sglab, RSgb = wt("Sglab", (128, 2, 128), BF16)
        RSg = [Reg() for _ in range(2)]; RSgb = [Reg() for _ in range(2)]
        cosT, Rcos = wt("cosT"); sinT, Rsin = wt("sinT")
        posi, Rposi = wt("posi", (128, 512), I32)
        kint, Rkint = wt("kint", (128, 512), I32)
        cst, Rcst = wt("cst", (128, NCST), F32)
        pv, Rpv = wt("pv", (128, dd * LP), F32)
        bA, RbA = wt("bA", (128, dd, 256), F32)
        wa2, Rwa2 = wt("wa2", (128, dd, 256), F32)
        idb, Ridb = wt("idb", (128, 128), BF16)
        onesD, RoD = wt("onesD", (128, 128), BF16)
        onesH, RoH = wt("onesH", (128, 128), BF16)
        xin = [wt("xin%d" % i, (128, D), F32) for i in range(2)]

        DBG = Reg()

        def tt(eng, out, in0, in1, o, reads, writes):
            op(eng, lambda e: e.tensor_tensor(out=out, in0=in0, in1=in1, op=o), reads, writes)

        def ts(eng, out, in0, s1, s2, o0, o1, reads, writes):
            if o1 is None:
                op(eng, lambda e: e.tensor_scalar(out=out, in0=in0, scalar1=s1, scalar2=None, op0=o0), reads, writes)
            else:
                op(eng, lambda e: e.tensor_scalar(out=out, in0=in0, scalar1=s1, scalar2=s2, op0=o0, op1=o1), reads, writes)

        def stt(out, in0, sc, in1, o0, o1, reads, writes):
            op("dve", lambda e: e.scalar_tensor_tensor(out=out, in0=in0, scalar=sc, in1=in1, op0=o0, op1=o1), reads, writes)

        def act(out, in_, func, reads, writes, bias=None, scale=None):
            kw = {}
            if bias is not None:
                kw["bias"] = bias
            if scale is not None:
                kw["scale"] = scale
            op("act", lambda e: e.activation(out=out, in_=in_, func=func, **kw), reads, writes)

        def acopy(out, in_, reads, writes):
            op("act", lambda e: e.copy(out=out, in_=in_), reads, writes)

        def mm(out, pairs, reads, writes):
            def f(e):
                inst = None
                n = len(pairs)
                for i, (l, r) in enumerate(pairs):
                    inst = e.matmul(out, lhsT=l, rhs=r, start=(i == 0), stop=(i == n - 1))
                return inst
            op("pe", f, reads, writes)

        def mm1(out, l, r, start, stop, reads, writes):
            op("pe", lambda e: e.matmul(out, lhsT=l, rhs=r, start=start, stop=stop), reads, writes)

        def tr(out, in_, ident, reads, writes):
            op("pe", lambda e: e.transpose(out=out, in_=in_, identity=ident), reads, writes)

        def pcol(l, off):
            return pv[:, l * LP + off: l * LP + off + 1]

        def tsl(tc):
            return slice(tc * 512, (tc + 1) * 512)

        units = []

        cur = {"l": 0, "n": 0}

        def unit(specs, fn):
            keys = []
            for _ in specs:
                keys.append((cur["l"], cur["n"]))
                cur["n"] += 1
            units.append((specs, fn, keys))

        def wseg(dram_ap_rows_by_cols, kc):
            return dram_ap_rows_by_cols.rearrange("(k p) n -> p k n", p=128)

        def setup(_):
            fw.dma("sp", [(cst[:], cst_d)], writes=[Rcst])
            fw.dma("sp", [(pv[:], pv_d)], writes=[Rpv])
            fw.dma("sp", [(bA[:], bA_d.rearrange("l p n -> p l n"))], writes=[RbA])
            op("dve", lambda e: e.memset(wa2[:], 0.0), [], [Rwa2])
            fw.dma("sp", [(wa2[0:16], wa2_d.rearrange("l r n -> r l n"))], writes=[Rwa2])
            for (g_, rg_) in gq:
                op("dve", lambda e: e.memset(g_[:], 0.0), [], [rg_])
            op("dve", lambda e: e.tensor_copy(out=idb[:], in_=cst[:, C_ID:C_ID + 128]), [Rcst], [Ridb])
            op("dve", lambda e: e.memset(onesD[:], 1.0 / D), [], [RoD])
            op("dve", lambda e: e.memset(onesH[:], 1.0 / 128), [], [RoH])

        unit([], setup)

        def load_x(tb):
            def f(_):
                xt_, rx_ = xin[tb % 2]
                fw.dma("sp", [(xt_[:], x_d[tb * 128:(tb + 1) * 128, :])], writes=[rx_])
                tc = tb // 4
                for half in range(2):
                    pb = (tb * 2 + half) % 4
                    for j in range(4):
                        k = half * 4 + j
                        tr(P[pb][:, j * 128:(j + 1) * 128], xt_[:, k * 128:(k + 1) * 128], cst[:, C_ID:C_ID + 128],
                           [rx_, Rcst], [RP[pb]])
                    dst = x[:, half * 4:half * 4 + 4, tb * 128:(tb + 1) * 128]
                    src = P[pb][:].rearrange("p (a b) -> p a b", a=4)
                    if half == 0:
                        op("act", lambda e: e.copy(out=dst, in_=src), [RP[pb]], [RX[k_][tc] for k_ in range(half * 4, half * 4 + 4)])
                    else:
                        op("dve", lambda e: e.tensor_copy(out=dst, in_=src), [RP[pb]], [RX[k_][tc] for k_ in range(half * 4, half * 4 + 4)])
            return f

        for tb in range(16):
            unit([], load_x(tb))

        def rstd_from(pb):
            act(T0[:], P[pb][:], AF.Ln, [RP[pb]], [R0], bias=EPS, scale=1.0)
            act(T1[:], T0[:], AF.Exp, [R0], [R1], scale=-0.5)

        def prenorm(l, tc, goff):
            def f(_):
                for k in range(KC):
                    sq, rsq = sqb[k % 2]
                    act(sq[:], x[:, k, tsl(tc)], AF.Square, [RX[k][tc]], [rsq])
                    mm1(P[7][:], onesD[:], sq[:], k == 0, k == KC - 1, [rsq, RoD], [RP[7]])
                rstd_from(7)
                for k in range(KC):
                    stt(hT[:, k, :], x[:, k, tsl(tc)], pcol(l, goff + k), T1[:], ALU.mult, ALU.mult,
                        [RX[k][tc], R1, Rpv], [RH[k]])
            return f

        def postnorm(l, tc, goff, dbgname=None):
            def f(_):
                for k in range(KC):
                    sq, rsq = sqb[k % 2]
                    act(sq[:], ACC[:, k, :], AF.Square, [RA[k]], [rsq])
                    mm1(P[7][:], onesD[:], sq[:], k == 0, k == KC - 1, [rsq, RoD], [RP[7]])
                rstd_from(7)
                for k in range(KC):
                    stt(T3[:], ACC[:, k, :], pcol(l, goff + k), T1[:], ALU.mult, ALU.mult, [RA[k], R1, Rpv], [R3])
                    tt("dve", x[:, k, tsl(tc)], x[:, k, tsl(tc)], T3[:], ALU.add, [R3, RX[k][tc]], [RX[k][tc]])
                    if debug and dbgname is not None and l == 0:
                        fw.dma("sp", [(dbg_d[dbgname][k, :, tsl(tc)], x[:, k, tsl(tc)])], reads=[RX[k][tc]])
            return f

        def proj(pb, wv, c0, reads_w, n=128):
            mm(P[pb][0:n, :], [(wv[:, k, c0:c0 + n], hT[:, k, :]) for k in range(KC)], [reads_w] + RH, [RP[pb]])

        def proj_tok(pb, ncol, wv, c0, reads_w, blk, o0):
            mm(P[pb][:, o0:o0 + ncol], [(hT[:, k, blk * 128:(blk + 1) * 128], wv[:, k, c0:c0 + ncol]) for k in range(KC)],
               [reads_w] + RH, [RP[pb]])

        def rope_tables(b_tc):
            tc = b_tc

            def f(_):
                fw.dma("sp", [(posi[:], pos_d[0:1, tsl(tc)].partition_broadcast(128))], writes=[Rposi])
                op("dve", lambda e: e.tensor_copy(out=T0[:], in_=posi[:]), [Rposi], [R0])
                ts("dve", T2[:], T0[:], cst[:, C_INVF:C_INVF + 1], None, ALU.mult, None, [R0, Rcst], [R2])
                ts("dve", kint[:], T2[:], 1.0 / (2 * math.pi), None, ALU.mult, None, [R2], [Rkint])
                op("dve", lambda e: e.tensor_copy(out=T0[:], in_=kint[:]), [Rkint], [R0])
                C1 = 6.28125
                C2 = 2 * math.pi - C1
                stt(T2[:], T0[:], -C1, T2[:], ALU.mult, ALU.add, [R0, R2], [R2])
                stt(T2[:], T0[:], -C2, T2[:], ALU.mult, ALU.add, [R0, R2], [R2])
                ts("dve", T2[:], T2[:], math.pi, -math.pi, ALU.min, ALU.max, [R2], [R2])
                act(T3[:], T2[:], AF.Sin, [R2], [R3])
                ts("dve", sinT[:], T3[:], cst[:, C_SIGN:C_SIGN + 1], None, ALU.mult, None, [R3, Rcst], [Rsin])
                act(T3[:], T2[:], AF.Sin, [R2], [R3], scale=0.5)
                tt("dve", T3[:], T3[:], T3[:], ALU.mult, [R3], [R3])
                ts("dve", cosT[:], T3[:], -2.0, 1.0, ALU.mult, ALU.add, [R3], [Rcos])
            return f

        def reset_states(_):
            op("dve", lambda e: e.memset(zc[:], 0.0), [], [Rzc])
            op("dve", lambda e: e.memset(Sret[:], 0.0), [], RSr)
            op("dve", lambda e: e.memset(Sretb[:], 0.0), [], RSrb)
            op("dve", lambda e: e.memset(Sgla[:], 0.0), [], RSg)
            op("dve", lambda e: e.memset(Sglab[:], 0.0), [], RSgb)

        def conv_unit(l, tc, c):
            win = w_in_d[l]
            specs = [[(0, 8, 128, wseg(win[:, O_CU + c * 128:O_CU + (c + 1) * 128], 8)),
                      (1024, 8, 128, wseg(win[:, O_CB + c * 128:O_CB + (c + 1) * 128], 8)),
                      (2048, 8, 128, wseg(win[:, O_CC + c * 128:O_CC + (c + 1) * 128], 8))]]

            def f(pc):
                (sl, rs) = pc[0]
                wv = sl[:, 0:3072].rearrange("p (s k n) -> p s k n", s=3, k=8)
                proj(0, wv[:, 0], 0, rs)
                proj(1, wv[:, 1], 0, rs)
                proj(2, wv[:, 2], 0, rs)
                acopy(T0[:], P[0][:], [RP[0]], [R0])
                acopy(zb[:, 0:2], zc[:, c, :], [Rzc], [Rzb])
                tt("dve", zb[:, 2:514], P[2][:], T0[:], ALU.mult, [RP[2], R0], [Rzb])
                acopy(zc[:, c, :], zb[:, 512:514], [Rzb], [Rzc])
                ts("dve", T2[:], zb[:, 2:514], pcol(l, 32 + 8 + c), None, ALU.mult, None, [Rzb, Rpv], [R2])
                stt(T2[:], zb[:, 1:513], pcol(l, 32 + 4 + c), T2[:], ALU.mult, ALU.add, [Rzb, Rpv, R2], [R2])
                stt(T2[:], zb[:, 0:512], pcol(l, 32 + 0 + c), T2[:], ALU.mult, ALU.add, [Rzb, Rpv, R2], [R2])
                tt("dve", ybuf[0][:, c, :], P[1][:], T2[:], ALU.mult, [RP[1], R2], [RY[0][c]])
                if debug and l == 0:
                    fw.dma("sp", [(dbg_d["ya"][c, :, tsl(tc)], ybuf[0][:, c, :])], reads=[RY[0][c]])
            unit(specs, f)

        def rope(pb):
            tt("dve", T0[:], P[pb][:], cosT[:], ALU.mult, [RP[pb], Rcos], [R0])
            acopy(T5[:], P[pb][:], [RP[pb]], [R5])
            acopy(T2[0:64, :], T5[64:128, :], [R5], [R2])
            acopy(T2[64:128, :], T5[0:64, :], [R5], [R2])
            tt("dve", T2[:], T2[:], sinT[:], ALU.mult, [R2, Rsin], [R2])
            tt("dve", T3[:], T0[:], T2[:], ALU.add, [R0, R2], [R3])

        def headnorm(pbo, pbm, sg_t, sg_r, gcol, out_ap, out_reg):
            sq, rsq = sqb[0]
            act(sq[:], P[pbo][:], AF.Square, [RP[pbo]], [rsq])
            mm1(P[pbm][:], onesH[:], sq[:], True, True, [rsq, RoH], [RP[pbm]])
            rstd_from(pbm)
            tt("dve", T3[:], P[pbo][:], T1[:], ALU.mult, [RP[pbo], R1], [R3])
            stt(out_ap, T3[:], gcol, sg_t[:], ALU.mult, ALU.mult, [R3, Rpv, sg_r], [out_reg])

        GAM = [1.0 - 2.0 ** (-5.0 - h) for h in range(4)]

        def ret_unit(l, tc, h):
            win = w_in_d[l]
            specs = [[(i * 1024, 8, 128, wseg(win[:, o + h * 128:o + (h + 1) * 128], 8))
                      for i, o in enumerate((O_RQ, O_RK, O_RV, O_RG))]]

            def f(pc):
                (sl, rs) = pc[0]
                wv = sl[:, 0:4096].rearrange("p (s k n) -> p s k n", s=4, k=8)
                proj(0, wv[:, 0], 0, rs)
                proj(1, wv[:, 1], 0, rs)
                proj(2, wv[:, 3], 0, rs)
                for blk in range(4):
                    proj_tok(3, 128, wv[:, 2], 0, rs, blk, blk * 128)
                if KSTOP <= 1: return
                if 'KOPS' in os.environ: fw.limit = int(os.environ['KOPS'])
                rope(0)
                op("dve", lambda e: e.tensor_copy(out=qh[:], in_=T3[:]), [R3], [Rqh])
                tt("dve", qt[:].rearrange("p (a b) -> p a b", a=4), T3[:].rearrange("p (a b) -> p a b", a=4),
                   cst[:, C_QDEC + h * 128:C_QDEC + (h + 1) * 128].unsqueeze(1).to_broadcast([128, 4, 128]),
                   ALU.mult, [R3, Rcst], [Rqt])
                if KSTOP <= 2:
                    fw.limit = None
                    return
                rope(1)
                op("dve", lambda e: e.tensor_copy(out=kh[:], in_=T3[:]), [R3], [Rkh])
                if KSTOP <= 3: return
                act(T4[:], P[2][:], AF.Silu, [RP[2]], [R4])
                if KSTOP <= 4: return
                pv3 = P[3][:].rearrange("p (a b) -> p a b", a=4)
                op("dve", lambda e: e.tensor_reduce(out=nm[:], in_=pv3, axis=AX.X, op=ALU.add), [RP[3]], [Rnm])
                ts("dve", nm[:], nm[:], -1.0 / 128, None, ALU.mult, None, [Rnm], [Rnm])
                for blk in range(4):
                    ts("dve", vtok[:, blk, 0:128], P[3][:, blk * 128:(blk + 1) * 128], nm[:, blk:blk + 1], None,
                       ALU.add, None, [RP[3], Rnm], [Rvt])
                if KSTOP <= 5: return
                p4b = P[4][:].bitcast(BF16)
                for blk in range(4):
                    tr(p4b[:, blk * 128:(blk + 1) * 128], kh[:, blk * 128:(blk + 1) * 128], idb[:], [Rkh, Ridb], [RP[4]])
                ts("dve", ktok[:].rearrange("p a b -> p (a b)"), p4b[:, 0:512], cst[:, C_KDEC + h:C_KDEC + h + 1], None,
                   ALU.mult, None, [RP[4], Rcst], [Rkt])
                if KSTOP <= 6: return
                g128 = GAM[h] ** 128
                for blk in range(4):
                    bs = slice(blk * 128, (blk + 1) * 128)
                    mm(P[5][:, 0:128], [(kh[:, bs], qh[:, bs])], [Rkh, Rqh], [RP[5]])
                    tt("dve", stm[:], P[5][:, 0:128], cst[:, C_RMASK + h * 128:C_RMASK + (h + 1) * 128], ALU.mult,
                       [RP[5], Rcst], [Rstm])
                    mm(P[6][:, bs], [(vtok[:, blk, 0:128], stm[:]), (Sretb[:, h, :], qt[:, bs])],
                       [Rvt, Rstm, RSrb[h], Rqt], [RP[6]])
                    mm(P[7][:, 0:128], [(ktok[:, blk, :], vtok[:, blk, 0:128])], [Rkt, Rvt], [RP[7]])
                    stt(Sret[:, h, :], Sret[:, h, :], g128, P[7][:, 0:128], ALU.mult, ALU.add, [RSr[h], RP[7]], [RSr[h]])
                    acopy(Sretb[:, h, :], Sret[:, h, :], [RSr[h]], [RSrb[h]])
                if KSTOP <= 7: return
                headnorm(6, 7, T4, R4, pcol(l, 44 + h), ybuf[1][:, h, :], RY[1][h])
                if debug and l == 0:
                    fw.dma("sp", [(dbg_d["yb"][h, :, tsl(tc)], ybuf[1][:, h, :])], reads=[RY[1][h]])
            unit(specs, f)

        def gla_prelude(l, tc):
            win = w_in_d[l]
            specs = [[(0, 8, 128, wseg(win[:, O_GA:O_GA + 128], 8))]]

            def f(pc):
                (sl, rs) = pc[0]
                wv = sl[:, 0:1024].rearrange("p (k n) -> p k n", k=8)
                mm(P[0][:, :], [(wv[:, k, :], hT[:, k, :]) for k in range(KC)], [rs] + RH, [RP[0]])
                acopy(gdb[:], P[0][:, :], [RP[0]], [Rgdb])
                for half in range(2):
                    pb = 1 + half
                    for j in range(2):
                        blk = half * 2 + j
                        mm(P[pb][:, j * 256:(j + 1) * 256], [(gdb[:, blk * 128:(blk + 1) * 128], wa2[:, l, :])],
                           [Rgdb, Rwa2], [RP[pb]])
                    sv = spb[:, half * 2:half * 2 + 2, :]
                    tt("dve", sv, P[pb][:].rearrange("p (a b) -> p a b", a=2),
                       bA[:, l, :].unsqueeze(1).to_broadcast([128, 2, 256]), ALU.add, [RP[pb], RbA], [Rspb])
                    act(sv, sv, AF.Exp, [Rspb], [Rspb], scale=-1.0)
                    act(sv, sv, AF.Ln, [Rspb], [Rspb], bias=1.0, scale=1.0)
                for hp in range(2):
                    for blk in range(4):
                        mm(P[3 + hp][:, blk * 128:(blk + 1) * 128],
                           [(spb[:, blk, hp * 128:(hp + 1) * 128], cst[:, C_UNEG:C_UNEG + 128])], [Rspb, Rcst], [RP[3 + hp]])
            unit(specs, f)

        def gla_unit(l, tc, hp):
            win = w_in_d[l]
            specs = [[(0, 8, 128, wseg(win[:, O_GQ + hp * 128:O_GQ + (hp + 1) * 128], 8)),
                      (1024, 8, 128, wseg(win[:, O_GK + hp * 128:O_GK + (hp + 1) * 128], 8)),
                      (2048, 8, 256, wseg(win[:, O_GV + hp * 256:O_GV + (hp + 1) * 256], 8))],
                     [(0, 8, 256, wseg(win[:, O_GR + hp * 256:O_GR + (hp + 1) * 256], 8))]]

            def f(pc):
                (sl, rs), (sl2, rs2) = pc
                wq = sl[:, 0:1024].rearrange("p (k n) -> p k n", k=8)
                wk = sl[:, 1024:2048].rearrange("p (k n) -> p k n", k=8)
                wvv = sl[:, 2048:4096].rearrange("p (k n) -> p k n", k=8)
                wr = sl2[:, 0:2048].rearrange("p (k n) -> p k n", k=8)
                pc_ = 3 + hp
                act(T0[:], P[pc_][:], AF.Exp, [RP[pc_]], [R0])
                act(T2[:], P[pc_][:], AF.Exp, [RP[pc_]], [R2], scale=-1.0)
                proj(0, wq, 0, rs)
                proj(1, wk, 0, rs)
                for e_ in range(2):
                    ps = slice(64 * e_, 64 * e_ + 64)
                    stt(gq[e_][0][ps, :], P[0][ps, :], 0.125, T0[ps, :], ALU.mult, ALU.mult, [RP[0], R0], [gq[e_][1]])
                tt("dve", kh[:], P[1][:], T2[:], ALU.mult, [RP[1], R2], [Rkh])
                for half, pb in ((0, 2), (1, 5)):
                    for j in range(2):
                        proj_tok(pb, 256, wvv, 0, rs, half * 2 + j, j * 256)
                    acopy(vtok[:, half * 2:half * 2 + 2, :], P[pb][:].rearrange("p (a b) -> p a b", a=2), [RP[pb]], [Rvt])
                p6b = P[6][:].bitcast(BF16)
                for blk in range(4):
                    tr(p6b[:, blk * 128:(blk + 1) * 128], kh[:, blk * 128:(blk + 1) * 128], idb[:], [Rkh, Ridb], [RP[6]])
                acopy(ktok[:].rearrange("p a b -> p (a b)"), p6b[:, 0:512], [RP[6]], [Rkt])
                proj(7, wr, 0, rs2)
                act(T4[:], P[7][:], AF.Silu, [RP[7]], [R4])
                proj(5, wr, 128, rs2)
                act(T5[:], P[5][:], AF.Silu, [RP[5]], [R5])
                for blk in range(4):
                    bs = slice(blk * 128, (blk + 1) * 128)
                    for e_ in range(2):
                        gq_, rgq_ = gq[e_]
                        mm(P[0][:, 0:128], [(kh[:, bs], gq_[:, bs])], [Rkh, rgq_], [RP[0]])
                        tt("dve", stm[:], P[0][:, 0:128], cst[:, C_CAUS:C_CAUS + 128], ALU.mult, [RP[0], Rcst], [Rstm])
                        mm(P[1 + e_][:, bs], [(vtok[:, blk, e_ * 128:(e_ + 1) * 128], stm[:]), (Sglab[:, hp, :], gq_[:, bs])],
                           [Rvt, Rstm, RSgb[hp], rgq_], [RP[1 + e_]])
                    for e_ in range(2):
                        mm(P[6][:, e_ * 128:(e_ + 1) * 128], [(ktok[:, blk, :], vtok[:, blk, e_ * 128:(e_ + 1) * 128])],
                           [Rkt, Rvt], [RP[6]])
                    for e_ in range(2):
                        ps = slice(64 * e_, 64 * e_ + 64)
                        tt("dve", tS[ps, :], P[6][ps, e_ * 128:(e_ + 1) * 128], Sgla[ps, hp, :], ALU.add, [RP[6], RSg[hp]], [RtS])
                    ts("dve", Sgla[:, hp, :], tS[:], T0[:, blk * 128 + 127:blk * 128 + 128], None, ALU.mult, None,
                       [RtS, R0], [RSg[hp]])
                    acopy(Sglab[:, hp, :], Sgla[:, hp, :], [RSg[hp]], [RSgb[hp]])
                for e_, (sg_t, sg_r) in enumerate(((T4, R4), (T5, R5))):
                    hh = 2 * hp + e_
                    headnorm(1 + e_, 7, sg_t, sg_r, pcol(l, 48 + hh), ybuf[2][:, hh, :], RY[2][hh])
                    if debug and l == 0:
                        fw.dma("sp", [(dbg_d["yc"][hh, :, tsl(tc)], ybuf[2][:, hh, :])], reads=[RY[2][hh]])
            unit(specs, f)

        def merge_unit(l, tc, i):
            win = w_in_d[l]
            c0 = O_GATE + i * 1024
            specs = [[(0, 8, 512, wseg(win[:, c0:c0 + 512], 8))],
                     [(0, 8, 512, wseg(win[:, c0 + 512:c0 + 1024], 8))],
                     [(0, 4, 1024, wseg(wbr_d[i][l], 4))]]

            def f(pc):
                (slb, rsb) = pc[2]
                wb_ = slb[:, 0:4096].rearrange("p (k n) -> p k n", k=4)
                for half in range(2):
                    (sl, rs) = pc[half]
                    wg_ = sl[:, 0:4096].rearrange("p (k n) -> p k n", k=8)
                    for j in range(4):
                        oc = half * 4 + j
                        pa, pb = (0, 1) if j % 2 == 0 else (2, 3)
                        proj(pa, wg_, j * 128, rs)
                        mm(P[pb][:], [(wb_[:, kk, oc * 128:(oc + 1) * 128], ybuf[i][:, kk, :]) for kk in range(4)],
                           [rsb] + RY[i], [RP[pb]])
                        tw, rw = (T0, R0) if j % 2 == 0 else (T2, R2)
                        act(tw[:], P[pa][:], AF.Tanh, [RP[pa]], [rw], scale=0.5)
                        if i == 0:
                            stt(ACC[:, oc, :], tw[:], 1.0, P[pb][:], ALU.add, ALU.mult, [rw, RP[pb]], [RA[oc]])
                        else:
                            stt(tw[:], tw[:], 1.0, P[pb][:], ALU.add, ALU.mult, [rw, RP[pb]], [rw])
                            tt("dve", ACC[:, oc, :], ACC[:, oc, :], tw[:], ALU.add, [RA[oc], rw], [RA[oc]])
                            if i == 2:
                                op("act", lambda e: e.mul(out=mrg[:, oc, :], in_=ACC[:, oc, :], mul=0.5), [RA[oc]], [RM[oc]])
            unit(specs, f)

        def out_unit(l, tc, half):
            specs = [[(0, 8, 512, wseg(wout_d[l][:, half * 512:(half + 1) * 512], 8))]]

            def f(pc):
                (sl, rs) = pc[0]
                wo = sl[:, 0:4096].rearrange("p (k n) -> p k n", k=8)
                for j in range(4):
                    oc = half * 4 + j
                    pa = j % 4
                    mm(P[pa][:], [(wo[:, k, j * 128:(j + 1) * 128], mrg[:, k, :]) for k in range(KC)], [rs] + RM, [RP[pa]])
                    acopy(ACC[:, oc, :], P[pa][:], [RP[pa]], [RA[oc]])
            unit(specs, f)

        def ffn_unit(l, tc, g):
            c0 = g * 256
            specs = [[(0, 8, 256, wseg(wg_d[l][:, c0:c0 + 256], 8)),
                      (2048, 8, 256, wseg(wu_d[l][:, c0:c0 + 256], 8))],
                     [(0, 2, 1024, wseg(wd_d[l][c0:c0 + 256, :], 2))]]

            def f(pc):
                (sl, rs), (sl2, rs2) = pc
                wg_ = sl[:, 0:2048].rearrange("p (k n) -> p k n", k=8)
                wu_ = sl[:, 2048:4096].rearrange("p (k n) -> p k n", k=8)
                wd_ = sl2[:, 0:2048].rearrange("p (k n) -> p k n", k=2)
                par = g % 2
                for j in range(2):
                    pa, pb = (0, 1) if j == 0 else (2, 3)
                    proj(pa, wg_, j * 128, rs)
                    proj(pb, wu_, j * 128, rs)
                    tw, rw = (T0, R0) if j == 0 else (T2, R2)
                    act(tw[:], P[pa][:], AF.Silu, [RP[pa]], [rw])
                    tt("dve", actb[:, par, j, :], tw[:], P[pb][:], ALU.mult, [rw, RP[pb]], [RACT[par][j]])
                for oc in range(KC):
                    pa = 4 + oc % 3
                    mm(P[pa][:], [(wd_[:, j, oc * 128:(oc + 1) * 128], actb[:, par, j, :]) for j in range(2)],
                       [rs2] + RACT[par], [RP[pa]])
                    if g == 0:
                        acopy(ACC[:, oc, :], P[pa][:], [RP[pa]], [RA[oc]])
                    else:
                        tt("dve", ACC[:, oc, :], ACC[:, oc, :], P[pa][:], ALU.add, [RA[oc], RP[pa]], [RA[oc]])
            unit(specs, f)

        ROUT = [Reg(), Reg()]

        def store_x(tb):
            def f(_):
                tc = tb // 4
                ot, ro = xin[tb % 2]
                for half in range(2):
                    pb = (tb * 2 + half) % 4
                    for j in range(4):
                        k = half * 4 + j
                        tr(P[pb][:, j * 128:(j + 1) * 128], x[:, k, tb * 128:(tb + 1) * 128], cst[:, C_ID:C_ID + 128],
                           [RX[k][tc], Rcst], [RP[pb]])
                    if half == 0:
                        acopy(ot[:, 0:512], P[pb][:], [RP[pb]], [ro])
                    else:
                        op("dve", lambda e: e.tensor_copy(out=ot[:, 512:1024], in_=P[pb][:]), [RP[pb]], [ro])
                fw.dma("sp", [(out_d[tb * 128:(tb + 1) * 128, :], ot[:])], reads=[ro], owner=ROUT[tb % 2])
            return f

        for l in range(depth):
            unit([], reset_states)
            for tc in range(ntc):
                cur["l"] = l
                cur["n"] = 0
                unit([], prenorm(l, tc, 0))
                if "A" in phases:
                    for c in range(4):
                        conv_unit(l, tc, c)
                if "B" in phases:
                    if os.environ.get('KROPE', '1') == '1':
                        unit([], rope_tables(tc))
                    for h in range(4):
                        ret_unit(l, tc, h)
                if "C" in phases:
                    gla_prelude(l, tc)
                    for hp in range(2):
                        gla_unit(l, tc, hp)
                if "D" in phases:
                    for i in range(3):
                        merge_unit(l, tc, i)
                if "E" in phases:
                    for half in range(2):
                        out_unit(l, tc, half)
                    unit([], postnorm(l, tc, 8, "xmix"))
                if "F" in phases:
                    unit([], prenorm(l, tc, 16))
                    for g in range(NF // 2):
                        ffn_unit(l, tc, g)
                    unit([], postnorm(l, tc, 24, "xffn"))
        for tb in range(16):
            unit([], store_x(tb))

        pieces = []
        upieces = []
        for ui, (specs, fn, keys) in enumerate(units):
            idxs = []
            for segs, key in zip(specs, keys):
                idxs.append(len(pieces))
                pieces.append((ui, segs, key))
            upieces.append(idxs)
        GSZ = 6
        first = {}
        for (ui, segs, key) in pieces:
            first.setdefault(key, segs)
        npc = max([k[1] for k in first] + [0]) + 1
        scr = [nc.dram_tensor("scr%d" % l, [npc, 128, SLOT], BF16).ap() for l in range(max(depth, 1))]
        ngrp = (npc + GSZ - 1) // GSZ
        RG = [[Reg() for _ in range(ngrp)] for _ in range(max(depth, 1))]
        for l in range(depth):
            for g in range(ngrp):
                csegs = []
                for j in range(g * GSZ, min(npc, (g + 1) * GSZ)):
                    if (l, j) not in first:
                        continue
                    for (off, kc, n, src) in first[(l, j)]:
                        dst = scr[l][j][:, off:off + kc * n].rearrange("p (k n) -> p k n", k=kc)
                        csegs.append((dst, src))
                if csegs:
                    fw.dma("pool", csegs, writes=[RG[l][g]])
        nextp = 0

        def issue_loads(cur_unit):
            nonlocal nextp
            while nextp < len(pieces):
                if nextp >= NSLOT and pieces[nextp - NSLOT][0] >= cur_unit:
                    break
                ui, segs, (l_, j_) = pieces[nextp]
                s_ = nextp % NSLOT
                L = max(off + kc * n for (off, kc, n, src) in segs)
                fw.dma("sp", [(slots[s_][:, 0:L], scr[l_][j_][:, 0:L])], reads=[RG[l_][j_ // GSZ]], writes=[RS[s_]])
                nextp += 1

        for ui, (specs, fn, keys) in enumerate(units):
            issue_loads(ui)
            for pi_ in upieces[ui]:
                assert pi_ < nextp, "piece not loaded: raise NSLOT"
            fn([(slots[pi_ % NSLOT], RS[pi_ % NSLOT]) for pi_ in upieces[ui]])

        fw.wait_all("sp", [xin[0][1], xin[1][1]])
        if debug:
            fw.wait_all("sp", [r for rr in RY for r in rr] + [r for rr in RX for r in rr])
    return nc


def _consts():
    c = np.zeros((128, NCST), np.float64)
    p = np.arange(128)
    half = 64
    invf = 10000.0 ** (-(np.arange(half, dtype=np.float32) / np.float32(half)))
    c[:, C_INVF] = np.concatenate([invf, invf]).astype(np.float32)
    c[:, C_SIGN] = np.where(p < 64, -1.0, 1.0)
    i = np.arange(128)
    for h in range(4):
        lg = math.log(1.0 - 2.0 ** (-5.0 - h))
        c[:, C_KDEC + h] = np.exp(lg * (127 - p))
        diff = i[None, :] - p[:, None]
        c[:, C_RMASK + h * 128:C_RMASK + (h + 1) * 128] = np.where(diff >= 0, np.exp(lg * np.maximum(diff, 0)), 0.0) * 128 ** -0.5
        c[:, C_QDEC + h * 128:C_QDEC + (h + 1) * 128] = (np.exp(lg * (i + 1.0)) * 128 ** -0.5)[None, :]
    c[:, C_CAUS:C_CAUS + 128] = (i[None, :] >= p[:, None]).astype(np.float64)
    c[:, C_UNEG:C_UNEG + 128] = np.where(p[:, None] <= i[None, :], -1.0 / 16.0, 0.0)
    c[:, C_ID:C_ID + 128] = np.eye(128)
    return c.astype(np.float32)


_CACHE = {}


def kernel(x, positions, norm_mix_pre, w_in, conv_w, ret_gn_w, gla_w_a2, gla_b_a, gla_gn_w,
           w_branch_a, w_branch_b, w_branch_c, w_out, norm_mix_post, norm_ffn_pre,
           w_ffn_gate, w_ffn_up, w_ffn_down, norm_ffn_post, _debug=False, _dd=DEPTH, _ncores=8):
    f32 = lambda a: np.ascontiguousarray(np.asarray(a, dtype=np.float32))
    x = f32(x)
    positions = np.ascontiguousarray(np.asarray(positions, dtype=np.int32))
    pv = np.zeros((128, _dd * LP), np.float32)
    for l in range(_dd):
        b = l * LP
        for j, g in enumerate((norm_mix_pre, norm_mix_post, norm_ffn_pre, norm_ffn_post)):
            pv[:, b + 8 * j:b + 8 * j + 8] = f32(g)[l].reshape(8, 128).T
        pv[:, b + 32:b + 44] = f32(conv_w)[l].reshape(3, 4, 128).transpose(2, 0, 1).reshape(128, 12)
        pv[:, b + 44:b + 48] = f32(ret_gn_w)[l].reshape(4, 128).T
        pv[:, b + 48:b + 52] = f32(gla_gn_w)[l].reshape(4, 128).T
    bA = np.ascontiguousarray(np.broadcast_to(f32(gla_b_a)[:_dd, None, :], (_dd, 128, 256)))
    shared = {
        "w_in": f32(w_in[:_dd]), "gla_w_a2": f32(gla_w_a2[:_dd]), "bA": bA,
        "w_branch_a": f32(w_branch_a[:_dd]), "w_branch_b": f32(w_branch_b[:_dd]), "w_branch_c": f32(w_branch_c[:_dd]),
        "w_out": f32(w_out[:_dd]), "w_ffn_gate": f32(w_ffn_gate[:_dd]), "w_ffn_up": f32(w_ffn_up[:_dd]),
        "w_ffn_down": f32(w_ffn_down[:_dd]), "pv": pv, "cst": _consts(),
    }
    key = bool(_debug)
    if key not in _CACHE:
        _CACHE[key] = build_program(debug=_debug)
    nc = _CACHE[key]
    in_maps = []
    for b in range(_ncores):
        m = dict(shared)
        m["x"] = x[b]
        m["pos"] = positions[b:b + 1]
        in_maps.append(m)
    res = run_bass_kernel_spmd(nc, in_maps, core_ids=list(range(_ncores)))
    out = np.stack([np.asarray(r["out"], dtype=np.float32) for r in res.results], axis=0)
    if _debug:
        kernel.last = res.results
    return out
```

```python
import math
import os
KSTOP = int(os.environ.get('KSTOP', '99'))
from contextlib import ExitStack

import numpy as np
import concourse.bass as bass
import concourse.mybir as mybir
from concourse.bass_utils import run_bass_kernel_spmd

F32 = mybir.dt.float32
BF16 = mybir.dt.bfloat16
I32 = mybir.dt.int32
ALU = mybir.AluOpType
AF = mybir.ActivationFunctionType
AX = mybir.AxisListType

D = 1024
T = 2048
DEPTH = 2
NTC = 4
KC = 8
IN_COLS = 8208
DFF = 2816
NF = 22
EPS = 1e-6
NSLOT = 5
SLOT = 4096
LP = 52

O_CU, O_CB, O_CC = 0, 512, 1024
O_RQ, O_RK, O_RV, O_RG = 1536, 2048, 2560, 3072
O_GQ, O_GK, O_GV, O_GR, O_GA = 3584, 3840, 4096, 4608, 5120
O_GATE = 5136

C_INVF, C_SIGN, C_KDEC, C_RMASK, C_QDEC, C_CAUS, C_UNEG, C_ID = 0, 1, 2, 6, 518, 1030, 1158, 1286
NCST = 1414


class Eng:
    def __init__(self, name, e, sem):
        self.name, self.e, self.sem = name, e, sem
        self.cnt = 0
        self.seen = {}


class Reg:
    __slots__ = ("w", "rs", "dsem", "dcnt", "excl")

    def __init__(self, excl=False):
        self.excl = excl
        self.w = None
        self.rs = {}
        self.dsem = None
        self.dcnt = 0


class FW:
    def __init__(self, nc, stack):
        self.nc = nc
        self.stack = stack
        self.E = {}
        for name, e in (("pe", nc.tensor), ("act", nc.scalar), ("dve", nc.vector),
                        ("pool", nc.gpsimd), ("sp", nc.sync)):
            sem = stack.enter_context(nc.semaphore("s_" + name))
            self.E[name] = Eng(name, e, sem)
        self.nsem = 5
        self.limit = None

    def sbuf(self, name, shape, dt):
        return self.stack.enter_context(self.nc.sbuf_tensor("sb_" + name, list(shape), dt))

    def psum(self, name, shape, dt):
        return self.stack.enter_context(self.nc.psum_tensor(name, list(shape), dt))

    def newsem(self):
        self.nsem += 1
        return self.stack.enter_context(self.nc.semaphore("d%d" % self.nsem))

    def _need(self, E, tok, need, same_ok):
        if tok is None:
            return
        if tok[0] == "e":
            F, n = tok[1], tok[2]
            if F is E and E.name in ("pe", "sp"):
                return
            key = F.name
            sem = F.sem
        else:
            _, sem, n, sid = tok
            key = ("d", sid)
        if E.seen.get(key, 0) >= n:
            return
        if need.get(key, (None, 0))[1] < n:
            need[key] = (sem, n)

    def _waits(self, E, reads, writes):
        need = {}
        for r in reads:
            self._need(E, r.w, need, False)
            if r.excl:
                for t in r.rs.values():
                    if t[0] == "e" and t[1] is E:
                        continue
                    self._need(E, t, need, True)
        for w in writes:
            self._need(E, w.w, need, False)
            for t in w.rs.values():
                self._need(E, t, need, True)
        for key, (sem, n) in need.items():
            E.e.wait_ge(sem, n)
            E.seen[key] = n

    def op(self, eng, fn, reads=(), writes=()):
        if self.limit is not None:
            if self.limit <= 0:
                return None
            self.limit -= 1
        E = self.E[eng]
        self._waits(E, reads, writes)
        inst = fn(E.e)
        E.cnt += 1
        inst.then_inc(E.sem, 1)
        tok = ("e", E, E.cnt)
        for r in reads:
            r.rs[E.name] = tok
        for w in writes:
            w.w = tok
            w.rs = {}
        return inst

    def dma(self, q, segs, reads=(), writes=(), owner=None):
        E = self.E[q]
        self._waits(E, reads, writes)
        owner = owner if owner is not None else (writes[0] if writes else reads[0])
        if owner.dsem is None:
            owner.dsem = self.newsem()
        for (o, i) in segs:
            inst = E.e.dma_start(out=o, in_=i)
            owner.dcnt += 16
            inst.then_inc(owner.dsem, 16)
        tok = ("d", owner.dsem, owner.dcnt, id(owner))
        for r in reads:
            r.rs[("d", id(owner))] = tok
        for w in writes:
            w.w = tok
            w.rs = {}

    def wait_all(self, eng, regs):
        self._waits(self.E[eng], [], regs)


def build_program(debug=False, depth=DEPTH, ntc=NTC, phases="ABCDEFG"):
    nc = bass.Bass("TRN2", target_bir_lowering=False)

    def din(name, shape, dt=F32):
        return nc.dram_tensor(name, list(shape), dt, kind="ExternalInput").ap()

    dd = max(depth, 1)
    x_d = din("x", [T, D])
    pos_d = din("pos", [1, T], I32)
    w_in_d = din("w_in", [dd, D, IN_COLS])
    wa2_d = din("gla_w_a2", [dd, 16, 256])
    bA_d = din("bA", [dd, 128, 256])
    wbr_d = [din("w_branch_" + n, [dd, 512, D]) for n in "abc"]
    wout_d = din("w_out", [dd, D, D])
    wg_d = din("w_ffn_gate", [dd, D, DFF])
    wu_d = din("w_ffn_up", [dd, D, DFF])
    wd_d = din("w_ffn_down", [dd, DFF, D])
    pv_d = din("pv", [128, dd * LP])
    cst_d = din("cst", [128, NCST])
    out_d = nc.dram_tensor("out", [T, D], F32, kind="ExternalOutput").ap()
    dbg_d = {}
    if debug:
        for n in ("ya", "yb", "yc"):
            dbg_d[n] = nc.dram_tensor("dbg_" + n, [4, 128, T], BF16, kind="ExternalOutput").ap()
        for n in ("xmix", "xffn"):
            dbg_d[n] = nc.dram_tensor("dbg_" + n, [8, 128, T], F32, kind="ExternalOutput").ap()

    st = ExitStack()
    with st:
        fw = FW(nc, st)
        op = fw.op

        x = fw.sbuf("x", [128, KC, T], F32)
        RX = [[Reg() for _ in range(NTC)] for _ in range(KC)]
        hT = fw.sbuf("hT", [128, KC, 512], BF16)
        RH = [Reg() for _ in range(KC)]
        ybuf = [fw.sbuf("y%d" % i, [128, 4, 512], BF16) for i in range(3)]
        RY = [[Reg() for _ in range(4)] for _ in range(3)]
        ACC = fw.sbuf("acc", [128, KC, 512], F32)
        RA = [Reg() for _ in range(KC)]
        mrg = fw.sbuf("mrg", [128, KC, 512], BF16)
        RM = [Reg() for _ in range(KC)]
        actb = fw.sbuf("actb", [128, 2, 2, 512], BF16)
        RACT = [[Reg() for _ in range(2)] for _ in range(2)]
        slots = [fw.sbuf("slot%d" % i, [128, SLOT], BF16) for i in range(NSLOT)]
        RS = [Reg() for _ in range(NSLOT)]
        P = [fw.psum("ps%d" % i, [128, 512], F32) for i in range(8)]
        RP = [Reg(excl=True) for _ in range(8)]

        def wt(name, shape=(128, 512), dt=F32):
            return fw.sbuf(name, shape, dt), Reg()

        T0, R0 = wt("T0"); T1, R1 = wt("T1"); T2, R2 = wt("T2"); T3, R3 = wt("T3")
        T4, R4 = wt("T4"); T5, R5 = wt("T5")
        sqb = [wt("sqb%d" % i, dt=BF16) for i in range(2)]
        qh, Rqh = wt("qh", dt=BF16); qt, Rqt = wt("qt", dt=BF16); kh, Rkh = wt("kh", dt=BF16)
        vtok, Rvt = wt("vtok", (128, 4, 256), BF16)
        ktok, Rkt = wt("ktok", (128, 4, 128), BF16)
        stm, Rstm = wt("stm", (128, 128), BF16)
        zb, Rzb = wt("zb", (128, 514), F32)
        zc, Rzc = wt("zc", (128, 4, 2), F32)
        spb, Rspb = wt("spb", (128, 4, 256), F32)
        gdb, Rgdb = wt("gdb", (128, 512), F32)
        gq = [wt("gq%d" % i, dt=BF16) for i in range(2)]
        nm, Rnm = wt("nm", (128, 4), F32)
        tS, RtS = wt("tS", (128, 128), F32)
        Sret, RSr = wt("Sret", (128, 4, 128), F32)
        Sretb, RSrb = wt("Sretb", (128, 4, 128), BF16)
        RSr = [Reg() for _ in range(4)]; RSrb = [Reg() for _ in range(4)]
        Sgla, RSg = wt("Sgla", (128, 2, 128), F32)
        Sglab, RSgb = wt("Sglab", (128, 2, 128), BF16)
        RSg = [Reg() for _ in range(2)]; RSgb = [Reg() for _ in range(2)]
        cosT, Rcos = wt("cosT"); sinT, Rsin = wt("sinT")
        posi, Rposi = wt("posi", (128, 512), I32)
        kint, Rkint = posi, Rposi
        cst, Rcst = wt("cst", (128, NCST), F32)
        pv, Rpv = wt("pv", (128, dd * LP), F32)
        bA, RbA = wt("bA", (128, dd, 256), F32)
        wa2, Rwa2 = wt("wa2", (128, dd, 256), F32)
        idb, Ridb = wt("idb", (128, 128), BF16)
        onesD, RoD = wt("onesD", (128, 128), BF16)
        onesH, RoH = wt("onesH", (128, 128), BF16)
        xin = [(ACC[:, 0:2, :].rearrange("p a b -> p (a b)"), [RA[0], RA[1]]),
               (ACC[:, 2:4, :].rearrange("p a b -> p (a b)"), [RA[2], RA[3]])]

        DBG = Reg()

        def tt(eng, out, in0, in1, o, reads, writes):
            op(eng, lambda e: e.tensor_tensor(out=out, in0=in0, in1=in1, op=o), reads, writes)

        def ts(eng, out, in0, s1, s2, o0, o1, reads, writes):
            if o1 is None:
                op(eng, lambda e: e.tensor_scalar(out=out, in0=in0, scalar1=s1, scalar2=None, op0=o0), reads, writes)
            else:
                op(eng, lambda e: e.tensor_scalar(out=out, in0=in0, scalar1=s1, scalar2=s2, op0=o0, op1=o1), reads, writes)

        def stt(out, in0, sc, in1, o0, o1, reads, writes):
            op("dve", lambda e: e.scalar_tensor_tensor(out=out, in0=in0, scalar=sc, in1=in1, op0=o0, op1=o1), reads, writes)

        def act(out, in_, func, reads, writes, bias=None, scale=None):
            kw = {}
            if bias is not None:
                kw["bias"] = bias
            if scale is not None:
                kw["scale"] = scale
            op("act", lambda e: e.activation(out=out, in_=in_, func=func, **kw), reads, writes)

        def acopy(out, in_, reads, writes):
            op("act", lambda e: e.copy(out=out, in_=in_), reads, writes)

        def mm(out, pairs, reads, writes):
            def f(e):
                inst = None
                n = len(pairs)
                for i, (l, r) in enumerate(pairs):
                    inst = e.matmul(out, lhsT=l, rhs=r, start=(i == 0), stop=(i == n - 1))
                return inst
            op("pe", f, reads, writes)

        def mm1(out, l, r, start, stop, reads, writes):
            op("pe", lambda e: e.matmul(out, lhsT=l, rhs=r, start=start, stop=stop), reads, writes)

        def tr(out, in_, ident, reads, writes):
            op("pe", lambda e: e.transpose(out=out, in_=in_, identity=ident), reads, writes)

        def pcol(l, off):
            return pv[:, l * LP + off: l * LP + off + 1]

        def tsl(tc):
            return slice(tc * 512, (tc + 1) * 512)

        units = []

        cur = {"l": 0, "n": 0}

        def unit(specs, fn):
            keys = []
            for _ in specs:
                keys.append((cur["l"], cur["n"]))
                cur["n"] += 1
            units.append((specs, fn, keys))

        def wseg(dram_ap_rows_by_cols, kc):
            return dram_ap_rows_by_cols.rearrange("(k p) n -> p k n", p=128)

        def setup(_):
            fw.dma("sp", [(cst[:], cst_d)], writes=[Rcst])
            fw.dma("sp", [(pv[:], pv_d)], writes=[Rpv])
            fw.dma("sp", [(bA[:], bA_d.rearrange("l p n -> p l n"))], writes=[RbA])
            op("dve", lambda e: e.memset(wa2[:], 0.0), [], [Rwa2])
            fw.dma("sp", [(wa2[0:16], wa2_d.rearrange("l r n -> r l n"))], writes=[Rwa2])
            for (g_, rg_) in gq:
                op("dve", lambda e: e.memset(g_[:], 0.0), [], [rg_])
            op("dve", lambda e: e.tensor_copy(out=idb[:], in_=cst[:, C_ID:C_ID + 128]), [Rcst], [Ridb])
            op("dve", lambda e: e.memset(onesD[:], 1.0 / D), [], [RoD])
            op("dve", lambda e: e.memset(onesH[:], 1.0 / 128), [], [RoH])

        unit([], setup)

        def load_x(tb):
            def f(_):
                xt_, rx_ = xin[tb % 2]
                fw.dma("sp", [(xt_, x_d[tb * 128:(tb + 1) * 128, :])], writes=rx_)
                tc = tb // 4
                for half in range(2):
                    pb = (tb * 2 + half) % 4
                    for j in range(4):
                        k = half * 4 + j
                        tr(P[pb][:, j * 128:(j + 1) * 128], xt_[:, k * 128:(k + 1) * 128], cst[:, C_ID:C_ID + 128],
                           rx_ + [Rcst], [RP[pb]])
                    dst = x[:, half * 4:half * 4 + 4, tb * 128:(tb + 1) * 128]
                    src = P[pb][:].rearrange("p (a b) -> p a b", a=4)
                    if half == 0:
                        op("act", lambda e: e.copy(out=dst, in_=src), [RP[pb]], [RX[k_][tc] for k_ in range(half * 4, half * 4 + 4)])
                    else:
                        op("dve", lambda e: e.tensor_copy(out=dst, in_=src), [RP[pb]], [RX[k_][tc] for k_ in range(half * 4, half * 4 + 4)])
            return f

        for tb in range(16):
            unit([], load_x(tb))

        def rstd_from(pb):
            act(T0[:], P[pb][:], AF.Ln, [RP[pb]], [R0], bias=EPS, scale=1.0)
            act(T1[:], T0[:], AF.Exp, [R0], [R1], scale=-0.5)

        def prenorm(l, tc, goff):
            def f(_):
                for k in range(KC):
                    sq, rsq = sqb[k % 2]
                    act(sq[:], x[:, k, tsl(tc)], AF.Square, [RX[k][tc]], [rsq])
                    mm1(P[7][:], onesD[:], sq[:], k == 0, k == KC - 1, [rsq, RoD], [RP[7]])
                rstd_from(7)
                for k in range(KC):
                    stt(hT[:, k, :], x[:, k, tsl(tc)], pcol(l, goff + k), T1[:], ALU.mult, ALU.mult,
                        [RX[k][tc], R1, Rpv], [RH[k]])
            return f

        def postnorm(l, tc, goff, dbgname=None):
            def f(_):
                for k in range(KC):
                    sq, rsq = sqb[k % 2]
                    act(sq[:], ACC[:, k, :], AF.Square, [RA[k]], [rsq])
                    mm1(P[7][:], onesD[:], sq[:], k == 0, k == KC - 1, [rsq, RoD], [RP[7]])
                rstd_from(7)
                for k in range(KC):
                    stt(T3[:], ACC[:, k, :], pcol(l, goff + k), T1[:], ALU.mult, ALU.mult, [RA[k], R1, Rpv], [R3])
                    tt("dve", x[:, k, tsl(tc)], x[:, k, tsl(tc)], T3[:], ALU.add, [R3, RX[k][tc]], [RX[k][tc]])
                    if debug and dbgname is not None and l == 0:
                        fw.dma("sp", [(dbg_d[dbgname][k, :, tsl(tc)], x[:, k, tsl(tc)])], reads=[RX[k][tc]])
            return f

        def proj(pb, wv, c0, reads_w, n=128):
            mm(P[pb][0:n, :], [(wv[:, k, c0:c0 + n], hT[:, k, :]) for k in range(KC)], [reads_w] + RH, [RP[pb]])

        def proj_tok(pb, ncol, wv, c0, reads_w, blk, o0):
            mm(P[pb][:, o0:o0 + ncol], [(hT[:, k, blk * 128:(blk + 1) * 128], wv[:, k, c0:c0 + ncol]) for k in range(KC)],
               [reads_w] + RH, [RP[pb]])

        def rope_tables(b_tc):
            tc = b_tc

            def f(_):
                fw.dma("sp", [(posi[:], pos_d[0:1, tsl(tc)].partition_broadcast(128))], writes=[Rposi])
                op("dve", lambda e: e.tensor_copy(out=T0[:], in_=posi[:]), [Rposi], [R0])
                ts("dve", T2[:], T0[:], cst[:, C_INVF:C_INVF + 1], None, ALU.mult, None, [R0, Rcst], [R2])
                ts("dve", kint[:], T2[:], 1.0 / (2 * math.pi), None, ALU.mult, None, [R2], [Rkint])
                op("dve", lambda e: e.tensor_copy(out=T0[:], in_=kint[:]), [Rkint], [R0])
                C1 = 6.28125
                C2 = 2 * math.pi - C1
                stt(T2[:], T0[:], -C1, T2[:], ALU.mult, ALU.add, [R0, R2], [R2])
                stt(T2[:], T0[:], -C2, T2[:], ALU.mult, ALU.add, [R0, R2], [R2])
                ts("dve", T2[:], T2[:], math.pi, -math.pi, ALU.min, ALU.max, [R2], [R2])
                act(T3[:], T2[:], AF.Sin, [R2], [R3])
                ts("dve", sinT[:], T3[:], cst[:, C_SIGN:C_SIGN + 1], None, ALU.mult, None, [R3, Rcst], [Rsin])
                act(T3[:], T2[:], AF.Sin, [R2], [R3], scale=0.5)
                tt("dve", T3[:], T3[:], T3[:], ALU.mult, [R3], [R3])
                ts("dve", cosT[:], T3[:], -2.0, 1.0, ALU.mult, ALU.add, [R3], [Rcos])
            return f

        def reset_states(_):
            op("dve", lambda e: e.memset(zc[:], 0.0), [], [Rzc])
            op("dve", lambda e: e.memset(Sret[:], 0.0), [], RSr)
            op("dve", lambda e: e.memset(Sretb[:], 0.0), [], RSrb)
            op("dve", lambda e: e.memset(Sgla[:], 0.0), [], RSg)
            op("dve", lambda e: e.memset(Sglab[:], 0.0), [], RSgb)

        def conv_unit(l, tc, c):
            win = w_in_d[l]
            specs = [[(0, 8, 128, wseg(win[:, O_CU + c * 128:O_CU + (c + 1) * 128], 8)),
                      (1024, 8, 128, wseg(win[:, O_CB + c * 128:O_CB + (c + 1) * 128], 8)),
                      (2048, 8, 128, wseg(win[:, O_CC + c * 128:O_CC + (c + 1) * 128], 8))]]

            def f(pc):
                (sl, rs) = pc[0]
                wv = sl[:, 0:3072].rearrange("p (s k n) -> p s k n", s=3, k=8)
                proj(0, wv[:, 0], 0, rs)
                proj(1, wv[:, 1], 0, rs)
                proj(2, wv[:, 2], 0, rs)
                acopy(T0[:], P[0][:], [RP[0]], [R0])
                acopy(zb[:, 0:2], zc[:, c, :], [Rzc], [Rzb])
                tt("dve", zb[:, 2:514], P[2][:], T0[:], ALU.mult, [RP[2], R0], [Rzb])
                acopy(zc[:, c, :], zb[:, 512:514], [Rzb], [Rzc])
                ts("dve", T2[:], zb[:, 2:514], pcol(l, 32 + 8 + c), None, ALU.mult, None, [Rzb, Rpv], [R2])
                stt(T2[:], zb[:, 1:513], pcol(l, 32 + 4 + c), T2[:], ALU.mult, ALU.add, [Rzb, Rpv, R2], [R2])
                stt(T2[:], zb[:, 0:512], pcol(l, 32 + 0 + c), T2[:], ALU.mult, ALU.add, [Rzb, Rpv, R2], [R2])
                tt("dve", ybuf[0][:, c, :], P[1][:], T2[:], ALU.mult, [RP[1], R2], [RY[0][c]])
                if debug and l == 0:
                    fw.dma("sp", [(dbg_d["ya"][c, :, tsl(tc)], ybuf[0][:, c, :])], reads=[RY[0][c]])
            unit(specs, f)

        def rope(pb):
            tt("dve", T0[:], P[pb][:], cosT[:], ALU.mult, [RP[pb], Rcos], [R0])
            acopy(T5[:], P[pb][:], [RP[pb]], [R5])
            acopy(T2[0:64, :], T5[64:128, :], [R5], [R2])
            acopy(T2[64:128, :], T5[0:64, :], [R5], [R2])
            tt("dve", T2[:], T2[:], sinT[:], ALU.mult, [R2, Rsin], [R2])
            tt("dve", T3[:], T0[:], T2[:], ALU.add, [R0, R2], [R3])

        def headnorm(pbo, pbm, sg_t, sg_r, gcol, out_ap, out_reg):
            sq, rsq = sqb[0]
            act(sq[:], P[pbo][:], AF.Square, [RP[pbo]], [rsq])
            mm1(P[pbm][:], onesH[:], sq[:], True, True, [rsq, RoH], [RP[pbm]])
            rstd_from(pbm)
            tt("dve", T3[:], P[pbo][:], T1[:], ALU.mult, [RP[pbo], R1], [R3])
            stt(out_ap, T3[:], gcol, sg_t[:], ALU.mult, ALU.mult, [R3, Rpv, sg_r], [out_reg])

        GAM = [1.0 - 2.0 ** (-5.0 - h) for h in range(4)]

        def ret_unit(l, tc, h):
            win = w_in_d[l]
            specs = [[(i * 1024, 8, 128, wseg(win[:, o + h * 128:o + (h + 1) * 128], 8))
                      for i, o in enumerate((O_RQ, O_RK, O_RV, O_RG))]]

            def f(pc):
                (sl, rs) = pc[0]
                wv = sl[:, 0:4096].rearrange("p (s k n) -> p s k n", s=4, k=8)
                proj(0, wv[:, 0], 0, rs)
                proj(1, wv[:, 1], 0, rs)
                proj(2, wv[:, 3], 0, rs)
                for blk in range(4):
                    proj_tok(3, 128, wv[:, 2], 0, rs, blk, blk * 128)
                if KSTOP <= 1: return
                if 'KOPS' in os.environ: fw.limit = int(os.environ['KOPS'])
                rope(0)
                op("dve", lambda e: e.tensor_copy(out=qh[:], in_=T3[:]), [R3], [Rqh])
                tt("dve", qt[:].rearrange("p (a b) -> p a b", a=4), T3[:].rearrange("p (a b) -> p a b", a=4),
                   cst[:, C_QDEC + h * 128:C_QDEC + (h + 1) * 128].unsqueeze(1).to_broadcast([128, 4, 128]),
                   ALU.mult, [R3, Rcst], [Rqt])
                if KSTOP <= 2:
                    fw.limit = None
                    return
                rope(1)
                op("dve", lambda e: e.tensor_copy(out=kh[:], in_=T3[:]), [R3], [Rkh])
                if KSTOP <= 3: return
                act(T4[:], P[2][:], AF.Silu, [RP[2]], [R4])
                if KSTOP <= 4: return
                pv3 = P[3][:].rearrange("p (a b) -> p a b", a=4)
                op("dve", lambda e: e.tensor_reduce(out=nm[:], in_=pv3, axis=AX.X, op=ALU.add), [RP[3]], [Rnm])
                ts("dve", nm[:], nm[:], -1.0 / 128, None, ALU.mult, None, [Rnm], [Rnm])
                for blk in range(4):
                    ts("dve", vtok[:, blk, 0:128], P[3][:, blk * 128:(blk + 1) * 128], nm[:, blk:blk + 1], None,
                       ALU.add, None, [RP[3], Rnm], [Rvt])
                if KSTOP <= 5: return
                p4b = P[4][:].bitcast(BF16)
                for blk in range(4):
                    tr(p4b[:, blk * 128:(blk + 1) * 128], kh[:, blk * 128:(blk + 1) * 128], idb[:], [Rkh, Ridb], [RP[4]])
                ts("dve", ktok[:].rearrange("p a b -> p (a b)"), p4b[:, 0:512], cst[:, C_KDEC + h:C_KDEC + h + 1], None,
                   ALU.mult, None, [RP[4], Rcst], [Rkt])
                if KSTOP <= 6: return
                g128 = GAM[h] ** 128
                for blk in range(4):
                    bs = slice(blk * 128, (blk + 1) * 128)
                    mm(P[5][:, 0:128], [(kh[:, bs], qh[:, bs])], [Rkh, Rqh], [RP[5]])
                    tt("dve", stm[:], P[5][:, 0:128], cst[:, C_RMASK + h * 128:C_RMASK + (h + 1) * 128], ALU.mult,
                       [RP[5], Rcst], [Rstm])
                    mm(P[6][:, bs], [(vtok[:, blk, 0:128], stm[:]), (Sretb[:, h, :], qt[:, bs])],
                       [Rvt, Rstm, RSrb[h], Rqt], [RP[6]])
                    mm(P[7][:, 0:128], [(ktok[:, blk, :], vtok[:, blk, 0:128])], [Rkt, Rvt], [RP[7]])
                    stt(Sret[:, h, :], Sret[:, h, :], g128, P[7][:, 0:128], ALU.mult, ALU.add, [RSr[h], RP[7]], [RSr[h]])
                    acopy(Sretb[:, h, :], Sret[:, h, :], [RSr[h]], [RSrb[h]])
                if KSTOP <= 7: return
                headnorm(6, 7, T4, R4, pcol(l, 44 + h), ybuf[1][:, h, :], RY[1][h])
                if debug and l == 0:
                    fw.dma("sp", [(dbg_d["yb"][h, :, tsl(tc)], ybuf[1][:, h, :])], reads=[RY[1][h]])
            unit(specs, f)

        def gla_prelude(l, tc):
            win = w_in_d[l]
            specs = [[(0, 8, 128, wseg(win[:, O_GA:O_GA + 128], 8))]]

            def f(pc):
                (sl, rs) = pc[0]
                wv = sl[:, 0:1024].rearrange("p (k n) -> p k n", k=8)
                mm(P[0][:, :], [(wv[:, k, :], hT[:, k, :]) for k in range(KC)], [rs] + RH, [RP[0]])
                acopy(gdb[:], P[0][:, :], [RP[0]], [Rgdb])
                for half in range(2):
                    pb = 1 + half
                    for j in range(2):
                        blk = half * 2 + j
                        mm(P[pb][:, j * 256:(j + 1) * 256], [(gdb[:, blk * 128:(blk + 1) * 128], wa2[:, l, :])],
                           [Rgdb, Rwa2], [RP[pb]])
                    sv = spb[:, half * 2:half * 2 + 2, :]
                    tt("dve", sv, P[pb][:].rearrange("p (a b) -> p a b", a=2),
                       bA[:, l, :].unsqueeze(1).to_broadcast([128, 2, 256]), ALU.add, [RP[pb], RbA], [Rspb])
                    act(sv, sv, AF.Exp, [Rspb], [Rspb], scale=-1.0)
                    act(sv, sv, AF.Ln, [Rspb], [Rspb], bias=1.0, scale=1.0)
                for hp in range(2):
                    for blk in range(4):
                        mm(P[3 + hp][:, blk * 128:(blk + 1) * 128],
                           [(spb[:, blk, hp * 128:(hp + 1) * 128], cst[:, C_UNEG:C_UNEG + 128])], [Rspb, Rcst], [RP[3 + hp]])
            unit(specs, f)

        def gla_unit(l, tc, hp):
            win = w_in_d[l]
            specs = [[(0, 8, 128, wseg(win[:, O_GQ + hp * 128:O_GQ + (hp + 1) * 128], 8)),
                      (1024, 8, 128, wseg(win[:, O_GK + hp * 128:O_GK + (hp + 1) * 128], 8)),
                      (2048, 8, 256, wseg(win[:, O_GV + hp * 256:O_GV + (hp + 1) * 256], 8))],
                     [(0, 8, 256, wseg(win[:, O_GR + hp * 256:O_GR + (hp + 1) * 256], 8))]]

            def f(pc):
                (sl, rs), (sl2, rs2) = pc
                wq = sl[:, 0:1024].rearrange("p (k n) -> p k n", k=8)
                wk = sl[:, 1024:2048].rearrange("p (k n) -> p k n", k=8)
                wvv = sl[:, 2048:4096].rearrange("p (k n) -> p k n", k=8)
                wr = sl2[:, 0:2048].rearrange("p (k n) -> p k n", k=8)
                pc_ = 3 + hp
                act(T0[:], P[pc_][:], AF.Exp, [RP[pc_]], [R0])
                act(T2[:], P[pc_][:], AF.Exp, [RP[pc_]], [R2], scale=-1.0)
                proj(0, wq, 0, rs)
                proj(1, wk, 0, rs)
                for e_ in range(2):
                    ps = slice(64 * e_, 64 * e_ + 64)
                    stt(gq[e_][0][ps, :], P[0][ps, :], 0.125, T0[ps, :], ALU.mult, ALU.mult, [RP[0], R0], [gq[e_][1]])
                tt("dve", kh[:], P[1][:], T2[:], ALU.mult, [RP[1], R2], [Rkh])
                for half, pb in ((0, 2), (1, 5)):
                    for j in range(2):
                        proj_tok(pb, 256, wvv, 0, rs, half * 2 + j, j * 256)
                    acopy(vtok[:, half * 2:half * 2 + 2, :], P[pb][:].rearrange("p (a b) -> p a b", a=2), [RP[pb]], [Rvt])
                p6b = P[6][:].bitcast(BF16)
                for blk in range(4):
                    tr(p6b[:, blk * 128:(blk + 1) * 128], kh[:, blk * 128:(blk + 1) * 128], idb[:], [Rkh, Ridb], [RP[6]])
                acopy(ktok[:].rearrange("p a b -> p (a b)"), p6b[:, 0:512], [RP[6]], [Rkt])
                proj(7, wr, 0, rs2)
                act(T4[:], P[7][:], AF.Silu, [RP[7]], [R4])
                proj(5, wr, 128, rs2)
                act(T5[:], P[5][:], AF.Silu, [RP[5]], [R5])
                for blk in range(4):
                    bs = slice(blk * 128, (blk + 1) * 128)
                    for e_ in range(2):
                        gq_, rgq_ = gq[e_]
                        mm(P[0][:, 0:128], [(kh[:, bs], gq_[:, bs])], [Rkh, rgq_], [RP[0]])
                        tt("dve", stm[:], P[0][:, 0:128], cst[:, C_CAUS:C_CAUS + 128], ALU.mult, [RP[0], Rcst], [Rstm])
                        mm(P[1 + e_][:, bs], [(vtok[:, blk, e_ * 128:(e_ + 1) * 128], stm[:]), (Sglab[:, hp, :], gq_[:, bs])],
                           [Rvt, Rstm, RSgb[hp], rgq_], [RP[1 + e_]])
                    for e_ in range(2):
                        mm(P[6][:, e_ * 128:(e_ + 1) * 128], [(ktok[:, blk, :], vtok[:, blk, e_ * 128:(e_ + 1) * 128])],
                           [Rkt, Rvt], [RP[6]])
                    for e_ in range(2):
                        ps = slice(64 * e_, 64 * e_ + 64)
                        tt("dve", tS[ps, :], P[6][ps, e_ * 128:(e_ + 1) * 128], Sgla[ps, hp, :], ALU.add, [RP[6], RSg[hp]], [RtS])
                    ts("dve", Sgla[:, hp, :], tS[:], T0[:, blk * 128 + 127:blk * 128 + 128], None, ALU.mult, None,
                       [RtS, R0], [RSg[hp]])
                    acopy(Sglab[:, hp, :], Sgla[:, hp, :], [RSg[hp]], [RSgb[hp]])
                for e_, (sg_t, sg_r) in enumerate(((T4, R4), (T5, R5))):
                    hh = 2 * hp + e_
                    headnorm(1 + e_, 7, sg_t, sg_r, pcol(l, 48 + hh), ybuf[2][:, hh, :], RY[2][hh])
                    if debug and l == 0:
                        fw.dma("sp", [(dbg_d["yc"][hh, :, tsl(tc)], ybuf[2][:, hh, :])], reads=[RY[2][hh]])
            unit(specs, f)

        def merge_unit(l, tc, i):
            win = w_in_d[l]
            c0 = O_GATE + i * 1024
            specs = [[(0, 8, 512, wseg(win[:, c0:c0 + 512], 8))],
                     [(0, 8, 512, wseg(win[:, c0 + 512:c0 + 1024], 8))],
                     [(0, 4, 1024, wseg(wbr_d[i][l], 4))]]

            def f(pc):
                (slb, rsb) = pc[2]
                wb_ = slb[:, 0:4096].rearrange("p (k n) -> p k n", k=4)
                for half in range(2):
                    (sl, rs) = pc[half]
                    wg_ = sl[:, 0:4096].rearrange("p (k n) -> p k n", k=8)
                    for j in range(4):
                        oc = half * 4 + j
                        pa, pb = (0, 1) if j % 2 == 0 else (2, 3)
                        proj(pa, wg_, j * 128, rs)
                        mm(P[pb][:], [(wb_[:, kk, oc * 128:(oc + 1) * 128], ybuf[i][:, kk, :]) for kk in range(4)],
                           [rsb] + RY[i], [RP[pb]])
                        tw, rw = (T0, R0) if j % 2 == 0 else (T2, R2)
                        act(tw[:], P[pa][:], AF.Tanh, [RP[pa]], [rw], scale=0.5)
                        if i == 0:
                            stt(ACC[:, oc, :], tw[:], 1.0, P[pb][:], ALU.add, ALU.mult, [rw, RP[pb]], [RA[oc]])
                        else:
                            stt(tw[:], tw[:], 1.0, P[pb][:], ALU.add, ALU.mult, [rw, RP[pb]], [rw])
                            tt("dve", ACC[:, oc, :], ACC[:, oc, :], tw[:], ALU.add, [RA[oc], rw], [RA[oc]])
                            if i == 2:
                                op("act", lambda e: e.mul(out=mrg[:, oc, :], in_=ACC[:, oc, :], mul=0.5), [RA[oc]], [RM[oc]])
            unit(specs, f)

        def out_unit(l, tc, half):
            specs = [[(0, 8, 512, wseg(wout_d[l][:, half * 512:(half + 1) * 512], 8))]]

            def f(pc):
                (sl, rs) = pc[0]
                wo = sl[:, 0:4096].rearrange("p (k n) -> p k n", k=8)
                for j in range(4):
                    oc = half * 4 + j
                    pa = j % 4
                    mm(P[pa][:], [(wo[:, k, j * 128:(j + 1) * 128], mrg[:, k, :]) for k in range(KC)], [rs] + RM, [RP[pa]])
                    acopy(ACC[:, oc, :], P[pa][:], [RP[pa]], [RA[oc]])
            unit(specs, f)

        def ffn_unit(l, tc, g):
            c0 = g * 256
            specs = [[(0, 8, 256, wseg(wg_d[l][:, c0:c0 + 256], 8)),
                      (2048, 8, 256, wseg(wu_d[l][:, c0:c0 + 256], 8))],
                     [(0, 2, 1024, wseg(wd_d[l][c0:c0 + 256, :], 2))]]

            def f(pc):
                (sl, rs), (sl2, rs2) = pc
                wg_ = sl[:, 0:2048].rearrange("p (k n) -> p k n", k=8)
                wu_ = sl[:, 2048:4096].rearrange("p (k n) -> p k n", k=8)
                wd_ = sl2[:, 0:2048].rearrange("p (k n) -> p k n", k=2)
                par = g % 2
                for j in range(2):
                    pa, pb = (0, 1) if j == 0 else (2, 3)
                    proj(pa, wg_, j * 128, rs)
                    proj(pb, wu_, j * 128, rs)
                    tw, rw = (T0, R0) if j == 0 else (T2, R2)
                    act(tw[:], P[pa][:], AF.Silu, [RP[pa]], [rw])
                    tt("dve", actb[:, par, j, :], tw[:], P[pb][:], ALU.mult, [rw, RP[pb]], [RACT[par][j]])
                for oc in range(KC):
                    pa = 4 + oc % 3
                    mm(P[pa][:], [(wd_[:, j, oc * 128:(oc + 1) * 128], actb[:, par, j, :]) for j in range(2)],
                       [rs2] + RACT[par], [RP[pa]])
                    if g == 0:
                        acopy(ACC[:, oc, :], P[pa][:], [RP[pa]], [RA[oc]])
                    else:
                        tt("dve", ACC[:, oc, :], ACC[:, oc, :], P[pa][:], ALU.add, [RA[oc], RP[pa]], [RA[oc]])
            unit(specs, f)

        ROUT = [Reg(), Reg()]

        def store_x(tb):
            def f(_):
                tc = tb // 4
                ot, ro = xin[tb % 2]
                for half in range(2):
                    pb = (tb * 2 + half) % 4
                    for j in range(4):
                        k = half * 4 + j
                        tr(P[pb][:, j * 128:(j + 1) * 128], x[:, k, tb * 128:(tb + 1) * 128], cst[:, C_ID:C_ID + 128],
                           [RX[k][tc], Rcst], [RP[pb]])
                    if half == 0:
                        acopy(ot[:, 0:512], P[pb][:], [RP[pb]], ro)
                    else:
                        op("dve", lambda e: e.tensor_copy(out=ot[:, 512:1024], in_=P[pb][:]), [RP[pb]], ro)
                fw.dma("sp", [(out_d[tb * 128:(tb + 1) * 128, :], ot)], reads=ro, owner=ROUT[tb % 2])
            return f

        for l in range(depth):
            unit([], reset_states)
            for tc in range(ntc):
                cur["l"] = l
                cur["n"] = 0
                unit([], prenorm(l, tc, 0))
                if "A" in phases:
                    for c in range(4):
                        conv_unit(l, tc, c)
                if "B" in phases:
                    if os.environ.get('KROPE', '1') == '1':
                        unit([], rope_tables(tc))
                    for h in range(4):
                        ret_unit(l, tc, h)
                if "C" in phases:
                    gla_prelude(l, tc)
                    for hp in range(2):
                        gla_unit(l, tc, hp)
                if "D" in phases:
                    for i in range(3):
                        merge_unit(l, tc, i)
                if "E" in phases:
                    for half in range(2):
                        out_unit(l, tc, half)
                    unit([], postnorm(l, tc, 8, "xmix"))
                if "F" in phases:
                    unit([], prenorm(l, tc, 16))
                    for g in range(NF // 2):
                        ffn_unit(l, tc, g)
                    unit([], postnorm(l, tc, 24, "xffn"))
        for tb in range(16):
            unit([], store_x(tb))

        pieces = []
        upieces = []
        for ui, (specs, fn, keys) in enumerate(units):
            idxs = []
            for segs, key in zip(specs, keys):
                idxs.append(len(pieces))
                pieces.append((ui, segs, key))
            upieces.append(idxs)
        GSZ = 6
        first = {}
        for (ui, segs, key) in pieces:
            first.setdefault(key, segs)
        npc = max([k[1] for k in first] + [0]) + 1
        scr = [nc.dram_tensor("scr%d" % l, [npc, 128, SLOT], BF16).ap() for l in range(max(depth, 1))]
        ngrp = (npc + GSZ - 1) // GSZ
        RG = [[Reg() for _ in range(ngrp)] for _ in range(max(depth, 1))]
        for l in range(depth):
            for g in range(ngrp):
                csegs = []
                for j in range(g * GSZ, min(npc, (g + 1) * GSZ)):
                    if (l, j) not in first:
                        continue
                    for (off, kc, n, src) in first[(l, j)]:
                        dst = scr[l][j][:, off:off + kc * n].rearrange("p (k n) -> p k n", k=kc)
                        csegs.append((dst, src))
                if csegs:
                    fw.dma("pool", csegs, writes=[RG[l][g]])
        nextp = 0

        def issue_loads(cur_unit):
            nonlocal nextp
            while nextp < len(pieces):
                if nextp >= NSLOT and pieces[nextp - NSLOT][0] >= cur_unit:
                    break
                ui, segs, (l_, j_) = pieces[nextp]
                s_ = nextp % NSLOT
                L = max(off + kc * n for (off, kc, n, src) in segs)
                fw.dma("sp", [(slots[s_][:, 0:L], scr[l_][j_][:, 0:L])], reads=[RG[l_][j_ // GSZ]], writes=[RS[s_]])
                nextp += 1

        for ui, (specs, fn, keys) in enumerate(units):
            issue_loads(ui)
            for pi_ in upieces[ui]:
                assert pi_ < nextp, "piece not loaded: raise NSLOT"
            fn([(slots[pi_ % NSLOT], RS[pi_ % NSLOT]) for pi_ in upieces[ui]])

        fw.wait_all("sp", xin[0][1] + xin[1][1])
        if debug:
            fw.wait_all("sp", [r for rr in RY for r in rr] + [r for rr in RX for r in rr])
    return nc


def _consts():
    c = np.zeros((128, NCST), np.float64)
    p = np.arange(128)
    half = 64
    invf = 10000.0 ** (-(np.arange(half, dtype=np.float32) / np.float32(half)))
    c[:, C_INVF] = np.concatenate([invf, invf]).astype(np.float32)
    c[:, C_SIGN] = np.where(p < 64, -1.0, 1.0)
    i = np.arange(128)
    for h in range(4):
        lg = math.log(1.0 - 2.0 ** (-5.0 - h))
        c[:, C_KDEC + h] = np.exp(lg * (127 - p))
        diff = i[None, :] - p[:, None]
        c[:, C_RMASK + h * 128:C_RMASK + (h + 1) * 128] = np.where(diff >= 0, np.exp(lg * np.maximum(diff, 0)), 0.0) * 128 ** -0.5
        c[:, C_QDEC + h * 128:C_QDEC + (h + 1) * 128] = (np.exp(lg * (i + 1.0)) * 128 ** -0.5)[None, :]
    c[:, C_CAUS:C_CAUS + 128] = (i[None, :] >= p[:, None]).astype(np.float64)
    c[:, C_UNEG:C_UNEG + 128] = np.where(p[:, None] <= i[None, :], -1.0 / 16.0, 0.0)
    c[:, C_ID:C_ID + 128] = np.eye(128)
    return c.astype(np.float32)


_CACHE = {}


def kernel(x, positions, norm_mix_pre, w_in, conv_w, ret_gn_w, gla_w_a2, gla_b_a, gla_gn_w,
           w_branch_a, w_branch_b, w_branch_c, w_out, norm_mix_post, norm_ffn_pre,
           w_ffn_gate, w_ffn_up, w_ffn_down, norm_ffn_post, _debug=False, _dd=DEPTH, _ncores=8):
    f32 = lambda a: np.ascontiguousarray(np.asarray(a, dtype=np.float32))
    x = f32(x)
    positions = np.ascontiguousarray(np.asarray(positions, dtype=np.int32))
    pv = np.zeros((128, _dd * LP), np.float32)
    for l in range(_dd):
        b = l * LP
        for j, g in enumerate((norm_mix_pre, norm_mix_post, norm_ffn_pre, norm_ffn_post)):
            pv[:, b + 8 * j:b + 8 * j + 8] = f32(g)[l].reshape(8, 128).T
        pv[:, b + 32:b + 44] = f32(conv_w)[l].reshape(3, 4, 128).transpose(2, 0, 1).reshape(128, 12)
        pv[:, b + 44:b + 48] = f32(ret_gn_w)[l].reshape(4, 128).T
        pv[:, b + 48:b + 52] = f32(gla_gn_w)[l].reshape(4, 128).T
    bA = np.ascontiguousarray(np.broadcast_to(f32(gla_b_a)[:_dd, None, :], (_dd, 128, 256)))
    shared = {
        "w_in": f32(w_in[:_dd]), "gla_w_a2": f32(gla_w_a2[:_dd]), "bA": bA,
        "w_branch_a": f32(w_branch_a[:_dd]), "w_branch_b": f32(w_branch_b[:_dd]), "w_branch_c": f32(w_branch_c[:_dd]),
        "w_out": f32(w_out[:_dd]), "w_ffn_gate": f32(w_ffn_gate[:_dd]), "w_ffn_up": f32(w_ffn_up[:_dd]),
        "w_ffn_down": f32(w_ffn_down[:_dd]), "pv": pv, "cst": _consts(),
    }
    key = bool(_debug)
    if key not in _CACHE:
        _CACHE[key] = build_program(debug=_debug)
    nc = _CACHE[key]
    in_maps = []
    for b in range(_ncores):
        m = dict(shared)
        m["x"] = x[b]
        m["pos"] = positions[b:b + 1]
        in_maps.append(m)
    res = run_bass_kernel_spmd(nc, in_maps, core_ids=list(range(_ncores)))
    out = np.stack([np.asarray(r["out"], dtype=np.float32) for r in res.results], axis=0)
    if _debug:
        kernel.last = res.results
    return out
```

```python
import math
import os
KSTOP = int(os.environ.get('KSTOP', '99'))
from contextlib import ExitStack

import numpy as np
import concourse.bass as bass
import concourse.mybir as mybir
from concourse.bass_utils import run_bass_kernel_spmd

F32 = mybir.dt.float32
BF16 = mybir.dt.bfloat16
I32 = mybir.dt.int32
ALU = mybir.AluOpType
AF = mybir.ActivationFunctionType
AX = mybir.AxisListType

D = 1024
T = 2048
DEPTH = 2
NTC = 4
KC = 8
IN_COLS = 8208
DFF = 2816
NF = 22
EPS = 1e-6
NSLOT = 5
SLOT = 4096
LP = 52

O_CU, O_CB, O_CC = 0, 512, 1024
O_RQ, O_RK, O_RV, O_RG = 1536, 2048, 2560, 3072
O_GQ, O_GK, O_GV, O_GR, O_GA = 3584, 3840, 4096, 4608, 5120
O_GATE = 5136

C_INVF, C_SIGN, C_KDEC, C_RMASK, C_QDEC, C_CAUS, C_UNEG, C_ID = 0, 1, 2, 6, 518, 1030, 1158, 1286
NCST = 1414


class Eng:
    def __init__(self, name, e, sem):
        self.name, self.e, self.sem = name, e, sem
        self.cnt = 0
        self.seen = {}


class Reg:
    __slots__ = ("w", "rs", "dsem", "dcnt", "excl")

    def __init__(self, excl=False):
        self.excl = excl
        self.w = None
        self.rs = {}
        self.dsem = None
        self.dcnt = 0


class FW:
    def __init__(self, nc, stack):
        self.nc = nc
        self.stack = stack
        self.E = {}
        for name, e in (("pe", nc.tensor), ("act", nc.scalar), ("dve", nc.vector),
                        ("pool", nc.gpsimd), ("sp", nc.sync)):
            sem = stack.enter_context(nc.semaphore("s_" + name))
            self.E[name] = Eng(name, e, sem)
        self.nsem = 5
        self.limit = None

    def sbuf(self, name, shape, dt):
        return self.stack.enter_context(self.nc.sbuf_tensor("sb_" + name, list(shape), dt))

    def psum(self, name, shape, dt):
        return self.stack.enter_context(self.nc.psum_tensor(name, list(shape), dt))

    def newsem(self):
        self.nsem += 1
        return self.stack.enter_context(self.nc.semaphore("d%d" % self.nsem))

    def _need(self, E, tok, need, same_ok):
        if tok is None:
            return
        if tok[0] == "e":
            F, n = tok[1], tok[2]
            if F is E and E.name in ("pe", "sp"):
                return
            key = F.name
            sem = F.sem
        else:
            _, sem, n, sid = tok
            key = ("d", sid)
        if E.seen.get(key, 0) >= n:
            return
        if need.get(key, (None, 0))[1] < n:
            need[key] = (sem, n)

    def _waits(self, E, reads, writes):
        need = {}
        for r in reads:
            self._need(E, r.w, need, False)
            if r.excl:
                for t in r.rs.values():
                    if t[0] == "e" and t[1] is E:
                        continue
                    self._need(E, t, need, True)
        for w in writes:
            self._need(E, w.w, need, False)
            for t in w.rs.values():
                self._need(E, t, need, True)
        for key, (sem, n) in need.items():
            E.e.wait_ge(sem, n)
            E.seen[key] = n

    def op(self, eng, fn, reads=(), writes=()):
        if self.limit is not None:
            if self.limit <= 0:
                return None
            self.limit -= 1
        E = self.E[eng]
        self._waits(E, reads, writes)
        inst = fn(E.e)
        E.cnt += 1
        inst.then_inc(E.sem, 1)
        tok = ("e", E, E.cnt)
        for r in reads:
            r.rs[E.name] = tok
        for w in writes:
            w.w = tok
            w.rs = {}
        return inst

    def dma(self, q, segs, reads=(), writes=(), owner=None):
        E = self.E[q]
        self._waits(E, reads, writes)
        owner = owner if owner is not None else (writes[0] if writes else reads[0])
        if owner.dsem is None:
            owner.dsem = self.newsem()
        for (o, i) in segs:
            inst = E.e.dma_start(out=o, in_=i)
            owner.dcnt += 16
            inst.then_inc(owner.dsem, 16)
        tok = ("d", owner.dsem, owner.dcnt, id(owner))
        for r in reads:
            r.rs[("d", id(owner))] = tok
        for w in writes:
            w.w = tok
            w.rs = {}

    def wait_all(self, eng, regs):
        self._waits(self.E[eng], [], regs)


def build_program(debug=False, depth=DEPTH, ntc=NTC, phases="ABCDEFG"):
    nc = bass.Bass("TRN2", target_bir_lowering=False)

    def din(name, shape, dt=F32):
        return nc.dram_tensor(name, list(shape), dt, kind="ExternalInput").ap()

    dd = max(depth, 1)
    x_d = din("x", [T, D])
    pos_d = din("pos", [1, T], I32)
    w_in_d = din("w_in", [dd, D, IN_COLS])
    wa2_d = din("gla_w_a2", [dd, 16, 256])
    bA_d = din("bA", [dd, 128, 256])
    wbr_d = [din("w_branch_" + n, [dd, 512, D]) for n in "abc"]
    wout_d = din("w_out", [dd, D, D])
    wg_d = din("w_ffn_gate", [dd, D, DFF])
    wu_d = din("w_ffn_up", [dd, D, DFF])
    wd_d = din("w_ffn_down", [dd, DFF, D])
    pv_d = din("pv", [128, dd * LP])
    cst_d = din("cst", [128, NCST])
    out_d = nc.dram_tensor("out", [T, D], F32, kind="ExternalOutput").ap()
    dbg_d = {}
    if debug:
        for n in ("ya", "yb", "yc"):
            dbg_d[n] = nc.dram_tensor("dbg_" + n, [4, 128, T], BF16, kind="ExternalOutput").ap()
        for n in ("xmix", "xffn"):
            dbg_d[n] = nc.dram_tensor("dbg_" + n, [8, 128, T], F32, kind="ExternalOutput").ap()

    st = ExitStack()
    with st:
        fw = FW(nc, st)
        op = fw.op

        x = fw.sbuf("x", [128, KC, T], F32)
        RX = [[Reg() for _ in range(NTC)] for _ in range(KC)]
        hT = fw.sbuf("hT", [128, KC, 512], BF16)
        RH = [Reg() for _ in range(KC)]
        ybuf = [fw.sbuf("y%d" % i, [128, 4, 512], BF16) for i in range(3)]
        RY = [[Reg() for _ in range(4)] for _ in range(3)]
        ACC = fw.sbuf("acc", [128, KC, 512], F32)
        RA = [Reg() for _ in range(KC)]
        mrg = fw.sbuf("mrg", [128, KC, 512], BF16)
        RM = [Reg() for _ in range(KC)]
        actb = fw.sbuf("actb", [128, 2, 2, 512], BF16)
        RACT = [[Reg() for _ in range(2)] for _ in range(2)]
        slots = [fw.sbuf("slot%d" % i, [128, SLOT], BF16) for i in range(NSLOT)]
        RS = [Reg() for _ in range(NSLOT)]
        P = [fw.psum("ps%d" % i, [128, 512], F32) for i in range(8)]
        RP = [Reg(excl=True) for _ in range(8)]

        def wt(name, shape=(128, 512), dt=F32):
            return fw.sbuf(name, shape, dt), Reg()

        T0, R0 = wt("T0"); T1, R1 = wt("T1"); T2, R2 = wt("T2"); T3, R3 = wt("T3")
        T4, R4 = wt("T4"); T5, R5 = wt("T5")
        sqb = [wt("sqb%d" % i, dt=BF16) for i in range(2)]
        qh, Rqh = wt("qh", dt=BF16); qt, Rqt = wt("qt", dt=BF16); kh, Rkh = wt("kh", dt=BF16)
        vtok, Rvt = wt("vtok", (128, 4, 256), BF16)
        ktok, Rkt = wt("ktok", (128, 4, 128), BF16)
        stm, Rstm = wt("stm", (128, 128), BF16)
        zb, Rzb = wt("zb", (128, 514), F32)
        zc, Rzc = wt("zc", (128, 4, 2), F32)
        spb, Rspb = wt("spb", (128, 4, 256), F32)
        gdb, Rgdb = wt("gdb", (128, 512), F32)
        gq = [wt("gq%d" % i, dt=BF16) for i in range(2)]
        nm, Rnm = wt("nm", (128, 4), F32)
        tS, RtS = wt("tS", (128, 128), F32)
        Sret, RSr = wt("Sret", (128, 4, 128), F32)
        Sretb, RSrb = wt("Sretb", (128, 4, 128), BF16)
        RSr = [Reg() for _ in range(4)]; RSrb = [Reg() for _ in range(4)]
        Sgla, RSg = wt("Sgla", (128, 2, 128), F32)
        Sglab, RSgb = wt("Sglab", (128, 2, 128), BF16)
        RSg = [Reg() for _ in range(2)]; RSgb = [Reg() for _ in range(2)]
        cosT, Rcos = wt("cosT"); sinT, Rsin = wt("sinT")
        posi, Rposi = wt("posi", (128, 512), I32)
        kint, Rkint = posi, Rposi
        cst, Rcst = wt("cst", (128, NCST), F32)
        pv, Rpv = wt("pv", (128, dd * LP), F32)
        bA, RbA = wt("bA", (128, dd, 256), F32)
        wa2, Rwa2 = wt("wa2", (128, dd, 256), F32)
        idb, Ridb = wt("idb", (128, 128), BF16)
        onesD, RoD = wt("onesD", (128, 128), BF16)
        onesH, RoH = wt("onesH", (128, 128), BF16)
        xin = [(ACC[:, 0:2, :].rearrange("p a b -> p (a b)"), [RA[0], RA[1]]),
               (ACC[:, 2:4, :].rearrange("p a b -> p (a b)"), [RA[2], RA[3]])]

        DBG = Reg()

        def tt(eng, out, in0, in1, o, reads, writes):
            op(eng, lambda e: e.tensor_tensor(out=out, in0=in0, in1=in1, op=o), reads, writes)

        def ts(eng, out, in0, s1, s2, o0, o1, reads, writes):
            if o1 is None:
                op(eng, lambda e: e.tensor_scalar(out=out, in0=in0, scalar1=s1, scalar2=None, op0=o0), reads, writes)
            else:
                op(eng, lambda e: e.tensor_scalar(out=out, in0=in0, scalar1=s1, scalar2=s2, op0=o0, op1=o1), reads, writes)

        def stt(out, in0, sc, in1, o0, o1, reads, writes):
            op("dve", lambda e: e.scalar_tensor_tensor(out=out, in0=in0, scalar=sc, in1=in1, op0=o0, op1=o1), reads, writes)

        def act(out, in_, func, reads, writes, bias=None, scale=None):
            kw = {}
            if bias is not None:
                kw["bias"] = bias
            if scale is not None:
                kw["scale"] = scale
            op("act", lambda e: e.activation(out=out, in_=in_, func=func, **kw), reads, writes)

        def acopy(out, in_, reads, writes):
            op("act", lambda e: e.copy(out=out, in_=in_), reads, writes)

        def mm(out, pairs, reads, writes):
            def f(e):
                inst = None
                n = len(pairs)
                for i, (l, r) in enumerate(pairs):
                    inst = e.matmul(out, lhsT=l, rhs=r, start=(i == 0), stop=(i == n - 1))
                return inst
            op("pe", f, reads, writes)

        def mm1(out, l, r, start, stop, reads, writes):
            op("pe", lambda e: e.matmul(out, lhsT=l, rhs=r, start=start, stop=stop), reads, writes)

        def tr(out, in_, ident, reads, writes):
            op("pe", lambda e: e.transpose(out=out, in_=in_, identity=ident), reads, writes)

        def pcol(l, off):
            return pv[:, l * LP + off: l * LP + off + 1]

        def tsl(tc):
            return slice(tc * 512, (tc + 1) * 512)

        units = []

        cur = {"l": 0, "n": 0}

        def unit(specs, fn):
            keys = []
            for _ in specs:
                keys.append((cur["l"], cur["n"]))
                cur["n"] += 1
            units.append((specs, fn, keys))

        def wseg(dram_ap_rows_by_cols, kc):
            return dram_ap_rows_by_cols.rearrange("(k p) n -> p k n", p=128)

        def setup(_):
            fw.dma("sp", [(cst[:], cst_d)], writes=[Rcst])
            fw.dma("sp", [(pv[:], pv_d)], writes=[Rpv])
            fw.dma("sp", [(bA[:], bA_d.rearrange("l p n -> p l n"))], writes=[RbA])
            op("dve", lambda e: e.memset(wa2[:], 0.0), [], [Rwa2])
            fw.dma("sp", [(wa2[0:16], wa2_d.rearrange("l r n -> r l n"))], writes=[Rwa2])
            for (g_, rg_) in gq:
                op("dve", lambda e: e.memset(g_[:], 0.0), [], [rg_])
            op("dve", lambda e: e.tensor_copy(out=idb[:], in_=cst[:, C_ID:C_ID + 128]), [Rcst], [Ridb])
            op("dve", lambda e: e.memset(onesD[:], 1.0 / D), [], [RoD])
            op("dve", lambda e: e.memset(onesH[:], 1.0 / 128), [], [RoH])

        unit([], setup)

        def load_x(tb):
            def f(_):
                xt_, rx_ = xin[tb % 2]
                fw.dma("sp", [(xt_, x_d[tb * 128:(tb + 1) * 128, :])], writes=rx_)
                tc = tb // 4
                for half in range(2):
                    pb = (tb * 2 + half) % 4
                    for j in range(4):
                        k = half * 4 + j
                        tr(P[pb][:, j * 128:(j + 1) * 128], xt_[:, k * 128:(k + 1) * 128], cst[:, C_ID:C_ID + 128],
                           rx_ + [Rcst], [RP[pb]])
                    dst = x[:, half * 4:half * 4 + 4, tb * 128:(tb + 1) * 128]
                    src = P[pb][:].rearrange("p (a b) -> p a b", a=4)
                    if half == 0:
                        op("act", lambda e: e.copy(out=dst, in_=src), [RP[pb]], [RX[k_][tc] for k_ in range(half * 4, half * 4 + 4)])
                    else:
                        op("dve", lambda e: e.tensor_copy(out=dst, in_=src), [RP[pb]], [RX[k_][tc] for k_ in range(half * 4, half * 4 + 4)])
            return f

        for tb in range(16):
            unit([], load_x(tb))

        def rstd_from(pb):
            act(T0[:], P[pb][:], AF.Ln, [RP[pb]], [R0], bias=EPS, scale=1.0)
            act(T1[:], T0[:], AF.Exp, [R0], [R1], scale=-0.5)

        def prenorm(l, tc, goff):
            def f(_):
                for k in range(KC):
                    sq, rsq = mrg[:, k, :], RM[k]
                    act(sq[:], x[:, k, tsl(tc)], AF.Square, [RX[k][tc]], [rsq])
                    mm1(P[7][:], onesD[:], sq[:], k == 0, k == KC - 1, [rsq, RoD], [RP[7]])
                rstd_from(7)
                for k in range(KC):
                    stt(hT[:, k, :], x[:, k, tsl(tc)], pcol(l, goff + k), T1[:], ALU.mult, ALU.mult,
                        [RX[k][tc], R1, Rpv], [RH[k]])
            return f

        def postnorm(l, tc, goff, dbgname=None):
            def f(_):
                for k in range(KC):
                    sq, rsq = mrg[:, k, :], RM[k]
                    act(sq[:], ACC[:, k, :], AF.Square, [RA[k]], [rsq])
                    mm1(P[7][:], onesD[:], sq[:], k == 0, k == KC - 1, [rsq, RoD], [RP[7]])
                rstd_from(7)
                for k in range(KC):
                    stt(T3[:], ACC[:, k, :], pcol(l, goff + k), T1[:], ALU.mult, ALU.mult, [RA[k], R1, Rpv], [R3])
                    tt("dve", x[:, k, tsl(tc)], x[:, k, tsl(tc)], T3[:], ALU.add, [R3, RX[k][tc]], [RX[k][tc]])
                    if debug and dbgname is not None and l == 0:
                        fw.dma("sp", [(dbg_d[dbgname][k, :, tsl(tc)], x[:, k, tsl(tc)])], reads=[RX[k][tc]])
            return f

        def proj(pb, wv, c0, reads_w, n=128):
            mm(P[pb][0:n, :], [(wv[:, k, c0:c0 + n], hT[:, k, :]) for k in range(KC)], [reads_w] + RH, [RP[pb]])

        def proj_tok(pb, ncol, wv, c0, reads_w, blk, o0):
            mm(P[pb][:, o0:o0 + ncol], [(hT[:, k, blk * 128:(blk + 1) * 128], wv[:, k, c0:c0 + ncol]) for k in range(KC)],
               [reads_w] + RH, [RP[pb]])

        def rope_tables(b_tc):
            tc = b_tc

            def f(_):
                fw.dma("sp", [(posi[:], pos_d[0:1, tsl(tc)].partition_broadcast(128))], writes=[Rposi])
                op("dve", lambda e: e.tensor_copy(out=T0[:], in_=posi[:]), [Rposi], [R0])
                ts("dve", T2[:], T0[:], cst[:, C_INVF:C_INVF + 1], None, ALU.mult, None, [R0, Rcst], [R2])
                ts("dve", kint[:], T2[:], 1.0 / (2 * math.pi), None, ALU.mult, None, [R2], [Rkint])
                op("dve", lambda e: e.tensor_copy(out=T0[:], in_=kint[:]), [Rkint], [R0])
                C1 = 6.28125
                C2 = 2 * math.pi - C1
                stt(T2[:], T0[:], -C1, T2[:], ALU.mult, ALU.add, [R0, R2], [R2])
                stt(T2[:], T0[:], -C2, T2[:], ALU.mult, ALU.add, [R0, R2], [R2])
                ts("dve", T2[:], T2[:], math.pi, -math.pi, ALU.min, ALU.max, [R2], [R2])
                act(T3[:], T2[:], AF.Sin, [R2], [R3])
                ts("dve", sinT[:], T3[:], cst[:, C_SIGN:C_SIGN + 1], None, ALU.mult, None, [R3, Rcst], [Rsin])
                act(T3[:], T2[:], AF.Sin, [R2], [R3], scale=0.5)
                tt("dve", T3[:], T3[:], T3[:], ALU.mult, [R3], [R3])
                ts("dve", cosT[:], T3[:], -2.0, 1.0, ALU.mult, ALU.add, [R3], [Rcos])
            return f

        def reset_states(_):
            op("dve", lambda e: e.memset(zc[:], 0.0), [], [Rzc])
            op("dve", lambda e: e.memset(Sret[:], 0.0), [], RSr)
            op("dve", lambda e: e.memset(Sretb[:], 0.0), [], RSrb)
            op("dve", lambda e: e.memset(Sgla[:], 0.0), [], RSg)
            op("dve", lambda e: e.memset(Sglab[:], 0.0), [], RSgb)

        def conv_unit(l, tc, c):
            win = w_in_d[l]
            specs = [[(0, 8, 128, wseg(win[:, O_CU + c * 128:O_CU + (c + 1) * 128], 8)),
                      (1024, 8, 128, wseg(win[:, O_CB + c * 128:O_CB + (c + 1) * 128], 8)),
                      (2048, 8, 128, wseg(win[:, O_CC + c * 128:O_CC + (c + 1) * 128], 8))]]

            def f(pc):
                (sl, rs) = pc[0]
                wv = sl[:, 0:3072].rearrange("p (s k n) -> p s k n", s=3, k=8)
                proj(0, wv[:, 0], 0, rs)
                proj(1, wv[:, 1], 0, rs)
                proj(2, wv[:, 2], 0, rs)
                acopy(T0[:], P[0][:], [RP[0]], [R0])
                acopy(zb[:, 0:2], zc[:, c, :], [Rzc], [Rzb])
                tt("dve", zb[:, 2:514], P[2][:], T0[:], ALU.mult, [RP[2], R0], [Rzb])
                acopy(zc[:, c, :], zb[:, 512:514], [Rzb], [Rzc])
                ts("dve", T2[:], zb[:, 2:514], pcol(l, 32 + 8 + c), None, ALU.mult, None, [Rzb, Rpv], [R2])
                stt(T2[:], zb[:, 1:513], pcol(l, 32 + 4 + c), T2[:], ALU.mult, ALU.add, [Rzb, Rpv, R2], [R2])
                stt(T2[:], zb[:, 0:512], pcol(l, 32 + 0 + c), T2[:], ALU.mult, ALU.add, [Rzb, Rpv, R2], [R2])
                tt("dve", ybuf[0][:, c, :], P[1][:], T2[:], ALU.mult, [RP[1], R2], [RY[0][c]])
                if debug and l == 0:
                    fw.dma("sp", [(dbg_d["ya"][c, :, tsl(tc)], ybuf[0][:, c, :])], reads=[RY[0][c]])
            unit(specs, f)

        def rope(pb):
            tt("dve", T0[:], P[pb][:], cosT[:], ALU.mult, [RP[pb], Rcos], [R0])
            acopy(T5[:], P[pb][:], [RP[pb]], [R5])
            acopy(T2[0:64, :], T5[64:128, :], [R5], [R2])
            acopy(T2[64:128, :], T5[0:64, :], [R5], [R2])
            tt("dve", T2[:], T2[:], sinT[:], ALU.mult, [R2, Rsin], [R2])
            tt("dve", T3[:], T0[:], T2[:], ALU.add, [R0, R2], [R3])

        def headnorm(pbo, pbm, sg_t, sg_r, gcol, out_ap, out_reg):
            sq, rsq = sqb[0]
            act(sq[:], P[pbo][:], AF.Square, [RP[pbo]], [rsq])
            mm1(P[pbm][:], onesH[:], sq[:], True, True, [rsq, RoH], [RP[pbm]])
            rstd_from(pbm)
            tt("dve", T3[:], P[pbo][:], T1[:], ALU.mult, [RP[pbo], R1], [R3])
            stt(out_ap, T3[:], gcol, sg_t[:], ALU.mult, ALU.mult, [R3, Rpv, sg_r], [out_reg])

        GAM = [1.0 - 2.0 ** (-5.0 - h) for h in range(4)]

        def ret_unit(l, tc, h):
            win = w_in_d[l]
            specs = [[(i * 1024, 8, 128, wseg(win[:, o + h * 128:o + (h + 1) * 128], 8))
                      for i, o in enumerate((O_RQ, O_RK, O_RV, O_RG))]]

            def f(pc):
                (sl, rs) = pc[0]
                wv = sl[:, 0:4096].rearrange("p (s k n) -> p s k n", s=4, k=8)
                proj(0, wv[:, 0], 0, rs)
                proj(1, wv[:, 1], 0, rs)
                proj(2, wv[:, 3], 0, rs)
                for blk in range(4):
                    proj_tok(3, 128, wv[:, 2], 0, rs, blk, blk * 128)
                if KSTOP <= 1: return
                if 'KOPS' in os.environ: fw.limit = int(os.environ['KOPS'])
                rope(0)
                op("dve", lambda e: e.tensor_copy(out=qh[:], in_=T3[:]), [R3], [Rqh])
                tt("dve", qt[:].rearrange("p (a b) -> p a b", a=4), T3[:].rearrange("p (a b) -> p a b", a=4),
                   cst[:, C_QDEC + h * 128:C_QDEC + (h + 1) * 128].unsqueeze(1).to_broadcast([128, 4, 128]),
                   ALU.mult, [R3, Rcst], [Rqt])
                if KSTOP <= 2:
                    fw.limit = None
                    return
                rope(1)
                op("dve", lambda e: e.tensor_copy(out=kh[:], in_=T3[:]), [R3], [Rkh])
                if KSTOP <= 3: return
                act(T4[:], P[2][:], AF.Silu, [RP[2]], [R4])
                if KSTOP <= 4: return
                pv3 = P[3][:].rearrange("p (a b) -> p a b", a=4)
                op("dve", lambda e: e.tensor_reduce(out=nm[:], in_=pv3, axis=AX.X, op=ALU.add), [RP[3]], [Rnm])
                ts("dve", nm[:], nm[:], -1.0 / 128, None, ALU.mult, None, [Rnm], [Rnm])
                for blk in range(4):
                    ts("dve", vtok[:, blk, 0:128], P[3][:, blk * 128:(blk + 1) * 128], nm[:, blk:blk + 1], None,
                       ALU.add, None, [RP[3], Rnm], [Rvt])
                if KSTOP <= 5: return
                p4b = P[4][:].bitcast(BF16)
                for blk in range(4):
                    tr(p4b[:, blk * 128:(blk + 1) * 128], kh[:, blk * 128:(blk + 1) * 128], idb[:], [Rkh, Ridb], [RP[4]])
                ts("dve", ktok[:].rearrange("p a b -> p (a b)"), p4b[:, 0:512], cst[:, C_KDEC + h:C_KDEC + h + 1], None,
                   ALU.mult, None, [RP[4], Rcst], [Rkt])
                if KSTOP <= 6: return
                g128 = GAM[h] ** 128
                for blk in range(4):
                    bs = slice(blk * 128, (blk + 1) * 128)
                    mm(P[5][:, 0:128], [(kh[:, bs], qh[:, bs])], [Rkh, Rqh], [RP[5]])
                    tt("dve", stm[:], P[5][:, 0:128], cst[:, C_RMASK + h * 128:C_RMASK + (h + 1) * 128], ALU.mult,
                       [RP[5], Rcst], [Rstm])
                    mm(P[6][:, bs], [(vtok[:, blk, 0:128], stm[:]), (Sretb[:, h, :], qt[:, bs])],
                       [Rvt, Rstm, RSrb[h], Rqt], [RP[6]])
                    mm(P[7][:, 0:128], [(ktok[:, blk, :], vtok[:, blk, 0:128])], [Rkt, Rvt], [RP[7]])
                    stt(Sret[:, h, :], Sret[:, h, :], g128, P[7][:, 0:128], ALU.mult, ALU.add, [RSr[h], RP[7]], [RSr[h]])
                    acopy(Sretb[:, h, :], Sret[:, h, :], [RSr[h]], [RSrb[h]])
                if KSTOP <= 7: return
                headnorm(6, 7, T4, R4, pcol(l, 44 + h), ybuf[1][:, h, :], RY[1][h])
                if debug and l == 0:
                    fw.dma("sp", [(dbg_d["yb"][h, :, tsl(tc)], ybuf[1][:, h, :])], reads=[RY[1][h]])
            unit(specs, f)

        def gla_prelude(l, tc):
            win = w_in_d[l]
            specs = [[(0, 8, 128, wseg(win[:, O_GA:O_GA + 128], 8))]]

            def f(pc):
                (sl, rs) = pc[0]
                wv = sl[:, 0:1024].rearrange("p (k n) -> p k n", k=8)
                mm(P[0][:, :], [(wv[:, k, :], hT[:, k, :]) for k in range(KC)], [rs] + RH, [RP[0]])
                acopy(gdb[:], P[0][:, :], [RP[0]], [Rgdb])
                for half in range(2):
                    pb = 1 + half
                    for j in range(2):
                        blk = half * 2 + j
                        mm(P[pb][:, j * 256:(j + 1) * 256], [(gdb[:, blk * 128:(blk + 1) * 128], wa2[:, l, :])],
                           [Rgdb, Rwa2], [RP[pb]])
                    sv = spb[:, half * 2:half * 2 + 2, :]
                    tt("dve", sv, P[pb][:].rearrange("p (a b) -> p a b", a=2),
                       bA[:, l, :].unsqueeze(1).to_broadcast([128, 2, 256]), ALU.add, [RP[pb], RbA], [Rspb])
                    act(sv, sv, AF.Exp, [Rspb], [Rspb], scale=-1.0)
                    act(sv, sv, AF.Ln, [Rspb], [Rspb], bias=1.0, scale=1.0)
                for hp in range(2):
                    for blk in range(4):
                        mm(P[3 + hp][:, blk * 128:(blk + 1) * 128],
                           [(spb[:, blk, hp * 128:(hp + 1) * 128], cst[:, C_UNEG:C_UNEG + 128])], [Rspb, Rcst], [RP[3 + hp]])
            unit(specs, f)

        def gla_unit(l, tc, hp):
            win = w_in_d[l]
            specs = [[(0, 8, 128, wseg(win[:, O_GQ + hp * 128:O_GQ + (hp + 1) * 128], 8)),
                      (1024, 8, 128, wseg(win[:, O_GK + hp * 128:O_GK + (hp + 1) * 128], 8)),
                      (2048, 8, 256, wseg(win[:, O_GV + hp * 256:O_GV + (hp + 1) * 256], 8))],
                     [(0, 8, 256, wseg(win[:, O_GR + hp * 256:O_GR + (hp + 1) * 256], 8))]]

            def f(pc):
                (sl, rs), (sl2, rs2) = pc
                wq = sl[:, 0:1024].rearrange("p (k n) -> p k n", k=8)
                wk = sl[:, 1024:2048].rearrange("p (k n) -> p k n", k=8)
                wvv = sl[:, 2048:4096].rearrange("p (k n) -> p k n", k=8)
                wr = sl2[:, 0:2048].rearrange("p (k n) -> p k n", k=8)
                pc_ = 3 + hp
                act(T0[:], P[pc_][:], AF.Exp, [RP[pc_]], [R0])
                act(T2[:], P[pc_][:], AF.Exp, [RP[pc_]], [R2], scale=-1.0)
                proj(0, wq, 0, rs)
                proj(1, wk, 0, rs)
                for e_ in range(2):
                    ps = slice(64 * e_, 64 * e_ + 64)
                    stt(gq[e_][0][ps, :], P[0][ps, :], 0.125, T0[ps, :], ALU.mult, ALU.mult, [RP[0], R0], [gq[e_][1]])
                tt("dve", kh[:], P[1][:], T2[:], ALU.mult, [RP[1], R2], [Rkh])
                for half, pb in ((0, 2), (1, 5)):
                    for j in range(2):
                        proj_tok(pb, 256, wvv, 0, rs, half * 2 + j, j * 256)
                    acopy(vtok[:, half * 2:half * 2 + 2, :], P[pb][:].rearrange("p (a b) -> p a b", a=2), [RP[pb]], [Rvt])
                p6b = P[6][:].bitcast(BF16)
                for blk in range(4):
                    tr(p6b[:, blk * 128:(blk + 1) * 128], kh[:, blk * 128:(blk + 1) * 128], idb[:], [Rkh, Ridb], [RP[6]])
                acopy(ktok[:].rearrange("p a b -> p (a b)"), p6b[:, 0:512], [RP[6]], [Rkt])
                proj(7, wr, 0, rs2)
                act(T4[:], P[7][:], AF.Silu, [RP[7]], [R4])
                proj(5, wr, 128, rs2)
                act(T5[:], P[5][:], AF.Silu, [RP[5]], [R5])
                for blk in range(4):
                    bs = slice(blk * 128, (blk + 1) * 128)
                    for e_ in range(2):
                        gq_, rgq_ = gq[e_]
                        mm(P[0][:, 0:128], [(kh[:, bs], gq_[:, bs])], [Rkh, rgq_], [RP[0]])
                        tt("dve", stm[:], P[0][:, 0:128], cst[:, C_CAUS:C_CAUS + 128], ALU.mult, [RP[0], Rcst], [Rstm])
                        mm(P[1 + e_][:, bs], [(vtok[:, blk, e_ * 128:(e_ + 1) * 128], stm[:]), (Sglab[:, hp, :], gq_[:, bs])],
                           [Rvt, Rstm, RSgb[hp], rgq_], [RP[1 + e_]])
                    for e_ in range(2):
                        mm(P[6][:, e_ * 128:(e_ + 1) * 128], [(ktok[:, blk, :], vtok[:, blk, e_ * 128:(e_ + 1) * 128])],
                           [Rkt, Rvt], [RP[6]])
                    for e_ in range(2):
                        ps = slice(64 * e_, 64 * e_ + 64)
                        tt("dve", tS[ps, :], P[6][ps, e_ * 128:(e_ + 1) * 128], Sgla[ps, hp, :], ALU.add, [RP[6], RSg[hp]], [RtS])
                    ts("dve", Sgla[:, hp, :], tS[:], T0[:, blk * 128 + 127:blk * 128 + 128], None, ALU.mult, None,
                       [RtS, R0], [RSg[hp]])
                    acopy(Sglab[:, hp, :], Sgla[:, hp, :], [RSg[hp]], [RSgb[hp]])
                for e_, (sg_t, sg_r) in enumerate(((T4, R4), (T5, R5))):
                    hh = 2 * hp + e_
                    headnorm(1 + e_, 7, sg_t, sg_r, pcol(l, 48 + hh), ybuf[2][:, hh, :], RY[2][hh])
                    if debug and l == 0:
                        fw.dma("sp", [(dbg_d["yc"][hh, :, tsl(tc)], ybuf[2][:, hh, :])], reads=[RY[2][hh]])
            unit(specs, f)

        def merge_unit(l, tc, i):
            win = w_in_d[l]
            c0 = O_GATE + i * 1024
            specs = [[(0, 8, 512, wseg(win[:, c0:c0 + 512], 8))],
                     [(0, 8, 512, wseg(win[:, c0 + 512:c0 + 1024], 8))],
                     [(0, 4, 1024, wseg(wbr_d[i][l], 4))]]

            def f(pc):
                (slb, rsb) = pc[2]
                wb_ = slb[:, 0:4096].rearrange("p (k n) -> p k n", k=4)
                for half in range(2):
                    (sl, rs) = pc[half]
                    wg_ = sl[:, 0:4096].rearrange("p (k n) -> p k n", k=8)
                    for j in range(4):
                        oc = half * 4 + j
                        pa, pb = (0, 1) if j % 2 == 0 else (2, 3)
                        proj(pa, wg_, j * 128, rs)
                        mm(P[pb][:], [(wb_[:, kk, oc * 128:(oc + 1) * 128], ybuf[i][:, kk, :]) for kk in range(4)],
                           [rsb] + RY[i], [RP[pb]])
                        tw, rw = (T0, R0) if j % 2 == 0 else (T2, R2)
                        act(tw[:], P[pa][:], AF.Tanh, [RP[pa]], [rw], scale=0.5)
                        if i == 0:
                            stt(ACC[:, oc, :], tw[:], 1.0, P[pb][:], ALU.add, ALU.mult, [rw, RP[pb]], [RA[oc]])
                        else:
                            stt(tw[:], tw[:], 1.0, P[pb][:], ALU.add, ALU.mult, [rw, RP[pb]], [rw])
                            tt("dve", ACC[:, oc, :], ACC[:, oc, :], tw[:], ALU.add, [RA[oc], rw], [RA[oc]])
                            if i == 2:
                                op("act", lambda e: e.mul(out=mrg[:, oc, :], in_=ACC[:, oc, :], mul=0.5), [RA[oc]], [RM[oc]])
            unit(specs, f)

        def out_unit(l, tc, half):
            specs = [[(0, 8, 512, wseg(wout_d[l][:, half * 512:(half + 1) * 512], 8))]]

            def f(pc):
                (sl, rs) = pc[0]
                wo = sl[:, 0:4096].rearrange("p (k n) -> p k n", k=8)
                for j in range(4):
                    oc = half * 4 + j
                    pa = j % 4
                    mm(P[pa][:], [(wo[:, k, j * 128:(j + 1) * 128], mrg[:, k, :]) for k in range(KC)], [rs] + RM, [RP[pa]])
                    acopy(ACC[:, oc, :], P[pa][:], [RP[pa]], [RA[oc]])
            unit(specs, f)

        def ffn_unit(l, tc, g):
            c0 = g * 256
            specs = [[(0, 8, 256, wseg(wg_d[l][:, c0:c0 + 256], 8)),
                      (2048, 8, 256, wseg(wu_d[l][:, c0:c0 + 256], 8))],
                     [(0, 2, 1024, wseg(wd_d[l][c0:c0 + 256, :], 2))]]

            def f(pc):
                (sl, rs), (sl2, rs2) = pc
                wg_ = sl[:, 0:2048].rearrange("p (k n) -> p k n", k=8)
                wu_ = sl[:, 2048:4096].rearrange("p (k n) -> p k n", k=8)
                wd_ = sl2[:, 0:2048].rearrange("p (k n) -> p k n", k=2)
                par = g % 2
                for j in range(2):
                    pa, pb = (0, 1) if j == 0 else (2, 3)
                    proj(pa, wg_, j * 128, rs)
                    proj(pb, wu_, j * 128, rs)
                    tw, rw = (T0, R0) if j == 0 else (T2, R2)
                    act(tw[:], P[pa][:], AF.Silu, [RP[pa]], [rw])
                    tt("dve", actb[:, par, j, :], tw[:], P[pb][:], ALU.mult, [rw, RP[pb]], [RACT[par][j]])
                for oc in range(KC):
                    pa = 4 + oc % 3
                    mm(P[pa][:], [(wd_[:, j, oc * 128:(oc + 1) * 128], actb[:, par, j, :]) for j in range(2)],
                       [rs2] + RACT[par], [RP[pa]])
                    if g == 0:
                        acopy(ACC[:, oc, :], P[pa][:], [RP[pa]], [RA[oc]])
                    else:
                        tt("dve", ACC[:, oc, :], ACC[:, oc, :], P[pa][:], ALU.add, [RA[oc], RP[pa]], [RA[oc]])
            unit(specs, f)

        ROUT = [Reg(), Reg()]

        def store_x(tb):
            def f(_):
                tc = tb // 4
                ot, ro = xin[tb % 2]
                for half in range(2):
                    pb = (tb * 2 + half) % 4
                    for j in range(4):
                        k = half * 4 + j
                        tr(P[pb][:, j * 128:(j + 1) * 128], x[:, k, tb * 128:(tb + 1) * 128], cst[:, C_ID:C_ID + 128],
                           [RX[k][tc], Rcst], [RP[pb]])
                    if half == 0:
                        acopy(ot[:, 0:512], P[pb][:], [RP[pb]], ro)
                    else:
                        op("dve", lambda e: e.tensor_copy(out=ot[:, 512:1024], in_=P[pb][:]), [RP[pb]], ro)
                fw.dma("sp", [(out_d[tb * 128:(tb + 1) * 128, :], ot)], reads=ro, owner=ROUT[tb % 2])
            return f

        for l in range(depth):
            unit([], reset_states)
            for tc in range(ntc):
                cur["l"] = l
                cur["n"] = 0
                unit([], prenorm(l, tc, 0))
                if "A" in phases:
                    for c in range(4):
                        conv_unit(l, tc, c)
                if "B" in phases:
                    if os.environ.get('KROPE', '1') == '1':
                        unit([], rope_tables(tc))
                    for h in range(4):
                        ret_unit(l, tc, h)
                if "C" in phases:
                    gla_prelude(l, tc)
                    for hp in range(2):
                        gla_unit(l, tc, hp)
                if "D" in phases:
                    for i in range(3):
                        merge_unit(l, tc, i)
                if "E" in phases:
                    for half in range(2):
                        out_unit(l, tc, half)
                    unit([], postnorm(l, tc, 8, "xmix"))
                if "F" in phases:
                    unit([], prenorm(l, tc, 16))
                    for g in range(NF // 2):
                        ffn_unit(l, tc, g)
                    unit([], postnorm(l, tc, 24, "xffn"))
        for tb in range(16):
            unit([], store_x(tb))

        pieces = []
        upieces = []
        for ui, (specs, fn, keys) in enumerate(units):
            idxs = []
            for segs, key in zip(specs, keys):
                idxs.append(len(pieces))
                pieces.append((ui, segs, key))
            upieces.append(idxs)
        GSZ = 6
        first = {}
        for (ui, segs, key) in pieces:
            first.setdefault(key, segs)
        npc = max([k[1] for k in first] + [0]) + 1
        scr = [nc.dram_tensor("scr%d" % l, [npc, 128, SLOT], BF16).ap() for l in range(max(depth, 1))]
        ngrp = (npc + GSZ - 1) // GSZ
        RG = [[Reg() for _ in range(ngrp)] for _ in range(max(depth, 1))]
        for l in range(depth):
            for g in range(ngrp):
                csegs = []
                for j in range(g * GSZ, min(npc, (g + 1) * GSZ)):
                    if (l, j) not in first:
                        continue
                    for (off, kc, n, src) in first[(l, j)]:
                        dst = scr[l][j][:, off:off + kc * n].rearrange("p (k n) -> p k n", k=kc)
                        csegs.append((dst, src))
                if csegs:
                    fw.dma("pool", csegs, writes=[RG[l][g]])
        nextp = 0

        def issue_loads(cur_unit):
            nonlocal nextp
            while nextp < len(pieces):
                if nextp >= NSLOT and pieces[nextp - NSLOT][0] >= cur_unit:
                    break
                ui, segs, (l_, j_) = pieces[nextp]
                s_ = nextp % NSLOT
                L = max(off + kc * n for (off, kc, n, src) in segs)
                fw.dma("sp", [(slots[s_][:, 0:L], scr[l_][j_][:, 0:L])], reads=[RG[l_][j_ // GSZ]], writes=[RS[s_]])
                nextp += 1

        for ui, (specs, fn, keys) in enumerate(units):
            issue_loads(ui)
            for pi_ in upieces[ui]:
                assert pi_ < nextp, "piece not loaded: raise NSLOT"
            fn([(slots[pi_ % NSLOT], RS[pi_ % NSLOT]) for pi_ in upieces[ui]])

        fw.wait_all("sp", xin[0][1] + xin[1][1])
        if debug:
            fw.wait_all("sp", [r for rr in RY for r in rr] + [r for rr in RX for r in rr])
    return nc


def _consts():
    c = np.zeros((128, NCST), np.float64)
    p = np.arange(128)
    half = 64
    invf = 10000.0 ** (-(np.arange(half, dtype=np.float32) / np.float32(half)))
    c[:, C_INVF] = np.concatenate([invf, invf]).astype(np.float32)
    c[:, C_SIGN] = np.where(p < 64, -1.0, 1.0)
    i = np.arange(128)
    for h in range(4):
        lg = math.log(1.0 - 2.0 ** (-5.0 - h))
        c[:, C_KDEC + h] = np.exp(lg * (127 - p))
        diff = i[None, :] - p[:, None]
        c[:, C_RMASK + h * 128:C_RMASK + (h + 1) * 128] = np.where(diff >= 0, np.exp(lg * np.maximum(diff, 0)), 0.0) * 128 ** -0.5
        c[:, C_QDEC + h * 128:C_QDEC + (h + 1) * 128] = (np.exp(lg * (i + 1.0)) * 128 ** -0.5)[None, :]
    c[:, C_CAUS:C_CAUS + 128] = (i[None, :] >= p[:, None]).astype(np.float64)
    c[:, C_UNEG:C_UNEG + 128] = np.where(p[:, None] <= i[None, :], -1.0 / 16.0, 0.0)
    c[:, C_ID:C_ID + 128] = np.eye(128)
    return c.astype(np.float32)


_CACHE = {}


def kernel(x, positions, norm_mix_pre, w_in, conv_w, ret_gn_w, gla_w_a2, gla_b_a, gla_gn_w,
           w_branch_a, w_branch_b, w_branch_c, w_out, norm_mix_post, norm_ffn_pre,
           w_ffn_gate, w_ffn_up, w_ffn_down, norm_ffn_post, _debug=False, _dd=DEPTH, _ncores=8):
    f32 = lambda a: np.ascontiguousarray(np.asarray(a, dtype=np.float32))
    x = f32(x)
    positions = np.ascontiguousarray(np.asarray(positions, dtype=np.int32))
    pv = np.zeros((128, _dd * LP), np.float32)
    for l in range(_dd):
        b = l * LP
        for j, g in enumerate((norm_mix_pre, norm_mix_post, norm_ffn_pre, norm_ffn_post)):
            pv[:, b + 8 * j:b + 8 * j + 8] = f32(g)[l].reshape(8, 128).T
        pv[:, b + 32:b + 44] = f32(conv_w)[l].reshape(3, 4, 128).transpose(2, 0, 1).reshape(128, 12)
        pv[:, b + 44:b + 48] = f32(ret_gn_w)[l].reshape(4, 128).T
        pv[:, b + 48:b + 52] = f32(gla_gn_w)[l].reshape(4, 128).T
    bA = np.ascontiguousarray(np.broadcast_to(f32(gla_b_a)[:_dd, None, :], (_dd, 128, 256)))
    shared = {
        "w_in": f32(w_in[:_dd]), "gla_w_a2": f32(gla_w_a2[:_dd]), "bA": bA,
        "w_branch_a": f32(w_branch_a[:_dd]), "w_branch_b": f32(w_branch_b[:_dd]), "w_branch_c": f32(w_branch_c[:_dd]),
        "w_out": f32(w_out[:_dd]), "w_ffn_gate": f32(w_ffn_gate[:_dd]), "w_ffn_up": f32(w_ffn_up[:_dd]),
        "w_ffn_down": f32(w_ffn_down[:_dd]), "pv": pv, "cst": _consts(),
    }
    key = bool(_debug)
    if key not in _CACHE:
        _CACHE[key] = build_program(debug=_debug)
    nc = _CACHE[key]
    in_maps = []
    for b in range(_ncores):
        m = dict(shared)
        m["x"] = x[b]
        m["pos"] = positions[b:b + 1]
        in_maps.append(m)
    res = run_bass_kernel_spmd(nc, in_maps, core_ids=list(range(_ncores)))
    out = np.stack([np.asarray(r["out"], dtype=np.float32) for r in res.results], axis=0)
    if _debug:
        kernel.last = res.results
    return out
```

```python
import math
import os
KSTOP = int(os.environ.get('KSTOP', '99'))
from contextlib import ExitStack

import numpy as np
import concourse.bass as bass
import concourse.mybir as mybir
from concourse.bass_utils import run_bass_kernel_spmd

F32 = mybir.dt.float32
BF16 = mybir.dt.bfloat16
I32 = mybir.dt.int32
ALU = mybir.AluOpType
AF = mybir.ActivationFunctionType
AX = mybir.AxisListType

D = 1024
T = 2048
DEPTH = 2
NTC = 4
KC = 8
IN_COLS = 8208
DFF = 2816
NF = 22
EPS = 1e-6
NSLOT = 5
SLOT = 4096
LP = 52

O_CU, O_CB, O_CC = 0, 512, 1024
O_RQ, O_RK, O_RV, O_RG = 1536, 2048, 2560, 3072
O_GQ, O_GK, O_GV, O_GR, O_GA = 3584, 3840, 4096, 4608, 5120
O_GATE = 5136

C_INVF, C_SIGN, C_KDEC, C_RMASK, C_QDEC, C_CAUS, C_UNEG, C_ID = 0, 1, 2, 6, 518, 1030, 1158, 1286
NCST = 1414


class Eng:
    def __init__(self, name, e, sem):
        self.name, self.e, self.sem = name, e, sem
        self.cnt = 0
        self.seen = {}


class Reg:
    __slots__ = ("w", "rs", "dsem", "dcnt", "excl")

    def __init__(self, excl=False):
        self.excl = excl
        self.w = None
        self.rs = {}
        self.dsem = None
        self.dcnt = 0


class FW:
    def __init__(self, nc, stack):
        self.nc = nc
        self.stack = stack
        self.E = {}
        for name, e in (("pe", nc.tensor), ("act", nc.scalar), ("dve", nc.vector),
                        ("pool", nc.gpsimd), ("sp", nc.sync)):
            sem = stack.enter_context(nc.semaphore("s_" + name))
            self.E[name] = Eng(name, e, sem)
        self.nsem = 5
        self.limit = None

    def sbuf(self, name, shape, dt):
        return self.stack.enter_context(self.nc.sbuf_tensor("sb_" + name, list(shape), dt))

    def psum(self, name, shape, dt):
        return self.stack.enter_context(self.nc.psum_tensor(name, list(shape), dt))

    def newsem(self):
        self.nsem += 1
        return self.stack.enter_context(self.nc.semaphore("d%d" % self.nsem))

    def _need(self, E, tok, need, same_ok):
        if tok is None:
            return
        if tok[0] == "e":
            F, n = tok[1], tok[2]
            if F is E and E.name in ("pe", "sp"):
                return
            key = F.name
            sem = F.sem
        else:
            _, sem, n, sid = tok
            key = ("d", sid)
        if E.seen.get(key, 0) >= n:
            return
        if need.get(key, (None, 0))[1] < n:
            need[key] = (sem, n)

    def _waits(self, E, reads, writes):
        need = {}
        for r in reads:
            self._need(E, r.w, need, False)
            if r.excl:
                for t in r.rs.values():
                    if t[0] == "e" and t[1] is E:
                        continue
                    self._need(E, t, need, True)
        for w in writes:
            self._need(E, w.w, need, False)
            for t in w.rs.values():
                self._need(E, t, need, True)
        for key, (sem, n) in need.items():
            E.e.wait_ge(sem, n)
            E.seen[key] = n

    def op(self, eng, fn, reads=(), writes=()):
        if self.limit is not None:
            if self.limit <= 0:
                return None
            self.limit -= 1
        E = self.E[eng]
        self._waits(E, reads, writes)
        inst = fn(E.e)
        E.cnt += 1
        inst.then_inc(E.sem, 1)
        tok = ("e", E, E.cnt)
        for r in reads:
            r.rs[E.name] = tok
        for w in writes:
            w.w = tok
            w.rs = {}
        return inst

    def dma(self, q, segs, reads=(), writes=(), owner=None):
        E = self.E[q]
        self._waits(E, reads, writes)
        owner = owner if owner is not None else (writes[0] if writes else reads[0])
        if owner.dsem is None:
            owner.dsem = self.newsem()
        for (o, i) in segs:
            inst = E.e.dma_start(out=o, in_=i)
            owner.dcnt += 16
            inst.then_inc(owner.dsem, 16)
        tok = ("d", owner.dsem, owner.dcnt, id(owner))
        for r in reads:
            r.rs[("d", id(owner))] = tok
        for w in writes:
            w.w = tok
            w.rs = {}

    def wait_all(self, eng, regs):
        self._waits(self.E[eng], [], regs)


def build_program(debug=False, depth=DEPTH, ntc=NTC, phases="ABCDEFG"):
    nc = bass.Bass("TRN2", target_bir_lowering=False)

    def din(name, shape, dt=F32):
        return nc.dram_tensor(name, list(shape), dt, kind="ExternalInput").ap()

    dd = max(depth, 1)
    x_d = din("x", [T, D])
    pos_d = din("pos", [1, T], I32)
    w_in_d = din("w_in", [dd, D, IN_COLS])
    wa2_d = din("gla_w_a2", [dd, 16, 256])
    bA_d = din("bA", [dd, 128, 256])
    wbr_d = [din("w_branch_" + n, [dd, 512, D]) for n in "abc"]
    wout_d = din("w_out", [dd, D, D])
    wg_d = din("w_ffn_gate", [dd, D, DFF])
    wu_d = din("w_ffn_up", [dd, D, DFF])
    wd_d = din("w_ffn_down", [dd, DFF, D])
    pv_d = din("pv", [128, dd * LP])
    cst_d = din("cst", [128, NCST])
    out_d = nc.dram_tensor("out", [T, D], F32, kind="ExternalOutput").ap()
    dbg_d = {}
    if debug:
        for n in ("ya", "yb", "yc"):
            dbg_d[n] = nc.dram_tensor("dbg_" + n, [4, 128, T], BF16, kind="ExternalOutput").ap()
        for n in ("xmix", "xffn"):
            dbg_d[n] = nc.dram_tensor("dbg_" + n, [8, 128, T], F32, kind="ExternalOutput").ap()

    st = ExitStack()
    with st:
        fw = FW(nc, st)
        op = fw.op

        x = fw.sbuf("x", [128, KC, T], F32)
        RX = [[Reg() for _ in range(NTC)] for _ in range(KC)]
        hT = fw.sbuf("hT", [128, KC, 512], BF16)
        RH = [Reg() for _ in range(KC)]
        ybuf = [fw.sbuf("y%d" % i, [128, 4, 512], BF16) for i in range(3)]
        RY = [[Reg() for _ in range(4)] for _ in range(3)]
        ACC = fw.sbuf("acc", [128, KC, 512], F32)
        RA = [Reg() for _ in range(KC)]
        mrg = fw.sbuf("mrg", [128, KC, 512], BF16)
        RM = [Reg() for _ in range(KC)]
        actb = fw.sbuf("actb", [128, 2, 2, 512], BF16)
        RACT = [[Reg() for _ in range(2)] for _ in range(2)]
        slots = [fw.sbuf("slot%d" % i, [128, SLOT], BF16) for i in range(NSLOT)]
        RS = [Reg() for _ in range(NSLOT)]
        P = [fw.psum("ps%d" % i, [128, 512], F32) for i in range(8)]
        RP = [Reg(excl=True) for _ in range(8)]

        def wt(name, shape=(128, 512), dt=F32):
            return fw.sbuf(name, shape, dt), Reg()

        T0, R0 = wt("T0"); T1, R1 = wt("T1"); T2, R2 = wt("T2"); T3, R3 = wt("T3")
        T4, R4 = wt("T4"); T5, R5 = wt("T5")
        sqb = [wt("sqb%d" % i, dt=BF16) for i in range(2)]
        qh, Rqh = wt("qh", dt=BF16); qt, Rqt = wt("qt", dt=BF16); kh, Rkh = wt("kh", dt=BF16)
        vtok, Rvt = wt("vtok", (128, 4, 256), BF16)
        ktok, Rkt = wt("ktok", (128, 4, 128), BF16)
        stm, Rstm = wt("stm", (128, 128), BF16)
        zb, Rzb = wt("zb", (128, 514), F32)
        zc, Rzc = wt("zc", (128, 4, 2), F32)
        spb, Rspb = wt("spb", (128, 4, 256), F32)
        gdb, Rgdb = wt("gdb", (128, 512), F32)
        gq = [wt("gq%d" % i, dt=BF16) for i in range(2)]
        nm, Rnm = wt("nm", (128, 4), F32)
        tS, RtS = wt("tS", (128, 128), F32)
        Sret, RSr = wt("Sret", (128, 4, 128), F32)
        Sretb, RSrb = wt("Sretb", (128, 4, 128), BF16)
        RSr = [Reg() for _ in range(4)]; RSrb = [Reg() for _ in range(4)]
        Sgla, RSg = wt("Sgla", (128, 2, 128), F32)
        Sglab, RSgb = wt("Sglab", (128, 2, 128), BF16)
        RSg = [Reg() for _ in range(2)]; RSgb = [Reg() for _ in range(2)]
        cosT, Rcos = wt("cosT"); sinT, Rsin = wt("sinT")
        posi, Rposi = wt("posi", (128, 512), I32)
        kint, Rkint = posi, Rposi
        cst, Rcst = wt("cst", (128, NCST), F32)
        pv, Rpv = wt("pv", (128, dd * LP), F32)
        bA, RbA = wt("bA", (128, dd, 256), F32)
        wa2, Rwa2 = wt("wa2", (128, dd, 256), F32)
        idb, Ridb = wt("idb", (128, 128), BF16)
        onesD, RoD = wt("onesD", (128, 128), BF16)
        onesH, RoH = wt("onesH", (128, 128), BF16)
        xin = [(ACC[:, 0:2, :].rearrange("p a b -> p (a b)"), [RA[0], RA[1]]),
               (ACC[:, 2:4, :].rearrange("p a b -> p (a b)"), [RA[2], RA[3]])]

        DBG = Reg()

        def tt(eng, out, in0, in1, o, reads, writes):
            op(eng, lambda e: e.tensor_tensor(out=out, in0=in0, in1=in1, op=o), reads, writes)

        def ts(eng, out, in0, s1, s2, o0, o1, reads, writes):
            if o1 is None:
                op(eng, lambda e: e.tensor_scalar(out=out, in0=in0, scalar1=s1, scalar2=None, op0=o0), reads, writes)
            else:
                op(eng, lambda e: e.tensor_scalar(out=out, in0=in0, scalar1=s1, scalar2=s2, op0=o0, op1=o1), reads, writes)

        def stt(out, in0, sc, in1, o0, o1, reads, writes):
            op("dve", lambda e: e.scalar_tensor_tensor(out=out, in0=in0, scalar=sc, in1=in1, op0=o0, op1=o1), reads, writes)

        def act(out, in_, func, reads, writes, bias=None, scale=None):
            kw = {}
            if bias is not None:
                kw["bias"] = bias
            if scale is not None:
                kw["scale"] = scale
            op("act", lambda e: e.activation(out=out, in_=in_, func=func, **kw), reads, writes)

        def acopy(out, in_, reads, writes):
            op("act", lambda e: e.copy(out=out, in_=in_), reads, writes)

        def mm(out, pairs, reads, writes):
            def f(e):
                inst = None
                n = len(pairs)
                for i, (l, r) in enumerate(pairs):
                    inst = e.matmul(out, lhsT=l, rhs=r, start=(i == 0), stop=(i == n - 1))
                return inst
            op("pe", f, reads, writes)

        def mm1(out, l, r, start, stop, reads, writes):
            op("pe", lambda e: e.matmul(out, lhsT=l, rhs=r, start=start, stop=stop), reads, writes)

        def tr(out, in_, ident, reads, writes):
            op("pe", lambda e: e.transpose(out=out, in_=in_, identity=ident), reads, writes)

        def pcol(l, off):
            return pv[:, l * LP + off: l * LP + off + 1]

        def tsl(tc):
            return slice(tc * 512, (tc + 1) * 512)

        units = []

        cur = {"l": 0, "n": 0}

        def unit(specs, fn):
            keys = []
            for _ in specs:
                keys.append((cur["l"], cur["n"]))
                cur["n"] += 1
            units.append((specs, fn, keys))

        def wseg(dram_ap_rows_by_cols, kc):
            return dram_ap_rows_by_cols.rearrange("(k p) n -> p k n", p=128)

        def setup(_):
            fw.dma("sp", [(cst[:], cst_d)], writes=[Rcst])
            fw.dma("sp", [(pv[:], pv_d)], writes=[Rpv])
            fw.dma("sp", [(bA[:], bA_d.rearrange("l p n -> p l n"))], writes=[RbA])
            op("dve", lambda e: e.memset(wa2[:], 0.0), [], [Rwa2])
            fw.dma("sp", [(wa2[0:16], wa2_d.rearrange("l r n -> r l n"))], writes=[Rwa2])
            for (g_, rg_) in gq:
                op("dve", lambda e: e.memset(g_[:], 0.0), [], [rg_])
            op("dve", lambda e: e.tensor_copy(out=idb[:], in_=cst[:, C_ID:C_ID + 128]), [Rcst], [Ridb])
            op("dve", lambda e: e.memset(onesD[:], 1.0 / D), [], [RoD])
            op("dve", lambda e: e.memset(onesH[:], 1.0 / 128), [], [RoH])

        unit([], setup)

        def load_x(tb):
            def f(_):
                xt_, rx_ = xin[tb % 2]
                fw.dma("sp", [(xt_, x_d[tb * 128:(tb + 1) * 128, :])], writes=rx_)
                tc = tb // 4
                for half in range(2):
                    pb = (tb * 2 + half) % 4
                    for j in range(4):
                        k = half * 4 + j
                        tr(P[pb][:, j * 128:(j + 1) * 128], xt_[:, k * 128:(k + 1) * 128], cst[:, C_ID:C_ID + 128],
                           rx_ + [Rcst], [RP[pb]])
                    dst = x[:, half * 4:half * 4 + 4, tb * 128:(tb + 1) * 128]
                    src = P[pb][:].rearrange("p (a b) -> p a b", a=4)
                    if half == 0:
                        op("act", lambda e: e.copy(out=dst, in_=src), [RP[pb]], [RX[k_][tc] for k_ in range(half * 4, half * 4 + 4)])
                    else:
                        op("dve", lambda e: e.tensor_copy(out=dst, in_=src), [RP[pb]], [RX[k_][tc] for k_ in range(half * 4, half * 4 + 4)])
            return f

        for tb in range(16):
            unit([], load_x(tb))

        def rstd_from(pb):
            act(T0[:], P[pb][:], AF.Ln, [RP[pb]], [R0], bias=EPS, scale=1.0)
            act(T1[:], T0[:], AF.Exp, [R0], [R1], scale=-0.5)

        def prenorm(l, tc, goff):
            def f(_):
                for k in range(KC):
                    sq, rsq = sqb[k % 2]
                    act(sq[:], x[:, k, tsl(tc)], AF.Square, [RX[k][tc]], [rsq])
                    mm1(P[7][:], onesD[:], sq[:], k == 0, k == KC - 1, [rsq, RoD], [RP[7]])
                rstd_from(7)
                for k in range(KC):
                    stt(hT[:, k, :], x[:, k, tsl(tc)], pcol(l, goff + k), T1[:], ALU.mult, ALU.mult,
                        [RX[k][tc], R1, Rpv], [RH[k]])
            return f

        def postnorm(l, tc, goff, dbgname=None):
            def f(_):
                for k in range(KC):
                    sq, rsq = sqb[k % 2]
                    act(sq[:], ACC[:, k, :], AF.Square, [RA[k]], [rsq])
                    mm1(P[7][:], onesD[:], sq[:], k == 0, k == KC - 1, [rsq, RoD], [RP[7]])
                rstd_from(7)
                for k in range(KC):
                    stt(T3[:], ACC[:, k, :], pcol(l, goff + k), T1[:], ALU.mult, ALU.mult, [RA[k], R1, Rpv], [R3])
                    tt("dve", x[:, k, tsl(tc)], x[:, k, tsl(tc)], T3[:], ALU.add, [R3, RX[k][tc]], [RX[k][tc]])
                    if debug and dbgname is not None and l == 0:
                        fw.dma("sp", [(dbg_d[dbgname][k, :, tsl(tc)], x[:, k, tsl(tc)])], reads=[RX[k][tc]])
            return f

        def proj(pb, wv, c0, reads_w, n=128):
            mm(P[pb][0:n, :], [(wv[:, k, c0:c0 + n], hT[:, k, :]) for k in range(KC)], [reads_w] + RH, [RP[pb]])

        def proj_tok(pb, ncol, wv, c0, reads_w, blk, o0):
            mm(P[pb][:, o0:o0 + ncol], [(hT[:, k, blk * 128:(blk + 1) * 128], wv[:, k, c0:c0 + ncol]) for k in range(KC)],
               [reads_w] + RH, [RP[pb]])

        def rope_tables(b_tc):
            tc = b_tc

            def f(_):
                fw.dma("sp", [(posi[:], pos_d[0:1, tsl(tc)].partition_broadcast(128))], writes=[Rposi])
                op("dve", lambda e: e.tensor_copy(out=T0[:], in_=posi[:]), [Rposi], [R0])
                ts("dve", T2[:], T0[:], cst[:, C_INVF:C_INVF + 1], None, ALU.mult, None, [R0, Rcst], [R2])
                ts("dve", kint[:], T2[:], 1.0 / (2 * math.pi), None, ALU.mult, None, [R2], [Rkint])
                op("dve", lambda e: e.tensor_copy(out=T0[:], in_=kint[:]), [Rkint], [R0])
                C1 = 6.28125
                C2 = 2 * math.pi - C1
                stt(T2[:], T0[:], -C1, T2[:], ALU.mult, ALU.add, [R0, R2], [R2])
                stt(T2[:], T0[:], -C2, T2[:], ALU.mult, ALU.add, [R0, R2], [R2])
                ts("dve", T2[:], T2[:], math.pi, -math.pi, ALU.min, ALU.max, [R2], [R2])
                act(T3[:], T2[:], AF.Sin, [R2], [R3])
                ts("dve", sinT[:], T3[:], cst[:, C_SIGN:C_SIGN + 1], None, ALU.mult, None, [R3, Rcst], [Rsin])
                act(T3[:], T2[:], AF.Sin, [R2], [R3], scale=0.5)
                tt("dve", T3[:], T3[:], T3[:], ALU.mult, [R3], [R3])
                ts("dve", cosT[:], T3[:], -2.0, 1.0, ALU.mult, ALU.add, [R3], [Rcos])
            return f

        def reset_states(_):
            op("dve", lambda e: e.memset(zc[:], 0.0), [], [Rzc])
            op("dve", lambda e: e.memset(Sret[:], 0.0), [], RSr)
            op("dve", lambda e: e.memset(Sretb[:], 0.0), [], RSrb)
            op("dve", lambda e: e.memset(Sgla[:], 0.0), [], RSg)
            op("dve", lambda e: e.memset(Sglab[:], 0.0), [], RSgb)

        def conv_unit(l, tc, c):
            win = w_in_d[l]
            specs = [[(0, 8, 128, wseg(win[:, O_CU + c * 128:O_CU + (c + 1) * 128], 8)),
                      (1024, 8, 128, wseg(win[:, O_CB + c * 128:O_CB + (c + 1) * 128], 8)),
                      (2048, 8, 128, wseg(win[:, O_CC + c * 128:O_CC + (c + 1) * 128], 8))]]

            def f(pc):
                (sl, rs) = pc[0]
                wv = sl[:, 0:3072].rearrange("p (s k n) -> p s k n", s=3, k=8)
                proj(0, wv[:, 0], 0, rs)
                proj(1, wv[:, 1], 0, rs)
                proj(2, wv[:, 2], 0, rs)
                acopy(T0[:], P[0][:], [RP[0]], [R0])
                acopy(zb[:, 0:2], zc[:, c, :], [Rzc], [Rzb])
                tt("dve", zb[:, 2:514], P[2][:], T0[:], ALU.mult, [RP[2], R0], [Rzb])
                acopy(zc[:, c, :], zb[:, 512:514], [Rzb], [Rzc])
                ts("dve", T2[:], zb[:, 2:514], pcol(l, 32 + 8 + c), None, ALU.mult, None, [Rzb, Rpv], [R2])
                stt(T2[:], zb[:, 1:513], pcol(l, 32 + 4 + c), T2[:], ALU.mult, ALU.add, [Rzb, Rpv, R2], [R2])
                stt(T2[:], zb[:, 0:512], pcol(l, 32 + 0 + c), T2[:], ALU.mult, ALU.add, [Rzb, Rpv, R2], [R2])
                tt("dve", ybuf[0][:, c, :], P[1][:], T2[:], ALU.mult, [RP[1], R2], [RY[0][c]])
                if debug and l == 0:
                    fw.dma("sp", [(dbg_d["ya"][c, :, tsl(tc)], ybuf[0][:, c, :])], reads=[RY[0][c]])
            unit(specs, f)

        def rope(pb):
            tt("dve", T0[:], P[pb][:], cosT[:], ALU.mult, [RP[pb], Rcos], [R0])
            acopy(T5[:], P[pb][:], [RP[pb]], [R5])
            acopy(T2[0:64, :], T5[64:128, :], [R5], [R2])
            acopy(T2[64:128, :], T5[0:64, :], [R5], [R2])
            tt("dve", T2[:], T2[:], sinT[:], ALU.mult, [R2, Rsin], [R2])
            tt("dve", T3[:], T0[:], T2[:], ALU.add, [R0, R2], [R3])

        def headnorm(pbo, pbm, sg_t, sg_r, gcol, out_ap, out_reg):
            sq, rsq = sqb[0]
            act(sq[:], P[pbo][:], AF.Square, [RP[pbo]], [rsq])
            mm1(P[pbm][:], onesH[:], sq[:], True, True, [rsq, RoH], [RP[pbm]])
            rstd_from(pbm)
            tt("dve", T3[:], P[pbo][:], T1[:], ALU.mult, [RP[pbo], R1], [R3])
            stt(out_ap, T3[:], gcol, sg_t[:], ALU.mult, ALU.mult, [R3, Rpv, sg_r], [out_reg])

        GAM = [1.0 - 2.0 ** (-5.0 - h) for h in range(4)]

        def ret_unit(l, tc, h):
            win = w_in_d[l]
            specs = [[(i * 1024, 8, 128, wseg(win[:, o + h * 128:o + (h + 1) * 128], 8))
                      for i, o in enumerate((O_RQ, O_RK, O_RV, O_RG))]]

            def f(pc):
                (sl, rs) = pc[0]
                wv = sl[:, 0:4096].rearrange("p (s k n) -> p s k n", s=4, k=8)
                proj(0, wv[:, 0], 0, rs)
                proj(1, wv[:, 1], 0, rs)
                proj(2, wv[:, 3], 0, rs)
                for blk in range(4):
                    proj_tok(3, 128, wv[:, 2], 0, rs, blk, blk * 128)
                if KSTOP <= 1: return
                if 'KOPS' in os.environ: fw.limit = int(os.environ['KOPS'])
                rope(0)
                op("dve", lambda e: e.tensor_copy(out=qh[:], in_=T3[:]), [R3], [Rqh])
                tt("dve", qt[:].rearrange("p (a b) -> p a b", a=4), T3[:].rearrange("p (a b) -> p a b", a=4),
                   cst[:, C_QDEC + h * 128:C_QDEC + (h + 1) * 128].unsqueeze(1).to_broadcast([128, 4, 128]),
                   ALU.mult, [R3, Rcst], [Rqt])
                if KSTOP <= 2:
                    fw.limit = None
                    return
                rope(1)
                op("dve", lambda e: e.tensor_copy(out=kh[:], in_=T3[:]), [R3], [Rkh])
                if KSTOP <= 3: return
                act(T4[:], P[2][:], AF.Silu, [RP[2]], [R4])
                if KSTOP <= 4: return
                pv3 = P[3][:].rearrange("p (a b) -> p a b", a=4)
                op("dve", lambda e: e.tensor_reduce(out=nm[:], in_=pv3, axis=AX.X, op=ALU.add), [RP[3]], [Rnm])
                ts("dve", nm[:], nm[:], -1.0 / 128, None, ALU.mult, None, [Rnm], [Rnm])
                for blk in range(4):
                    ts("dve", vtok[:, blk, 0:128], P[3][:, blk * 128:(blk + 1) * 128], nm[:, blk:blk + 1], None,
                       ALU.add, None, [RP[3], Rnm], [Rvt])
                if KSTOP <= 5: return
                p4b = P[4][:].bitcast(BF16)
                for blk in range(4):
                    tr(p4b[:, blk * 128:(blk + 1) * 128], kh[:, blk * 128:(blk + 1) * 128], idb[:], [Rkh, Ridb], [RP[4]])
                ts("dve", ktok[:].rearrange("p a b -> p (a b)"), p4b[:, 0:512], cst[:, C_KDEC + h:C_KDEC + h + 1], None,
                   ALU.mult, None, [RP[4], Rcst], [Rkt])
                if KSTOP <= 6: return
                g128 = GAM[h] ** 128
                for blk in range(4):
                    bs = slice(blk * 128, (blk + 1) * 128)
                    mm(P[5][:, 0:128], [(kh[:, bs], qh[:, bs])], [Rkh, Rqh], [RP[5]])
                    tt("dve", stm[:], P[5][:, 0:128], cst[:, C_RMASK + h * 128:C_RMASK + (h + 1) * 128], ALU.mult,
                       [RP[5], Rcst], [Rstm])
                    mm(P[6][:, bs], [(vtok[:, blk, 0:128], stm[:]), (Sretb[:, h, :], qt[:, bs])],
                       [Rvt, Rstm, RSrb[h], Rqt], [RP[6]])
                    mm(P[7][:, 0:128], [(ktok[:, blk, :], vtok[:, blk, 0:128])], [Rkt, Rvt], [RP[7]])
                    stt(Sret[:, h, :], Sret[:, h, :], g128, P[7][:, 0:128], ALU.mult, ALU.add, [RSr[h], RP[7]], [RSr[h]])
                    acopy(Sretb[:, h, :], Sret[:, h, :], [RSr[h]], [RSrb[h]])
                if KSTOP <= 7: return
                headnorm(6, 7, T4, R4, pcol(l, 44 + h), ybuf[1][:, h, :], RY[1][h])
                if debug and l == 0:
                    fw.dma("sp", [(dbg_d["yb"][h, :, tsl(tc)], ybuf[1][:, h, :])], reads=[RY[1][h]])
            unit(specs, f)

        def gla_prelude(l, tc):
            win = w_in_d[l]
            specs = [[(0, 8, 128, wseg(win[:, O_GA:O_GA + 128], 8))]]

            def f(pc):
                (sl, rs) = pc[0]
                wv = sl[:, 0:1024].rearrange("p (k n) -> p k n", k=8)
                mm(P[0][:, :], [(wv[:, k, :], hT[:, k, :]) for k in range(KC)], [rs] + RH, [RP[0]])
                acopy(gdb[:], P[0][:, :], [RP[0]], [Rgdb])
                for half in range(2):
                    pb = 1 + half
                    for j in range(2):
                        blk = half * 2 + j
                        mm(P[pb][:, j * 256:(j + 1) * 256], [(gdb[:, blk * 128:(blk + 1) * 128], wa2[:, l, :])],
                           [Rgdb, Rwa2], [RP[pb]])
                    sv = spb[:, half * 2:half * 2 + 2, :]
                    tt("dve", sv, P[pb][:].rearrange("p (a b) -> p a b", a=2),
                       bA[:, l, :].unsqueeze(1).to_broadcast([128, 2, 256]), ALU.add, [RP[pb], RbA], [Rspb])
                    act(sv, sv, AF.Exp, [Rspb], [Rspb], scale=-1.0)
                    act(sv, sv, AF.Ln, [Rspb], [Rspb], bias=1.0, scale=1.0)
                for hp in range(2):
                    for blk in range(4):
                        mm(P[3 + hp][:, blk * 128:(blk + 1) * 128],
                           [(spb[:, blk, hp * 128:(hp + 1) * 128], cst[:, C_UNEG:C_UNEG + 128])], [Rspb, Rcst], [RP[3 + hp]])
            unit(specs, f)

        def gla_unit(l, tc, hp):
            win = w_in_d[l]
            specs = [[(0, 8, 128, wseg(win[:, O_GQ + hp * 128:O_GQ + (hp + 1) * 128], 8)),
                      (1024, 8, 128, wseg(win[:, O_GK + hp * 128:O_GK + (hp + 1) * 128], 8)),
                      (2048, 8, 256, wseg(win[:, O_GV + hp * 256:O_GV + (hp + 1) * 256], 8))],
                     [(0, 8, 256, wseg(win[:, O_GR + hp * 256:O_GR + (hp + 1) * 256], 8))]]

            def f(pc):
                (sl, rs), (sl2, rs2) = pc
                wq = sl[:, 0:1024].rearrange("p (k n) -> p k n", k=8)
                wk = sl[:, 1024:2048].rearrange("p (k n) -> p k n", k=8)
                wvv = sl[:, 2048:4096].rearrange("p (k n) -> p k n", k=8)
                wr = sl2[:, 0:2048].rearrange("p (k n) -> p k n", k=8)
                pc_ = 3 + hp
                act(T0[:], P[pc_][:], AF.Exp, [RP[pc_]], [R0])
                act(T2[:], P[pc_][:], AF.Exp, [RP[pc_]], [R2], scale=-1.0)
                proj(0, wq, 0, rs)
                proj(1, wk, 0, rs)
                for e_ in range(2):
                    ps = slice(64 * e_, 64 * e_ + 64)
                    stt(gq[e_][0][ps, :], P[0][ps, :], 0.125, T0[ps, :], ALU.mult, ALU.mult, [RP[0], R0], [gq[e_][1]])
                tt("dve", kh[:], P[1][:], T2[:], ALU.mult, [RP[1], R2], [Rkh])
                for half, pb in ((0, 2), (1, 5)):
                    for j in range(2):
                        proj_tok(pb, 256, wvv, 0, rs, half * 2 + j, j * 256)
                    acopy(vtok[:, half * 2:half * 2 + 2, :], P[pb][:].rearrange("p (a b) -> p a b", a=2), [RP[pb]], [Rvt])
                p6b = P[6][:].bitcast(BF16)
                for blk in range(4):
                    tr(p6b[:, blk * 128:(blk + 1) * 128], kh[:, blk * 128:(blk + 1) * 128], idb[:], [Rkh, Ridb], [RP[6]])
                acopy(ktok[:].rearrange("p a b -> p (a b)"), p6b[:, 0:512], [RP[6]], [Rkt])
                proj(7, wr, 0, rs2)
                act(T4[:], P[7][:], AF.Silu, [RP[7]], [R4])
                proj(5, wr, 128, rs2)
                act(T5[:], P[5][:], AF.Silu, [RP[5]], [R5])
                for blk in range(4):
                    bs = slice(blk * 128, (blk + 1) * 128)
                    for e_ in range(2):
                        gq_, rgq_ = gq[e_]
                        mm(P[0][:, 0:128], [(kh[:, bs], gq_[:, bs])], [Rkh, rgq_], [RP[0]])
                        tt("dve", stm[:], P[0][:, 0:128], cst[:, C_CAUS:C_CAUS + 128], ALU.mult, [RP[0], Rcst], [Rstm])
                        mm(P[1 + e_][:, bs], [(vtok[:, blk, e_ * 128:(e_ + 1) * 128], stm[:]), (Sglab[:, hp, :], gq_[:, bs])],
                           [Rvt, Rstm, RSgb[hp], rgq_], [RP[1 + e_]])
                    for e_ in range(2):
                        mm(P[6][:, e_ * 128:(e_ + 1) * 128], [(ktok[:, blk, :], vtok[:, blk, e_ * 128:(e_ + 1) * 128])],
                           [Rkt, Rvt], [RP[6]])
                    for e_ in range(2):
                        ps = slice(64 * e_, 64 * e_ + 64)
                        tt("dve", tS[ps, :], P[6][ps, e_ * 128:(e_ + 1) * 128], Sgla[ps, hp, :], ALU.add, [RP[6], RSg[hp]], [RtS])
                    ts("dve", Sgla[:, hp, :], tS[:], T0[:, blk * 128 + 127:blk * 128 + 128], None, ALU.mult, None,
                       [RtS, R0], [RSg[hp]])
                    acopy(Sglab[:, hp, :], Sgla[:, hp, :], [RSg[hp]], [RSgb[hp]])
                for e_, (sg_t, sg_r) in enumerate(((T4, R4), (T5, R5))):
                    hh = 2 * hp + e_
                    headnorm(1 + e_, 7, sg_t, sg_r, pcol(l, 48 + hh), ybuf[2][:, hh, :], RY[2][hh])
                    if debug and l == 0:
                        fw.dma("sp", [(dbg_d["yc"][hh, :, tsl(tc)], ybuf[2][:, hh, :])], reads=[RY[2][hh]])
            unit(specs, f)

        def merge_unit(l, tc, i):
            win = w_in_d[l]
            c0 = O_GATE + i * 1024
            specs = [[(0, 8, 512, wseg(win[:, c0:c0 + 512], 8))],
                     [(0, 8, 512, wseg(win[:, c0 + 512:c0 + 1024], 8))],
                     [(0, 4, 1024, wseg(wbr_d[i][l], 4))]]

            def f(pc):
                (slb, rsb) = pc[2]
                wb_ = slb[:, 0:4096].rearrange("p (k n) -> p k n", k=4)
                for half in range(2):
                    (sl, rs) = pc[half]
                    wg_ = sl[:, 0:4096].rearrange("p (k n) -> p k n", k=8)
                    for j in range(4):
                        oc = half * 4 + j
                        pa, pb = (0, 1) if j % 2 == 0 else (2, 3)
                        proj(pa, wg_, j * 128, rs)
                        mm(P[pb][:], [(wb_[:, kk, oc * 128:(oc + 1) * 128], ybuf[i][:, kk, :]) for kk in range(4)],
                           [rsb] + RY[i], [RP[pb]])
                        tw, rw = (T0, R0) if j % 2 == 0 else (T2, R2)
                        act(tw[:], P[pa][:], AF.Tanh, [RP[pa]], [rw], scale=0.5)
                        if i == 0:
                            stt(ACC[:, oc, :], tw[:], 1.0, P[pb][:], ALU.add, ALU.mult, [rw, RP[pb]], [RA[oc]])
                        else:
                            stt(tw[:], tw[:], 1.0, P[pb][:], ALU.add, ALU.mult, [rw, RP[pb]], [rw])
                            tt("dve", ACC[:, oc, :], ACC[:, oc, :], tw[:], ALU.add, [RA[oc], rw], [RA[oc]])
                            if i == 2:
                                op("act", lambda e: e.mul(out=mrg[:, oc, :], in_=ACC[:, oc, :], mul=0.5), [RA[oc]], [RM[oc]])
            unit(specs, f)

        def out_unit(l, tc, half):
            specs = [[(0, 8, 512, wseg(wout_d[l][:, half * 512:(half + 1) * 512], 8))]]

            def f(pc):
                (sl, rs) = pc[0]
                wo = sl[:, 0:4096].rearrange("p (k n) -> p k n", k=8)
                for j in range(4):
                    oc = half * 4 + j
                    pa = j % 4
                    mm(P[pa][:], [(wo[:, k, j * 128:(j + 1) * 128], mrg[:, k, :]) for k in range(KC)], [rs] + RM, [RP[pa]])
                    acopy(ACC[:, oc, :], P[pa][:], [RP[pa]], [RA[oc]])
            unit(specs, f)

        def ffn_unit(l, tc, g):
            NG = NF // 2
            specs = []
            if g < NG:
                c0 = g * 256
                specs.append([(0, 8, 256, wseg(wg_d[l][:, c0:c0 + 256], 8)),
                              (2048, 8, 256, wseg(wu_d[l][:, c0:c0 + 256], 8))])
            if g >= 1:
                cd = (g - 1) * 256
                specs.append([(0, 2, 1024, wseg(wd_d[l][cd:cd + 256, :], 2))])

            def f(pc):
                if g < NG:
                    (sl, rs) = pc[0]
                    wg_ = sl[:, 0:2048].rearrange("p (k n) -> p k n", k=8)
                    wu_ = sl[:, 2048:4096].rearrange("p (k n) -> p k n", k=8)
                    par = g % 2
                    for j in range(2):
                        pa, pb = (0, 1) if j == 0 else (2, 3)
                        proj(pa, wg_, j * 128, rs)
                        proj(pb, wu_, j * 128, rs)
                        tw, rw = (T0, R0) if j == 0 else (T2, R2)
                        act(tw[:], P[pa][:], AF.Silu, [RP[pa]], [rw])
                        tt("dve", actb[:, par, j, :], tw[:], P[pb][:], ALU.mult, [rw, RP[pb]], [RACT[par][j]])
                if g >= 1:
                    (sl2, rs2) = pc[-1]
                    wd_ = sl2[:, 0:2048].rearrange("p (k n) -> p k n", k=2)
                    gd = g - 1
                    pard = gd % 2
                    for oc in range(KC):
                        pa = 4 + oc % 3
                        mm(P[pa][:], [(wd_[:, j, oc * 128:(oc + 1) * 128], actb[:, pard, j, :]) for j in range(2)],
                           [rs2] + RACT[pard], [RP[pa]])
                        if gd == 0:
                            acopy(ACC[:, oc, :], P[pa][:], [RP[pa]], [RA[oc]])
                        else:
                            tt("dve", ACC[:, oc, :], ACC[:, oc, :], P[pa][:], ALU.add, [RA[oc], RP[pa]], [RA[oc]])
            unit(specs, f)

        ROUT = [Reg(), Reg()]

        def store_x(tb):
            def f(_):
                tc = tb // 4
                ot, ro = xin[tb % 2]
                for half in range(2):
                    pb = (tb * 2 + half) % 4
                    for j in range(4):
                        k = half * 4 + j
                        tr(P[pb][:, j * 128:(j + 1) * 128], x[:, k, tb * 128:(tb + 1) * 128], cst[:, C_ID:C_ID + 128],
                           [RX[k][tc], Rcst], [RP[pb]])
                    if half == 0:
                        acopy(ot[:, 0:512], P[pb][:], [RP[pb]], ro)
                    else:
                        op("dve", lambda e: e.tensor_copy(out=ot[:, 512:1024], in_=P[pb][:]), [RP[pb]], ro)
                fw.dma("sp", [(out_d[tb * 128:(tb + 1) * 128, :], ot)], reads=ro, owner=ROUT[tb % 2])
            return f

        for l in range(depth):
            unit([], reset_states)
            for tc in range(ntc):
                cur["l"] = l
                cur["n"] = 0
                unit([], prenorm(l, tc, 0))
                if "A" in phases:
                    for c in range(4):
                        conv_unit(l, tc, c)
                if "B" in phases:
                    if os.environ.get('KROPE', '1') == '1':
                        unit([], rope_tables(tc))
                    for h in range(4):
                        ret_unit(l, tc, h)
                if "C" in phases:
                    gla_prelude(l, tc)
                    for hp in range(2):
                        gla_unit(l, tc, hp)
                if "D" in phases:
                    for i in range(3):
                        merge_unit(l, tc, i)
                if "E" in phases:
                    for half in range(2):
                        out_unit(l, tc, half)
                    unit([], postnorm(l, tc, 8, "xmix"))
                if "F" in phases:
                    unit([], prenorm(l, tc, 16))
                    for g in range(NF // 2 + 1):
                        ffn_unit(l, tc, g)
                    unit([], postnorm(l, tc, 24, "xffn"))
        for tb in range(16):
            unit([], store_x(tb))

        pieces = []
        upieces = []
        for ui, (specs, fn, keys) in enumerate(units):
            idxs = []
            for segs, key in zip(specs, keys):
                idxs.append(len(pieces))
                pieces.append((ui, segs, key))
            upieces.append(idxs)
        GSZ = 6
        first = {}
        for (ui, segs, key) in pieces:
            first.setdefault(key, segs)
        npc = max([k[1] for k in first] + [0]) + 1
        scr = [nc.dram_tensor("scr%d" % l, [npc, 128, SLOT], BF16).ap() for l in range(max(depth, 1))]
        ngrp = (npc + GSZ - 1) // GSZ
        RG = [[Reg() for _ in range(ngrp)] for _ in range(max(depth, 1))]
        for l in range(depth):
            for g in range(ngrp):
                csegs = []
                for j in range(g * GSZ, min(npc, (g + 1) * GSZ)):
                    if (l, j) not in first:
                        continue
                    for (off, kc, n, src) in first[(l, j)]:
                        dst = scr[l][j][:, off:off + kc * n].rearrange("p (k n) -> p k n", k=kc)
                        csegs.append((dst, src))
                if csegs:
                    fw.dma("pool", csegs, writes=[RG[l][g]])
        nextp = 0

        def issue_loads(cur_unit):
            nonlocal nextp
            while nextp < len(pieces):
                if nextp >= NSLOT and pieces[nextp - NSLOT][0] >= cur_unit:
                    break
                ui, segs, (l_, j_) = pieces[nextp]
                s_ = nextp % NSLOT
                L = max(off + kc * n for (off, kc, n, src) in segs)
                fw.dma("sp", [(slots[s_][:, 0:L], scr[l_][j_][:, 0:L])], reads=[RG[l_][j_ // GSZ]], writes=[RS[s_]])
                nextp += 1

        for ui, (specs, fn, keys) in enumerate(units):
            issue_loads(ui)
            for pi_ in upieces[ui]:
                assert pi_ < nextp, "piece not loaded: raise NSLOT"
            fn([(slots[pi_ % NSLOT], RS[pi_ % NSLOT]) for pi_ in upieces[ui]])

        fw.wait_all("sp", xin[0][1] + xin[1][1])
        if debug:
            fw.wait_all("sp", [r for rr in RY for r in rr] + [r for rr in RX for r in rr])
    return nc


def _consts():
    c = np.zeros((128, NCST), np.float64)
    p = np.arange(128)
    half = 64
    invf = 10000.0 ** (-(np.arange(half, dtype=np.float32) / np.float32(half)))
    c[:, C_INVF] = np.concatenate([invf, invf]).astype(np.float32)
    c[:, C_SIGN] = np.where(p < 64, -1.0, 1.0)
    i = np.arange(128)
    for h in range(4):
        lg = math.log(1.0 - 2.0 ** (-5.0 - h))
        c[:, C_KDEC + h] = np.exp(lg * (127 - p))
        diff = i[None, :] - p[:, None]
        c[:, C_RMASK + h * 128:C_RMASK + (h + 1) * 128] = np.where(diff >= 0, np.exp(lg * np.maximum(diff, 0)), 0.0) * 128 ** -0.5
        c[:, C_QDEC + h * 128:C_QDEC + (h + 1) * 128] = (np.exp(lg * (i + 1.0)) * 128 ** -0.5)[None, :]
    c[:, C_CAUS:C_CAUS + 128] = (i[None, :] >= p[:, None]).astype(np.float64)
    c[:, C_UNEG:C_UNEG + 128] = np.where(p[:, None] <= i[None, :], -1.0 / 16.0, 0.0)
    c[:, C_ID:C_ID + 128] = np.eye(128)
    return c.astype(np.float32)


_CACHE = {}


def kernel(x, positions, norm_mix_pre, w_in, conv_w, ret_gn_w, gla_w_a2, gla_b_a, gla_gn_w,
           w_branch_a, w_branch_b, w_branch_c, w_out, norm_mix_post, norm_ffn_pre,
           w_ffn_gate, w_ffn_up, w_ffn_down, norm_ffn_post, _debug=False, _dd=DEPTH, _ncores=8):
    f32 = lambda a: np.ascontiguousarray(np.asarray(a, dtype=np.float32))
    x = f32(x)
    positions = np.ascontiguousarray(np.asarray(positions, dtype=np.int32))
    pv = np.zeros((128, _dd * LP), np.float32)
    for l in range(_dd):
        b = l * LP
        for j, g in enumerate((norm_mix_pre, norm_mix_post, norm_ffn_pre, norm_ffn_post)):
            pv[:, b + 8 * j:b + 8 * j + 8] = f32(g)[l].reshape(8, 128).T
        pv[:, b + 32:b + 44] = f32(conv_w)[l].reshape(3, 4, 128).transpose(2, 0, 1).reshape(128, 12)
        pv[:, b + 44:b + 48] = f32(ret_gn_w)[l].reshape(4, 128).T
        pv[:, b + 48:b + 52] = f32(gla_gn_w)[l].reshape(4, 128).T
    bA = np.ascontiguousarray(np.broadcast_to(f32(gla_b_a)[:_dd, None, :], (_dd, 128, 256)))
    shared = {
        "w_in": f32(w_in[:_dd]), "gla_w_a2": f32(gla_w_a2[:_dd]), "bA": bA,
        "w_branch_a": f32(w_branch_a[:_dd]), "w_branch_b": f32(w_branch_b[:_dd]), "w_branch_c": f32(w_branch_c[:_dd]),
        "w_out": f32(w_out[:_dd]), "w_ffn_gate": f32(w_ffn_gate[:_dd]), "w_ffn_up": f32(w_ffn_up[:_dd]),
        "w_ffn_down": f32(w_ffn_down[:_dd]), "pv": pv, "cst": _consts(),
    }
    key = bool(_debug)
    if key not in _CACHE:
        _CACHE[key] = build_program(debug=_debug)
    nc = _CACHE[key]
    in_maps = []
    for b in range(_ncores):
        m = dict(shared)
        m["x"] = x[b]
        m["pos"] = positions[b:b + 1]
        in_maps.append(m)
    res = run_bass_kernel_spmd(nc, in_maps, core_ids=list(range(_ncores)))
    out = np.stack([np.asarray(r["out"], dtype=np.float32) for r in res.results], axis=0)
    if _debug:
        kernel.last = res.results
    return out
```

```python
import math
import os
KSTOP = int(os.environ.get('KSTOP', '99'))
from contextlib import ExitStack

import numpy as np
import concourse.bass as bass
import concourse.mybir as mybir
from concourse.bass_utils import run_bass_kernel_spmd

F32 = mybir.dt.float32
BF16 = mybir.dt.bfloat16
I32 = mybir.dt.int32
ALU = mybir.AluOpType
AF = mybir.ActivationFunctionType
AX = mybir.AxisListType

D = 1024
T = 2048
DEPTH = 2
NTC = 4
KC = 8
IN_COLS = 8208
DFF = 2816
NF = 22
EPS = 1e-6
NSLOT = 5
SLOT = 4096
LP = 52

O_CU, O_CB, O_CC = 0, 512, 1024
O_RQ, O_RK, O_RV, O_RG = 1536, 2048, 2560, 3072
O_GQ, O_GK, O_GV, O_GR, O_GA = 3584, 3840, 4096, 4608, 5120
O_GATE = 5136

C_INVF, C_SIGN, C_KDEC, C_RMASK, C_QDEC, C_CAUS, C_UNEG, C_ID = 0, 1, 2, 6, 518, 1030, 1158, 1286
NCST = 1414


class Eng:
    def __init__(self, name, e, sem):
        self.name, self.e, self.sem = name, e, sem
        self.cnt = 0
        self.seen = {}


class Reg:
    __slots__ = ("w", "rs", "dsem", "dcnt", "excl")

    def __init__(self, excl=False):
        self.excl = excl
        self.w = None
        self.rs = {}
        self.dsem = None
        self.dcnt = 0


class FW:
    def __init__(self, nc, stack):
        self.nc = nc
        self.stack = stack
        self.E = {}
        for name, e in (("pe", nc.tensor), ("act", nc.scalar), ("dve", nc.vector),
                        ("pool", nc.gpsimd), ("sp", nc.sync)):
            sem = stack.enter_context(nc.semaphore("s_" + name))
            self.E[name] = Eng(name, e, sem)
        self.nsem = 5
        self.limit = None

    def sbuf(self, name, shape, dt):
        return self.stack.enter_context(self.nc.sbuf_tensor("sb_" + name, list(shape), dt))

    def psum(self, name, shape, dt):
        return self.stack.enter_context(self.nc.psum_tensor(name, list(shape), dt))

    def newsem(self):
        self.nsem += 1
        return self.stack.enter_context(self.nc.semaphore("d%d" % self.nsem))

    def _need(self, E, tok, need, same_ok):
        if tok is None:
            return
        if tok[0] == "e":
            F, n = tok[1], tok[2]
            if F is E and E.name in ("pe", "sp"):
                return
            key = F.name
            sem = F.sem
        else:
            _, sem, n, sid = tok
            key = ("d", sid)
        if E.seen.get(key, 0) >= n:
            return
        if need.get(key, (None, 0))[1] < n:
            need[key] = (sem, n)

    def _waits(self, E, reads, writes):
        need = {}
        for r in reads:
            self._need(E, r.w, need, False)
            if r.excl:
                for t in r.rs.values():
                    if t[0] == "e" and t[1] is E:
                        continue
                    self._need(E, t, need, True)
        for w in writes:
            self._need(E, w.w, need, False)
            for t in w.rs.values():
                self._need(E, t, need, True)
        for key, (sem, n) in need.items():
            E.e.wait_ge(sem, n)
            E.seen[key] = n

    def op(self, eng, fn, reads=(), writes=()):
        if self.limit is not None:
            if self.limit <= 0:
                return None
            self.limit -= 1
        E = self.E[eng]
        self._waits(E, reads, writes)
        inst = fn(E.e)
        E.cnt += 1
        inst.then_inc(E.sem, 1)
        tok = ("e", E, E.cnt)
        for r in reads:
            r.rs[E.name] = tok
        for w in writes:
            w.w = tok
            w.rs = {}
        return inst

    def dma(self, q, segs, reads=(), writes=(), owner=None):
        E = self.E[q]
        self._waits(E, reads, writes)
        owner = owner if owner is not None else (writes[0] if writes else reads[0])
        if owner.dsem is None:
            owner.dsem = self.newsem()
        for (o, i) in segs:
            inst = E.e.dma_start(out=o, in_=i)
            owner.dcnt += 16
            inst.then_inc(owner.dsem, 16)
        tok = ("d", owner.dsem, owner.dcnt, id(owner))
        for r in reads:
            r.rs[("d", id(owner))] = tok
        for w in writes:
            w.w = tok
            w.rs = {}

    def wait_all(self, eng, regs):
        self._waits(self.E[eng], [], regs)


def build_program(debug=False, depth=DEPTH, ntc=NTC, phases="ABCDEFG"):
    nc = bass.Bass("TRN2", target_bir_lowering=False)

    def din(name, shape, dt=F32):
        return nc.dram_tensor(name, list(shape), dt, kind="ExternalInput").ap()

    dd = max(depth, 1)
    x_d = din("x", [T, D])
    pos_d = din("pos", [1, T], I32)
    w_in_d = din("w_in", [dd, D, IN_COLS])
    wa2_d = din("gla_w_a2", [dd, 16, 256])
    bA_d = din("bA", [dd, 128, 256])
    wbr_d = [din("w_branch_" + n, [dd, 512, D]) for n in "abc"]
    wout_d = din("w_out", [dd, D, D])
    wg_d = din("w_ffn_gate", [dd, D, DFF])
    wu_d = din("w_ffn_up", [dd, D, DFF])
    wd_d = din("w_ffn_down", [dd, DFF, D])
    pv_d = din("pv", [128, dd * LP])
    cst_d = din("cst", [128, NCST])
    out_d = nc.dram_tensor("out", [T, D], F32, kind="ExternalOutput").ap()
    dbg_d = {}
    if debug:
        for n in ("ya", "yb", "yc"):
            dbg_d[n] = nc.dram_tensor("dbg_" + n, [4, 128, T], BF16, kind="ExternalOutput").ap()
        for n in ("xmix", "xffn"):
            dbg_d[n] = nc.dram_tensor("dbg_" + n, [8, 128, T], F32, kind="ExternalOutput").ap()

    st = ExitStack()
    with st:
        fw = FW(nc, st)
        op = fw.op

        x = fw.sbuf("x", [128, KC, T], F32)
        RX = [[Reg() for _ in range(NTC)] for _ in range(KC)]
        hT = fw.sbuf("hT", [128, KC, 512], BF16)
        RH = [Reg() for _ in range(KC)]
        ybuf = [fw.sbuf("y%d" % i, [128, 4, 512], BF16) for i in range(3)]
        RY = [[Reg() for _ in range(4)] for _ in range(3)]
        ACC = fw.sbuf("acc", [128, KC, 512], F32)
        RA = [Reg() for _ in range(KC)]
        mrg = fw.sbuf("mrg", [128, KC, 512], BF16)
        RM = [Reg() for _ in range(KC)]
        actb = fw.sbuf("actb", [128, 2, 2, 512], BF16)
        RACT = [[Reg() for _ in range(2)] for _ in range(2)]
        slots = [fw.sbuf("slot%d" % i, [128, SLOT], BF16) for i in range(NSLOT)]
        RS = [Reg() for _ in range(NSLOT)]
        P = [fw.psum("ps%d" % i, [128, 512], F32) for i in range(8)]
        RP = [Reg(excl=True) for _ in range(8)]

        def wt(name, shape=(128, 512), dt=F32):
            return fw.sbuf(name, shape, dt), Reg()

        T0, R0 = wt("T0"); T1, R1 = wt("T1"); T2, R2 = wt("T2"); T3, R3 = wt("T3")
        T4, R4 = wt("T4"); T5, R5 = wt("T5")
        sqb = [wt("sqb%d" % i, dt=BF16) for i in range(2)]
        qh, Rqh = wt("qh", dt=BF16); qt, Rqt = wt("qt", dt=BF16); kh, Rkh = wt("kh", dt=BF16)
        vtok, Rvt = wt("vtok", (128, 4, 256), BF16)
        ktok, Rkt = wt("ktok", (128, 4, 128), BF16)
        stm, Rstm = wt("stm", (128, 128), BF16)
        stmA = [(stm, Rstm), wt("stm2", (128, 128), BF16)]
        zb, Rzb = wt("zb", (128, 514), F32)
        zc, Rzc = wt("zc", (128, 4, 2), F32)
        spb, Rspb = wt("spb", (128, 4, 256), F32)
        gdb, Rgdb = wt("gdb", (128, 512), F32)
        gq = [wt("gq%d" % i, dt=BF16) for i in range(2)]
        nm, Rnm = wt("nm", (128, 4), F32)
        tS, RtS = wt("tS", (128, 128), F32)
        Sret, RSr = wt("Sret", (128, 4, 128), F32)
        Sretb, RSrb = wt("Sretb", (128, 4, 128), BF16)
        RSr = [Reg() for _ in range(4)]; RSrb = [Reg() for _ in range(4)]
        Sgla, RSg = wt("Sgla", (128, 2, 128), F32)
        Sglab, RSgb = wt("Sglab", (128, 2, 128), BF16)
        RSg = [Reg() for _ in range(2)]; RSgb = [Reg() for _ in range(2)]
        cosT, Rcos = wt("cosT"); sinT, Rsin = wt("sinT")
        posi, Rposi = wt("posi", (128, 512), I32)
        kint, Rkint = posi, Rposi
        cst, Rcst = wt("cst", (128, NCST), F32)
        pv, Rpv = wt("pv", (128, dd * LP), F32)
        bA, RbA = wt("bA", (128, dd, 256), F32)
        wa2, Rwa2 = wt("wa2", (128, dd, 256), F32)
        idb, Ridb = wt("idb", (128, 128), BF16)
        onesD, RoD = wt("onesD", (128, 128), BF16)
        onesH, RoH = wt("onesH", (128, 128), BF16)
        xin = [(ACC[:, 0:2, :].rearrange("p a b -> p (a b)"), [RA[0], RA[1]]),
               (ACC[:, 2:4, :].rearrange("p a b -> p (a b)"), [RA[2], RA[3]])]

        DBG = Reg()

        def tt(eng, out, in0, in1, o, reads, writes):
            op(eng, lambda e: e.tensor_tensor(out=out, in0=in0, in1=in1, op=o), reads, writes)

        def ts(eng, out, in0, s1, s2, o0, o1, reads, writes):
            if o1 is None:
                op(eng, lambda e: e.tensor_scalar(out=out, in0=in0, scalar1=s1, scalar2=None, op0=o0), reads, writes)
            else:
                op(eng, lambda e: e.tensor_scalar(out=out, in0=in0, scalar1=s1, scalar2=s2, op0=o0, op1=o1), reads, writes)

        def stt(out, in0, sc, in1, o0, o1, reads, writes):
            op("dve", lambda e: e.scalar_tensor_tensor(out=out, in0=in0, scalar=sc, in1=in1, op0=o0, op1=o1), reads, writes)

        def act(out, in_, func, reads, writes, bias=None, scale=None):
            kw = {}
            if bias is not None:
                kw["bias"] = bias
            if scale is not None:
                kw["scale"] = scale
            op("act", lambda e: e.activation(out=out, in_=in_, func=func, **kw), reads, writes)

        def acopy(out, in_, reads, writes):
            op("act", lambda e: e.copy(out=out, in_=in_), reads, writes)

        def mm(out, pairs, reads, writes):
            def f(e):
                inst = None
                n = len(pairs)
                for i, (l, r) in enumerate(pairs):
                    inst = e.matmul(out, lhsT=l, rhs=r, start=(i == 0), stop=(i == n - 1))
                return inst
            op("pe", f, reads, writes)

        def mm1(out, l, r, start, stop, reads, writes):
            op("pe", lambda e: e.matmul(out, lhsT=l, rhs=r, start=start, stop=stop), reads, writes)

        def tr(out, in_, ident, reads, writes):
            op("pe", lambda e: e.transpose(out=out, in_=in_, identity=ident), reads, writes)

        def pcol(l, off):
            return pv[:, l * LP + off: l * LP + off + 1]

        def tsl(tc):
            return slice(tc * 512, (tc + 1) * 512)

        units = []

        cur = {"l": 0, "n": 0}

        def unit(specs, fn):
            keys = []
            for _ in specs:
                keys.append((cur["l"], cur["n"]))
                cur["n"] += 1
            units.append((specs, fn, keys))

        def wseg(dram_ap_rows_by_cols, kc):
            return dram_ap_rows_by_cols.rearrange("(k p) n -> p k n", p=128)

        def setup(_):
            fw.dma("sp", [(cst[:], cst_d)], writes=[Rcst])
            fw.dma("sp", [(pv[:], pv_d)], writes=[Rpv])
            fw.dma("sp", [(bA[:], bA_d.rearrange("l p n -> p l n"))], writes=[RbA])
            op("dve", lambda e: e.memset(wa2[:], 0.0), [], [Rwa2])
            fw.dma("sp", [(wa2[0:16], wa2_d.rearrange("l r n -> r l n"))], writes=[Rwa2])
            for (g_, rg_) in gq:
                op("dve", lambda e: e.memset(g_[:], 0.0), [], [rg_])
            op("dve", lambda e: e.tensor_copy(out=idb[:], in_=cst[:, C_ID:C_ID + 128]), [Rcst], [Ridb])
            op("dve", lambda e: e.memset(onesD[:], 1.0 / D), [], [RoD])
            op("dve", lambda e: e.memset(onesH[:], 1.0 / 128), [], [RoH])

        unit([], setup)

        def load_x(tb):
            def f(_):
                xt_, rx_ = xin[tb % 2]
                fw.dma("sp", [(xt_, x_d[tb * 128:(tb + 1) * 128, :])], writes=rx_)
                tc = tb // 4
                for half in range(2):
                    pb = (tb * 2 + half) % 4
                    for j in range(4):
                        k = half * 4 + j
                        tr(P[pb][:, j * 128:(j + 1) * 128], xt_[:, k * 128:(k + 1) * 128], cst[:, C_ID:C_ID + 128],
                           rx_ + [Rcst], [RP[pb]])
                    dst = x[:, half * 4:half * 4 + 4, tb * 128:(tb + 1) * 128]
                    src = P[pb][:].rearrange("p (a b) -> p a b", a=4)
                    if half == 0:
                        op("act", lambda e: e.copy(out=dst, in_=src), [RP[pb]], [RX[k_][tc] for k_ in range(half * 4, half * 4 + 4)])
                    else:
                        op("dve", lambda e: e.tensor_copy(out=dst, in_=src), [RP[pb]], [RX[k_][tc] for k_ in range(half * 4, half * 4 + 4)])
            return f

        for tb in range(16):
            unit([], load_x(tb))

        def rstd_from(pb):
            act(T0[:], P[pb][:], AF.Ln, [RP[pb]], [R0], bias=EPS, scale=1.0)
            act(T1[:], T0[:], AF.Exp, [R0], [R1], scale=-0.5)

        def prenorm(l, tc, goff):
            def f(_):
                for k in range(KC):
                    sq, rsq = sqb[k % 2]
                    act(sq[:], x[:, k, tsl(tc)], AF.Square, [RX[k][tc]], [rsq])
                    mm1(P[7][:], onesD[:], sq[:], k == 0, k == KC - 1, [rsq, RoD], [RP[7]])
                rstd_from(7)
                for k in range(KC):
                    stt(hT[:, k, :], x[:, k, tsl(tc)], pcol(l, goff + k), T1[:], ALU.mult, ALU.mult,
                        [RX[k][tc], R1, Rpv], [RH[k]])
            return f

        def postnorm(l, tc, goff, dbgname=None):
            def f(_):
                for k in range(KC):
                    sq, rsq = sqb[k % 2]
                    act(sq[:], ACC[:, k, :], AF.Square, [RA[k]], [rsq])
                    mm1(P[7][:], onesD[:], sq[:], k == 0, k == KC - 1, [rsq, RoD], [RP[7]])
                rstd_from(7)
                for k in range(KC):
                    stt(T3[:], ACC[:, k, :], pcol(l, goff + k), T1[:], ALU.mult, ALU.mult, [RA[k], R1, Rpv], [R3])
                    tt("dve", x[:, k, tsl(tc)], x[:, k, tsl(tc)], T3[:], ALU.add, [R3, RX[k][tc]], [RX[k][tc]])
                    if debug and dbgname is not None and l == 0:
                        fw.dma("sp", [(dbg_d[dbgname][k, :, tsl(tc)], x[:, k, tsl(tc)])], reads=[RX[k][tc]])
            return f

        def proj(pb, wv, c0, reads_w, n=128):
            mm(P[pb][0:n, :], [(wv[:, k, c0:c0 + n], hT[:, k, :]) for k in range(KC)], [reads_w] + RH, [RP[pb]])

        def proj_tok(pb, ncol, wv, c0, reads_w, blk, o0):
            mm(P[pb][:, o0:o0 + ncol], [(hT[:, k, blk * 128:(blk + 1) * 128], wv[:, k, c0:c0 + ncol]) for k in range(KC)],
               [reads_w] + RH, [RP[pb]])

        def rope_tables(b_tc):
            tc = b_tc

            def f(_):
                fw.dma("sp", [(posi[:], pos_d[0:1, tsl(tc)].partition_broadcast(128))], writes=[Rposi])
                op("dve", lambda e: e.tensor_copy(out=T0[:], in_=posi[:]), [Rposi], [R0])
                ts("dve", T2[:], T0[:], cst[:, C_INVF:C_INVF + 1], None, ALU.mult, None, [R0, Rcst], [R2])
                ts("dve", kint[:], T2[:], 1.0 / (2 * math.pi), None, ALU.mult, None, [R2], [Rkint])
                op("dve", lambda e: e.tensor_copy(out=T0[:], in_=kint[:]), [Rkint], [R0])
                C1 = 6.28125
                C2 = 2 * math.pi - C1
                stt(T2[:], T0[:], -C1, T2[:], ALU.mult, ALU.add, [R0, R2], [R2])
                stt(T2[:], T0[:], -C2, T2[:], ALU.mult, ALU.add, [R0, R2], [R2])
                ts("dve", T2[:], T2[:], math.pi, -math.pi, ALU.min, ALU.max, [R2], [R2])
                act(T3[:], T2[:], AF.Sin, [R2], [R3])
                ts("dve", sinT[:], T3[:], cst[:, C_SIGN:C_SIGN + 1], None, ALU.mult, None, [R3, Rcst], [Rsin])
                act(T3[:], T2[:], AF.Sin, [R2], [R3], scale=0.5)
                tt("dve", T3[:], T3[:], T3[:], ALU.mult, [R3], [R3])
                ts("dve", cosT[:], T3[:], -2.0, 1.0, ALU.mult, ALU.add, [R3], [Rcos])
            return f

        def reset_states(_):
            op("dve", lambda e: e.memset(zc[:], 0.0), [], [Rzc])
            op("dve", lambda e: e.memset(Sret[:], 0.0), [], RSr)
            op("dve", lambda e: e.memset(Sretb[:], 0.0), [], RSrb)
            op("dve", lambda e: e.memset(Sgla[:], 0.0), [], RSg)
            op("dve", lambda e: e.memset(Sglab[:], 0.0), [], RSgb)

        def conv_unit(l, tc, c):
            win = w_in_d[l]
            specs = [[(0, 8, 128, wseg(win[:, O_CU + c * 128:O_CU + (c + 1) * 128], 8)),
                      (1024, 8, 128, wseg(win[:, O_CB + c * 128:O_CB + (c + 1) * 128], 8)),
                      (2048, 8, 128, wseg(win[:, O_CC + c * 128:O_CC + (c + 1) * 128], 8))]]

            def f(pc):
                (sl, rs) = pc[0]
                wv = sl[:, 0:3072].rearrange("p (s k n) -> p s k n", s=3, k=8)
                proj(0, wv[:, 0], 0, rs)
                proj(1, wv[:, 1], 0, rs)
                proj(2, wv[:, 2], 0, rs)
                acopy(T0[:], P[0][:], [RP[0]], [R0])
                acopy(zb[:, 0:2], zc[:, c, :], [Rzc], [Rzb])
                tt("dve", zb[:, 2:514], P[2][:], T0[:], ALU.mult, [RP[2], R0], [Rzb])
                acopy(zc[:, c, :], zb[:, 512:514], [Rzb], [Rzc])
                ts("dve", T2[:], zb[:, 2:514], pcol(l, 32 + 8 + c), None, ALU.mult, None, [Rzb, Rpv], [R2])
                stt(T2[:], zb[:, 1:513], pcol(l, 32 + 4 + c), T2[:], ALU.mult, ALU.add, [Rzb, Rpv, R2], [R2])
                stt(T2[:], zb[:, 0:512], pcol(l, 32 + 0 + c), T2[:], ALU.mult, ALU.add, [Rzb, Rpv, R2], [R2])
                tt("dve", ybuf[0][:, c, :], P[1][:], T2[:], ALU.mult, [RP[1], R2], [RY[0][c]])
                if debug and l == 0:
                    fw.dma("sp", [(dbg_d["ya"][c, :, tsl(tc)], ybuf[0][:, c, :])], reads=[RY[0][c]])
            unit(specs, f)

        def rope(pb):
            tt("dve", T0[:], P[pb][:], cosT[:], ALU.mult, [RP[pb], Rcos], [R0])
            acopy(T5[:], P[pb][:], [RP[pb]], [R5])
            acopy(T2[0:64, :], T5[64:128, :], [R5], [R2])
            acopy(T2[64:128, :], T5[0:64, :], [R5], [R2])
            tt("dve", T2[:], T2[:], sinT[:], ALU.mult, [R2, Rsin], [R2])
            tt("dve", T3[:], T0[:], T2[:], ALU.add, [R0, R2], [R3])

        def headnorm(pbo, pbm, sg_t, sg_r, gcol, out_ap, out_reg):
            sq, rsq = sqb[0]
            act(sq[:], P[pbo][:], AF.Square, [RP[pbo]], [rsq])
            mm1(P[pbm][:], onesH[:], sq[:], True, True, [rsq, RoH], [RP[pbm]])
            rstd_from(pbm)
            tt("dve", T3[:], P[pbo][:], T1[:], ALU.mult, [RP[pbo], R1], [R3])
            stt(out_ap, T3[:], gcol, sg_t[:], ALU.mult, ALU.mult, [R3, Rpv, sg_r], [out_reg])

        GAM = [1.0 - 2.0 ** (-5.0 - h) for h in range(4)]

        def ret_unit(l, tc, h):
            win = w_in_d[l]
            specs = [[(i * 1024, 8, 128, wseg(win[:, o + h * 128:o + (h + 1) * 128], 8))
                      for i, o in enumerate((O_RQ, O_RK, O_RV, O_RG))]]

            def f(pc):
                (sl, rs) = pc[0]
                wv = sl[:, 0:4096].rearrange("p (s k n) -> p s k n", s=4, k=8)
                proj(0, wv[:, 0], 0, rs)
                proj(1, wv[:, 1], 0, rs)
                proj(2, wv[:, 3], 0, rs)
                for blk in range(4):
                    proj_tok(3, 128, wv[:, 2], 0, rs, blk, blk * 128)
                if KSTOP <= 1: return
                if 'KOPS' in os.environ: fw.limit = int(os.environ['KOPS'])
                rope(0)
                op("dve", lambda e: e.tensor_copy(out=qh[:], in_=T3[:]), [R3], [Rqh])
                tt("dve", qt[:].rearrange("p (a b) -> p a b", a=4), T3[:].rearrange("p (a b) -> p a b", a=4),
                   cst[:, C_QDEC + h * 128:C_QDEC + (h + 1) * 128].unsqueeze(1).to_broadcast([128, 4, 128]),
                   ALU.mult, [R3, Rcst], [Rqt])
                if KSTOP <= 2:
                    fw.limit = None
                    return
                rope(1)
                op("dve", lambda e: e.tensor_copy(out=kh[:], in_=T3[:]), [R3], [Rkh])
                if KSTOP <= 3: return
                act(T4[:], P[2][:], AF.Silu, [RP[2]], [R4])
                if KSTOP <= 4: return
                pv3 = P[3][:].rearrange("p (a b) -> p a b", a=4)
                op("dve", lambda e: e.tensor_reduce(out=nm[:], in_=pv3, axis=AX.X, op=ALU.add), [RP[3]], [Rnm])
                ts("dve", nm[:], nm[:], -1.0 / 128, None, ALU.mult, None, [Rnm], [Rnm])
                for blk in range(4):
                    ts("dve", vtok[:, blk, 0:128], P[3][:, blk * 128:(blk + 1) * 128], nm[:, blk:blk + 1], None,
                       ALU.add, None, [RP[3], Rnm], [Rvt])
                if KSTOP <= 5: return
                p4b = P[4][:].bitcast(BF16)
                for blk in range(4):
                    tr(p4b[:, blk * 128:(blk + 1) * 128], kh[:, blk * 128:(blk + 1) * 128], idb[:], [Rkh, Ridb], [RP[4]])
                ts("dve", ktok[:].rearrange("p a b -> p (a b)"), p4b[:, 0:512], cst[:, C_KDEC + h:C_KDEC + h + 1], None,
                   ALU.mult, None, [RP[4], Rcst], [Rkt])
                if KSTOP <= 6: return
                g128 = GAM[h] ** 128
                sbank = (5, 4)

                def scores(blk):
                    bs_ = slice(blk * 128, (blk + 1) * 128)
                    pb_ = sbank[blk % 2]
                    st_, rst_ = stmA[blk % 2]
                    mm(P[pb_][:, 0:128], [(kh[:, bs_], qh[:, bs_])], [Rkh, Rqh], [RP[pb_]])
                    tt("dve", st_[:], P[pb_][:, 0:128], cst[:, C_RMASK + h * 128:C_RMASK + (h + 1) * 128], ALU.mult,
                       [RP[pb_], Rcst], [rst_])

                scores(0)
                for blk in range(4):
                    bs = slice(blk * 128, (blk + 1) * 128)
                    if blk + 1 < 4:
                        scores(blk + 1)
                    st_, rst_ = stmA[blk % 2]
                    mm(P[6][:, bs], [(vtok[:, blk, 0:128], st_[:]), (Sretb[:, h, :], qt[:, bs])],
                       [Rvt, rst_, RSrb[h], Rqt], [RP[6]])
                    mm(P[7][:, 0:128], [(ktok[:, blk, :], vtok[:, blk, 0:128])], [Rkt, Rvt], [RP[7]])
                    stt(Sret[:, h, :], Sret[:, h, :], g128, P[7][:, 0:128], ALU.mult, ALU.add, [RSr[h], RP[7]], [RSr[h]])
                    acopy(Sretb[:, h, :], Sret[:, h, :], [RSr[h]], [RSrb[h]])
                if KSTOP <= 7: return
                headnorm(6, 7, T4, R4, pcol(l, 44 + h), ybuf[1][:, h, :], RY[1][h])
                if debug and l == 0:
                    fw.dma("sp", [(dbg_d["yb"][h, :, tsl(tc)], ybuf[1][:, h, :])], reads=[RY[1][h]])
            unit(specs, f)

        def gla_prelude(l, tc):
            win = w_in_d[l]
            specs = [[(0, 8, 128, wseg(win[:, O_GA:O_GA + 128], 8))]]

            def f(pc):
                (sl, rs) = pc[0]
                wv = sl[:, 0:1024].rearrange("p (k n) -> p k n", k=8)
                mm(P[0][:, :], [(wv[:, k, :], hT[:, k, :]) for k in range(KC)], [rs] + RH, [RP[0]])
                acopy(gdb[:], P[0][:, :], [RP[0]], [Rgdb])
                for half in range(2):
                    pb = 1 + half
                    for j in range(2):
                        blk = half * 2 + j
                        mm(P[pb][:, j * 256:(j + 1) * 256], [(gdb[:, blk * 128:(blk + 1) * 128], wa2[:, l, :])],
                           [Rgdb, Rwa2], [RP[pb]])
                    sv = spb[:, half * 2:half * 2 + 2, :]
                    tt("dve", sv, P[pb][:].rearrange("p (a b) -> p a b", a=2),
                       bA[:, l, :].unsqueeze(1).to_broadcast([128, 2, 256]), ALU.add, [RP[pb], RbA], [Rspb])
                    act(sv, sv, AF.Exp, [Rspb], [Rspb], scale=-1.0)
                    act(sv, sv, AF.Ln, [Rspb], [Rspb], bias=1.0, scale=1.0)
                for hp in range(2):
                    for blk in range(4):
                        mm(P[3 + hp][:, blk * 128:(blk + 1) * 128],
                           [(spb[:, blk, hp * 128:(hp + 1) * 128], cst[:, C_UNEG:C_UNEG + 128])], [Rspb, Rcst], [RP[3 + hp]])
            unit(specs, f)

        def gla_unit(l, tc, hp):
            win = w_in_d[l]
            specs = [[(0, 8, 128, wseg(win[:, O_GQ + hp * 128:O_GQ + (hp + 1) * 128], 8)),
                      (1024, 8, 128, wseg(win[:, O_GK + hp * 128:O_GK + (hp + 1) * 128], 8)),
                      (2048, 8, 256, wseg(win[:, O_GV + hp * 256:O_GV + (hp + 1) * 256], 8))],
                     [(0, 8, 256, wseg(win[:, O_GR + hp * 256:O_GR + (hp + 1) * 256], 8))]]

            def f(pc):
                (sl, rs), (sl2, rs2) = pc
                wq = sl[:, 0:1024].rearrange("p (k n) -> p k n", k=8)
                wk = sl[:, 1024:2048].rearrange("p (k n) -> p k n", k=8)
                wvv = sl[:, 2048:4096].rearrange("p (k n) -> p k n", k=8)
                wr = sl2[:, 0:2048].rearrange("p (k n) -> p k n", k=8)
                pc_ = 3 + hp
                act(T0[:], P[pc_][:], AF.Exp, [RP[pc_]], [R0])
                act(T2[:], P[pc_][:], AF.Exp, [RP[pc_]], [R2], scale=-1.0)
                proj(0, wq, 0, rs)
                proj(1, wk, 0, rs)
                for e_ in range(2):
                    ps = slice(64 * e_, 64 * e_ + 64)
                    stt(gq[e_][0][ps, :], P[0][ps, :], 0.125, T0[ps, :], ALU.mult, ALU.mult, [RP[0], R0], [gq[e_][1]])
                tt("dve", kh[:], P[1][:], T2[:], ALU.mult, [RP[1], R2], [Rkh])
                for half, pb in ((0, 2), (1, 5)):
                    for j in range(2):
                        proj_tok(pb, 256, wvv, 0, rs, half * 2 + j, j * 256)
                    acopy(vtok[:, half * 2:half * 2 + 2, :], P[pb][:].rearrange("p (a b) -> p a b", a=2), [RP[pb]], [Rvt])
                p6b = P[6][:].bitcast(BF16)
                for blk in range(4):
                    tr(p6b[:, blk * 128:(blk + 1) * 128], kh[:, blk * 128:(blk + 1) * 128], idb[:], [Rkh, Ridb], [RP[6]])
                acopy(ktok[:].rearrange("p a b -> p (a b)"), p6b[:, 0:512], [RP[6]], [Rkt])
                proj(7, wr, 0, rs2)
                act(T4[:], P[7][:], AF.Silu, [RP[7]], [R4])
                proj(5, wr, 128, rs2)
                act(T5[:], P[5][:], AF.Silu, [RP[5]], [R5])
                for blk in range(4):
                    bs = slice(blk * 128, (blk + 1) * 128)
                    for e_ in range(2):
                        gq_, rgq_ = gq[e_]
                        mm(P[0][:, 0:128], [(kh[:, bs], gq_[:, bs])], [Rkh, rgq_], [RP[0]])
                        tt("dve", stm[:], P[0][:, 0:128], cst[:, C_CAUS:C_CAUS + 128], ALU.mult, [RP[0], Rcst], [Rstm])
                        mm(P[1 + e_][:, bs], [(vtok[:, blk, e_ * 128:(e_ + 1) * 128], stm[:]), (Sglab[:, hp, :], gq_[:, bs])],
                           [Rvt, Rstm, RSgb[hp], rgq_], [RP[1 + e_]])
                    for e_ in range(2):
                        mm(P[6][:, e_ * 128:(e_ + 1) * 128], [(ktok[:, blk, :], vtok[:, blk, e_ * 128:(e_ + 1) * 128])],
                           [Rkt, Rvt], [RP[6]])
                    for e_ in range(2):
                        ps = slice(64 * e_, 64 * e_ + 64)
                        tt("dve", tS[ps, :], P[6][ps, e_ * 128:(e_ + 1) * 128], Sgla[ps, hp, :], ALU.add, [RP[6], RSg[hp]], [RtS])
                    ts("dve", Sgla[:, hp, :], tS[:], T0[:, blk * 128 + 127:blk * 128 + 128], None, ALU.mult, None,
                       [RtS, R0], [RSg[hp]])
                    acopy(Sglab[:, hp, :], Sgla[:, hp, :], [RSg[hp]], [RSgb[hp]])
                for e_, (sg_t, sg_r) in enumerate(((T4, R4), (T5, R5))):
                    hh = 2 * hp + e_
                    headnorm(1 + e_, 7, sg_t, sg_r, pcol(l, 48 + hh), ybuf[2][:, hh, :], RY[2][hh])
                    if debug and l == 0:
                        fw.dma("sp", [(dbg_d["yc"][hh, :, tsl(tc)], ybuf[2][:, hh, :])], reads=[RY[2][hh]])
            unit(specs, f)

        def merge_unit(l, tc, i):
            win = w_in_d[l]
            c0 = O_GATE + i * 1024
            specs = [[(0, 8, 512, wseg(win[:, c0:c0 + 512], 8))],
                     [(0, 8, 512, wseg(win[:, c0 + 512:c0 + 1024], 8))],
                     [(0, 4, 1024, wseg(wbr_d[i][l], 4))]]

            def f(pc):
                (slb, rsb) = pc[2]
                wb_ = slb[:, 0:4096].rearrange("p (k n) -> p k n", k=4)
                for half in range(2):
                    (sl, rs) = pc[half]
                    wg_ = sl[:, 0:4096].rearrange("p (k n) -> p k n", k=8)
                    for j in range(4):
                        oc = half * 4 + j
                        pa, pb = (0, 1) if j % 2 == 0 else (2, 3)
                        proj(pa, wg_, j * 128, rs)
                        mm(P[pb][:], [(wb_[:, kk, oc * 128:(oc + 1) * 128], ybuf[i][:, kk, :]) for kk in range(4)],
                           [rsb] + RY[i], [RP[pb]])
                        tw, rw = (T0, R0) if j % 2 == 0 else (T2, R2)
                        act(tw[:], P[pa][:], AF.Tanh, [RP[pa]], [rw], scale=0.5)
                        if i == 0:
                            stt(ACC[:, oc, :], tw[:], 1.0, P[pb][:], ALU.add, ALU.mult, [rw, RP[pb]], [RA[oc]])
                        else:
                            stt(tw[:], tw[:], 1.0, P[pb][:], ALU.add, ALU.mult, [rw, RP[pb]], [rw])
                            tt("dve", ACC[:, oc, :], ACC[:, oc, :], tw[:], ALU.add, [RA[oc], rw], [RA[oc]])
                            if i == 2:
                                op("act", lambda e: e.mul(out=mrg[:, oc, :], in_=ACC[:, oc, :], mul=0.5), [RA[oc]], [RM[oc]])
            unit(specs, f)

        def out_unit(l, tc, half):
            specs = [[(0, 8, 512, wseg(wout_d[l][:, half * 512:(half + 1) * 512], 8))]]

            def f(pc):
                (sl, rs) = pc[0]
                wo = sl[:, 0:4096].rearrange("p (k n) -> p k n", k=8)
                for j in range(4):
                    oc = half * 4 + j
                    pa = j % 4
                    mm(P[pa][:], [(wo[:, k, j * 128:(j + 1) * 128], mrg[:, k, :]) for k in range(KC)], [rs] + RM, [RP[pa]])
                    acopy(ACC[:, oc, :], P[pa][:], [RP[pa]], [RA[oc]])
            unit(specs, f)

        def ffn_unit(l, tc, g):
            NG = NF // 2
            specs = []
            if g < NG:
                c0 = g * 256
                specs.append([(0, 8, 256, wseg(wg_d[l][:, c0:c0 + 256], 8)),
                              (2048, 8, 256, wseg(wu_d[l][:, c0:c0 + 256], 8))])
            if g >= 1:
                cd = (g - 1) * 256
                specs.append([(0, 2, 1024, wseg(wd_d[l][cd:cd + 256, :], 2))])

            def f(pc):
                if g < NG:
                    (sl, rs) = pc[0]
                    wg_ = sl[:, 0:2048].rearrange("p (k n) -> p k n", k=8)
                    wu_ = sl[:, 2048:4096].rearrange("p (k n) -> p k n", k=8)
                    par = g % 2
                    for j in range(2):
                        pa, pb = (0, 1) if j == 0 else (2, 3)
                        proj(pa, wg_, j * 128, rs)
                        proj(pb, wu_, j * 128, rs)
                        tw, rw = (T0, R0) if j == 0 else (T2, R2)
                        act(tw[:], P[pa][:], AF.Silu, [RP[pa]], [rw])
                        tt("dve", actb[:, par, j, :], tw[:], P[pb][:], ALU.mult, [rw, RP[pb]], [RACT[par][j]])
                if g >= 1:
                    (sl2, rs2) = pc[-1]
                    wd_ = sl2[:, 0:2048].rearrange("p (k n) -> p k n", k=2)
                    gd = g - 1
                    pard = gd % 2
                    for oc in range(KC):
                        pa = 4 + oc % 3
                        mm(P[pa][:], [(wd_[:, j, oc * 128:(oc + 1) * 128], actb[:, pard, j, :]) for j in range(2)],
                           [rs2] + RACT[pard], [RP[pa]])
                        if gd == 0:
                            acopy(ACC[:, oc, :], P[pa][:], [RP[pa]], [RA[oc]])
                        else:
                            tt("dve", ACC[:, oc, :], ACC[:, oc, :], P[pa][:], ALU.add, [RA[oc], RP[pa]], [RA[oc]])
            unit(specs, f)

        ROUT = [Reg(), Reg()]

        def store_x(tb):
            def f(_):
                tc = tb // 4
                ot, ro = xin[tb % 2]
                for half in range(2):
                    pb = (tb * 2 + half) % 4
                    for j in range(4):
                        k = half * 4 + j
                        tr(P[pb][:, j * 128:(j + 1) * 128], x[:, k, tb * 128:(tb + 1) * 128], cst[:, C_ID:C_ID + 128],
                           [RX[k][tc], Rcst], [RP[pb]])
                    if half == 0:
                        acopy(ot[:, 0:512], P[pb][:], [RP[pb]], ro)
                    else:
                        op("dve", lambda e: e.tensor_copy(out=ot[:, 512:1024], in_=P[pb][:]), [RP[pb]], ro)
                fw.dma("sp", [(out_d[tb * 128:(tb + 1) * 128, :], ot)], reads=ro, owner=ROUT[tb % 2])
            return f

        for l in range(depth):
            unit([], reset_states)
            for tc in range(ntc):
                cur["l"] = l
                cur["n"] = 0
                unit([], prenorm(l, tc, 0))
                if "A" in phases:
                    for c in range(4):
                        conv_unit(l, tc, c)
                if "B" in phases:
                    if os.environ.get('KROPE', '1') == '1':
                        unit([], rope_tables(tc))
                    for h in range(4):
                        ret_unit(l, tc, h)
                if "C" in phases:
                    gla_prelude(l, tc)
                    for hp in range(2):
                        gla_unit(l, tc, hp)
                if "D" in phases:
                    for i in range(3):
                        merge_unit(l, tc, i)
                if "E" in phases:
                    for half in range(2):
                        out_unit(l, tc, half)
                    unit([], postnorm(l, tc, 8, "xmix"))
                if "F" in phases:
                    unit([], prenorm(l, tc, 16))
                    for g in range(NF // 2 + 1):
                        ffn_unit(l, tc, g)
                    unit([], postnorm(l, tc, 24, "xffn"))
        for tb in range(16):
            unit([], store_x(tb))

        pieces = []
        upieces = []
        for ui, (specs, fn, keys) in enumerate(units):
            idxs = []
            for segs, key in zip(specs, keys):
                idxs.append(len(pieces))
                pieces.append((ui, segs, key))
            upieces.append(idxs)
        GSZ = 6
        first = {}
        for (ui, segs, key) in pieces:
            first.setdefault(key, segs)
        npc = max([k[1] for k in first] + [0]) + 1
        scr = [nc.dram_tensor("scr%d" % l, [npc, 128, SLOT], BF16).ap() for l in range(max(depth, 1))]
        ngrp = (npc + GSZ - 1) // GSZ
        RG = [[Reg() for _ in range(ngrp)] for _ in range(max(depth, 1))]
        for l in range(depth):
            for g in range(ngrp):
                csegs = []
                for j in range(g * GSZ, min(npc, (g + 1) * GSZ)):
                    if (l, j) not in first:
                        continue
                    for (off, kc, n, src) in first[(l, j)]:
                        dst = scr[l][j][:, off:off + kc * n].rearrange("p (k n) -> p k n", k=kc)
                        csegs.append((dst, src))
                if csegs:
                    fw.dma("pool", csegs, writes=[RG[l][g]])
        nextp = 0

        def issue_loads(cur_unit):
            nonlocal nextp
            while nextp < len(pieces):
                if nextp >= NSLOT and pieces[nextp - NSLOT][0] >= cur_unit:
                    break
                ui, segs, (l_, j_) = pieces[nextp]
                s_ = nextp % NSLOT
                L = max(off + kc * n for (off, kc, n, src) in segs)
                fw.dma("sp", [(slots[s_][:, 0:L], scr[l_][j_][:, 0:L])], reads=[RG[l_][j_ // GSZ]], writes=[RS[s_]])
                nextp += 1

        for ui, (specs, fn, keys) in enumerate(units):
            issue_loads(ui)
            for pi_ in upieces[ui]:
                assert pi_ < nextp, "piece not loaded: raise NSLOT"
            fn([(slots[pi_ % NSLOT], RS[pi_ % NSLOT]) for pi_ in upieces[ui]])

        fw.wait_all("sp", xin[0][1] + xin[1][1])
        if debug:
            fw.wait_all("sp", [r for rr in RY for r in rr] + [r for rr in RX for r in rr])
    return nc


def _consts():
    c = np.zeros((128, NCST), np.float64)
    p = np.arange(128)
    half = 64
    invf = 10000.0 ** (-(np.arange(half, dtype=np.float32) / np.float32(half)))
    c[:, C_INVF] = np.concatenate([invf, invf]).astype(np.float32)
    c[:, C_SIGN] = np.where(p < 64, -1.0, 1.0)
    i = np.arange(128)
    for h in range(4):
        lg = math.log(1.0 - 2.0 ** (-5.0 - h))
        c[:, C_KDEC + h] = np.exp(lg * (127 - p))
        diff = i[None, :] - p[:, None]
        c[:, C_RMASK + h * 128:C_RMASK + (h + 1) * 128] = np.where(diff >= 0, np.exp(lg * np.maximum(diff, 0)), 0.0) * 128 ** -0.5
        c[:, C_QDEC + h * 128:C_QDEC + (h + 1) * 128] = (np.exp(lg * (i + 1.0)) * 128 ** -0.5)[None, :]
    c[:, C_CAUS:C_CAUS + 128] = (i[None, :] >= p[:, None]).astype(np.float64)
    c[:, C_UNEG:C_UNEG + 128] = np.where(p[:, None] <= i[None, :], -1.0 / 16.0, 0.0)
    c[:, C_ID:C_ID + 128] = np.eye(128)
    return c.astype(np.float32)


_CACHE = {}


def kernel(x, positions, norm_mix_pre, w_in, conv_w, ret_gn_w, gla_w_a2, gla_b_a, gla_gn_w,
           w_branch_a, w_branch_b, w_branch_c, w_out, norm_mix_post, norm_ffn_pre,
           w_ffn_gate, w_ffn_up, w_ffn_down, norm_ffn_post, _debug=False, _dd=DEPTH, _ncores=8):
    f32 = lambda a: np.ascontiguousarray(np.asarray(a, dtype=np.float32))
    x = f32(x)
    positions = np.ascontiguousarray(np.asarray(positions, dtype=np.int32))
    pv = np.zeros((128, _dd * LP), np.float32)
    for l in range(_dd):
        b = l * LP
        for j, g in enumerate((norm_mix_pre, norm_mix_post, norm_ffn_pre, norm_ffn_post)):
            pv[:, b + 8 * j:b + 8 * j + 8] = f32(g)[l].reshape(8, 128).T
        pv[:, b + 32:b + 44] = f32(conv_w)[l].reshape(3, 4, 128).transpose(2, 0, 1).reshape(128, 12)
        pv[:, b + 44:b + 48] = f32(ret_gn_w)[l].reshape(4, 128).T
        pv[:, b + 48:b + 52] = f32(gla_gn_w)[l].reshape(4, 128).T
    bA = np.ascontiguousarray(np.broadcast_to(f32(gla_b_a)[:_dd, None, :], (_dd, 128, 256)))
    shared = {
        "w_in": f32(w_in[:_dd]), "gla_w_a2": f32(gla_w_a2[:_dd]), "bA": bA,
        "w_branch_a": f32(w_branch_a[:_dd]), "w_branch_b": f32(w_branch_b[:_dd]), "w_branch_c": f32(w_branch_c[:_dd]),
        "w_out": f32(w_out[:_dd]), "w_ffn_gate": f32(w_ffn_gate[:_dd]), "w_ffn_up": f32(w_ffn_up[:_dd]),
        "w_ffn_down": f32(w_ffn_down[:_dd]), "pv": pv, "cst": _consts(),
    }
    key = bool(_debug)
    if key not in _CACHE:
        _CACHE[key] = build_program(debug=_debug)
    nc = _CACHE[key]
    in_maps = []
    for b in range(_ncores):
        m = dict(shared)
        m["x"] = x[b]
        m["pos"] = positions[b:b + 1]
        in_maps.append(m)
    res = run_bass_kernel_spmd(nc, in_maps, core_ids=list(range(_ncores)))
    out = np.stack([np.asarray(r["out"], dtype=np.float32) for r in res.results], axis=0)
    if _debug:
        kernel.last = res.results
    return out
```
